# Optimizing a Trainium2 kernel written in Bass

```python
import math
import jax
import jax.numpy as jnp
from jax import lax
import numpy as np

D_MODEL = 2048
BATCH = 2
SEQ = 4096
DEPTH = 4

CTX_LEN = 256
GRID_W = 64
EPS = 1e-6
N_DIR = 2

GLA_HEADS = 4
GLA_DK = 128
GLA_DV = 256
GLA_KEY = GLA_HEADS * GLA_DK
GLA_WIDTH = GLA_HEADS * GLA_DV
GLA_RANK = 16
GLA_TAU = 16.0
GLA_CHUNK = 64

S5_WIDTH = 512
S5_GROUP = 16
S5_GROUPS = S5_WIDTH // S5_GROUP
S5_STATE = 64

HY_WIDTH = 512
HY_ORDER = 2
HY_EMB = 33
HY_BANDS = (HY_EMB - 1) // 2
HY_FFN = 64
HY_SHORT = 3
HY_MIN_DECAY = math.log(1e-2) / 0.3
HY_MAX_DECAY = math.log(1e-2) / 1.5

MIX_WIDTH = GLA_WIDTH + S5_WIDTH + HY_WIDTH
IN_SPLITS = (GLA_KEY, GLA_KEY, GLA_WIDTH, N_DIR * GLA_RANK, S5_WIDTH,
             GLA_WIDTH, S5_WIDTH, 3 * HY_WIDTH, HY_WIDTH)
N_STATE_SPLITS = 5
IN_WIDTH = sum(IN_SPLITS)
STATE_WIDTH = sum(IN_SPLITS[:N_STATE_SPLITS])

kernel_name = "hybrid_gla_s5_hyena_prefix_trunk"


def rmsnorm(x, g):
    xf = x.astype(jnp.float32)
    y = xf * lax.rsqrt(jnp.mean(xf * xf, axis=-1, keepdims=True) + EPS)
    return (y * g.astype(jnp.float32)).astype(x.dtype)


def split_cols(p, sizes):
    idx = np.cumsum(sizes)[:-1].tolist()
    return jnp.split(p, idx, axis=-1)


def grid_transpose(h, a, b):
    return h.reshape(h.shape[0], a, b, h.shape[-1]).transpose(0, 2, 1, 3).reshape(h.shape)


def gla_chunked(q, k, v, g, s0, with_out):
    bn, L, H, dk = q.shape
    n = L // GLA_CHUNK
    C = GLA_CHUNK
    rs = lambda t: t.reshape(bn, n, C, H, t.shape[-1])
    q, k, v, g = rs(q), rs(k), rs(v), rs(g)
    G = jnp.cumsum(g, axis=2)
    G_last = G[:, :, -1:]
    dS = jnp.einsum('bnshk,bnshv->bnhkv', k * jnp.exp(G_last - G), v)
    a = jnp.exp(G_last[:, :, 0])

    def step(S, inp):
        a_n, dS_n = inp
        return a_n[..., None] * S + dS_n, (S if with_out else None)

    S_fin, S_prev = lax.scan(step, s0, (jnp.moveaxis(a, 1, 0), jnp.moveaxis(dS, 1, 0)))
    if not with_out:
        return None, S_fin
    S_prev = jnp.moveaxis(S_prev, 0, 1)
    qg = q * jnp.exp(G)
    att = jnp.einsum('bnthk,bnshk->bnhts', qg, k * jnp.exp(-G))
    att = jnp.where(jnp.tril(jnp.ones((C, C), dtype=bool)), att, 0.0)
    o = jnp.einsum('bnhts,bnshv->bnthv', att, v) + jnp.einsum('bnthk,bnhkv->bnthv', qg, S_prev)
    return o.reshape(bn, L, H, v.shape[-1]), S_fin


def gla_branch(pc, pl, w_gate, b_gate, norm_g, ctx_out):
    f32 = jnp.float32

    def prep(q, k, v, lr):
        bn, L, _ = q.shape
        qh = q.astype(f32).reshape(bn, L, GLA_HEADS, GLA_DK) * (GLA_DK ** -0.5)
        kh = k.astype(f32).reshape(bn, L, GLA_HEADS, GLA_DK)
        vh = v.astype(f32).reshape(bn, L, GLA_HEADS, GLA_DV)
        lr = lr.astype(f32).reshape(bn, L, N_DIR, GLA_RANK)
        z = jnp.einsum('bldr,drk->bldk', lr, w_gate.astype(f32)) + b_gate.astype(f32)
        glog = (jax.nn.log_sigmoid(z) / GLA_TAU).reshape(bn, L, N_DIR, GLA_HEADS, GLA_DK)
        return qh, kh, vh, glog

    flip = lambda t: jnp.flip(t, axis=1)
    qc, kc, vc, gc = prep(*pc)
    ql, kl, vl, gl = prep(*pl)
    zero = jnp.zeros((qc.shape[0], GLA_HEADS, GLA_DK, GLA_DV), f32)
    oc_f, s_f = gla_chunked(qc, kc, vc, gc[:, :, 0], zero, ctx_out)
    oc_b, s_b = gla_chunked(flip(qc), flip(kc), flip(vc), flip(gc[:, :, 1]), zero, ctx_out)
    ol_f, _ = gla_chunked(ql, kl, vl, gl[:, :, 0], s_f, True)
    ol_b, _ = gla_chunked(flip(ql), flip(kl), flip(vl), flip(gl[:, :, 1]), s_b, True)
    yl = ol_f + flip(ol_b) - jnp.sum(ql * kl, -1, keepdims=True) * vl
    out_l = rmsnorm(yl, norm_g).reshape(yl.shape[0], yl.shape[1], GLA_WIDTH)
    if not ctx_out:
        return out_l, None
    yc = oc_f + flip(oc_b) - jnp.sum(qc * kc, -1, keepdims=True) * vc
    out_c = rmsnorm(yc, norm_g).reshape(yc.shape[0], yc.shape[1], GLA_WIDTH)
    return out_l, out_c


def _lin_op(e1, e2):
    a1, b1 = e1
    a2, b2 = e2
    return a1 * a2, a2 * b1 + b2


def s5_scan(bu, lam_bar, s0, reverse):
    if s0 is not None:
        bu = bu.at[:, -1 if reverse else 0].add(lam_bar * s0)
    a = jnp.broadcast_to(lam_bar, bu.shape)
    _, xs = lax.associative_scan(_lin_op, (a, bu), reverse=reverse, axis=1)
    return xs


def s5_branch(uc, ul, lam_re, lam_im, log_step, b_re, b_im, c_re, c_im, d_skip, w_glu, b_glu, ctx_out):
    f32 = jnp.float32
    lam = lax.complex(lam_re.astype(f32), lam_im.astype(f32))
    lam_bar = jnp.exp(lam * jnp.exp(log_step.astype(f32))[..., None])
    b_bar = ((lam_bar - 1.0) / lam)[..., None] * lax.complex(b_re.astype(f32), b_im.astype(f32))
    cmat = lax.complex(c_re.astype(f32), c_im.astype(f32))
    d = d_skip.astype(f32)
    wg = w_glu.astype(f32)
    bg = b_glu.astype(f32)

    def grouped(u):
        u = u.astype(f32)
        return u, u.reshape(u.shape[0], u.shape[1], S5_GROUPS, S5_GROUP)

    def readout(xs, cm):
        y = jnp.einsum('blgp,ghp->blgh', xs, cm).real
        return y.reshape(y.shape[0], y.shape[1], S5_WIDTH)

    def glu(y):
        yg = jax.nn.gelu(y)
        return yg * jax.nn.sigmoid(yg @ wg + bg)

    uc, ucg = grouped(uc)
    ul, ulg = grouped(ul)
    yl = d * ul
    yc = d * uc if ctx_out else None
    for di, rev in enumerate((False, True)):
        xs_c = s5_scan(jnp.einsum('blgh,gph->blgp', ucg, b_bar[di]), lam_bar[di], None, rev)
        s_c = xs_c[:, 0] if rev else xs_c[:, -1]
        xs_l = s5_scan(jnp.einsum('blgh,gph->blgp', ulg, b_bar[di]), lam_bar[di], s_c, rev)
        yl = yl + readout(xs_l, cmat[di])
        if ctx_out:
            yc = yc + readout(xs_c, cmat[di])
    return glu(yl), (glu(yc) if ctx_out else None)


def hyena_spectra(L, w1, b1, f1, w2, b2, f2, w3):
    f32 = jnp.float32
    t = jnp.linspace(0.0, 1.0, L, dtype=f32)[:, None]
    ang = (2.0 * math.pi / L) * jnp.arange(L, dtype=f32)[:, None]
    bands = jnp.linspace(1e-4, HY_BANDS - 1, HY_BANDS, dtype=f32)[None, :]
    feats = jnp.concatenate([t, jnp.cos(bands * ang), -jnp.sin(bands * ang)], axis=-1)
    h = jnp.sin(f1.astype(f32) * (feats @ w1.astype(f32) + b1.astype(f32)))
    h = jnp.sin(f2.astype(f32) * (h @ w2.astype(f32) + b2.astype(f32)))
    h = (h @ w3.astype(f32)).reshape(L, HY_ORDER, N_DIR, HY_WIDTH)
    deltas = jnp.abs(jnp.linspace(HY_MIN_DECAY, HY_MAX_DECAY, HY_WIDTH, dtype=f32))
    h = h * jnp.exp(-t * deltas)[:, None, None, :]
    h_fwd, h_bwd = h[:, :, 0], h[:, :, 1]
    taps = jnp.concatenate([h_fwd, jnp.zeros_like(h_fwd[:1]), h_bwd[:0:-1]], axis=0)
    return jnp.fft.rfft(taps, axis=0)


def short_conv(x, w, b):
    pad = HY_SHORT // 2
    L = x.shape[1]
    xp = jnp.pad(x, ((0, 0), (pad, pad), (0, 0)))
    return sum(xp[:, j:j + L] * w[j] for j in range(HY_SHORT)) + b


def long_conv(u, spec, d):
    L = u.shape[1]
    U = jnp.fft.rfft(u, n=2 * L, axis=1)
    y = jnp.fft.irfft(U * spec[None], n=2 * L, axis=1)[:, :L]
    return y + d * u


def hyena_seq(p, conv_w, conv_b, spec, hy_d):
    f32 = jnp.float32
    s = short_conv(p.astype(f32), conv_w.astype(f32), conv_b.astype(f32))
    v, x1, x2 = jnp.split(s, 3, axis=-1)
    d = hy_d.astype(f32)
    z = x1 * long_conv(v, spec[:, 0], d[0])
    return x2 * long_conv(z, spec[:, 1], d[1])


def setup_inputs(seed: int = 0) -> dict:
    key = jax.random.key(seed)
    ks = iter(jax.random.split(key, 40))
    f32 = jnp.float32
    nrm = lambda shape, s: s * jax.random.normal(next(ks), shape, f32)
    x = nrm((BATCH, SEQ, D_MODEL), 1.0)
    c = nrm((BATCH, D_MODEL), 1.0)
    ctx = nrm((BATCH, CTX_LEN, D_MODEL), 1.0)
    c_ctx = nrm((D_MODEL,), 1.0)
    w_mod = nrm((DEPTH, D_MODEL, 3 * D_MODEL), 0.5 * D_MODEL ** -0.5)
    b_mod = nrm((DEPTH, 3 * D_MODEL), 0.01)
    g_pre = 1.0 + nrm((DEPTH, D_MODEL), 0.01)
    g_post = 1.0 + nrm((DEPTH, D_MODEL), 0.01)
    w_in = nrm((DEPTH, D_MODEL, IN_WIDTH), D_MODEL ** -0.5)
    w_out = nrm((DEPTH, MIX_WIDTH, D_MODEL), MIX_WIDTH ** -0.5)
    gla_w_gate = nrm((DEPTH, N_DIR, GLA_RANK, GLA_KEY), GLA_RANK ** -0.5)
    gla_b_gate = nrm((DEPTH, N_DIR, GLA_KEY), 0.1)
    gla_norm = 1.0 + nrm((DEPTH, GLA_DV), 0.01)
    s5_lam_re = -0.5 + nrm((DEPTH, N_DIR, S5_GROUPS, S5_STATE), 0.01)
    s5_lam_im = math.pi * jnp.arange(S5_STATE, dtype=f32) + nrm((DEPTH, N_DIR, S5_GROUPS, S5_STATE), 0.01)
    s5_log_step = jax.random.uniform(next(ks), (DEPTH, N_DIR, S5_GROUPS), f32, math.log(1e-3), math.log(1e-1))
    s5_b_re = nrm((DEPTH, N_DIR, S5_GROUPS, S5_STATE, S5_GROUP), (2 * S5_GROUP) ** -0.5)
    s5_b_im = nrm((DEPTH, N_DIR, S5_GROUPS, S5_STATE, S5_GROUP), (2 * S5_GROUP) ** -0.5)
    s5_c_re = nrm((DEPTH, N_DIR, S5_GROUPS, S5_GROUP, S5_STATE), S5_STATE ** -0.5)
    s5_c_im = nrm((DEPTH, N_DIR, S5_GROUPS, S5_GROUP, S5_STATE), S5_STATE ** -0.5)
    s5_d = nrm((DEPTH, S5_WIDTH), 1.0)
    s5_w_glu = nrm((DEPTH, S5_WIDTH, S5_WIDTH), S5_WIDTH ** -0.5)
    s5_b_glu = nrm((DEPTH, S5_WIDTH), 0.01)
    hy_conv_w = nrm((DEPTH, HY_SHORT, 3 * HY_WIDTH), HY_SHORT ** -0.5)
    hy_conv_b = nrm((DEPTH, 3 * HY_WIDTH), 0.01)
    hy_w1 = nrm((DEPTH, HY_EMB, HY_FFN), HY_EMB ** -0.5)
    hy_b1 = nrm((DEPTH, HY_FFN), 0.01)
    hy_f1 = 1.0 + nrm((DEPTH, HY_FFN), 0.01)
    hy_w2 = nrm((DEPTH, HY_FFN, HY_FFN), HY_FFN ** -0.5)
    hy_b2 = nrm((DEPTH, HY_FFN), 0.01)
    hy_f2 = 1.0 + nrm((DEPTH, HY_FFN), 0.01)
    hy_w3 = nrm((DEPTH, HY_FFN, HY_ORDER * N_DIR * HY_WIDTH), 0.02)
    hy_d = nrm((DEPTH, HY_ORDER, HY_WIDTH), 1.0)
    return {"x": x, "c": c, "ctx": ctx, "c_ctx": c_ctx, "w_mod": w_mod, "b_mod": b_mod,
            "g_pre": g_pre, "g_post": g_post, "w_in": w_in, "w_out": w_out,
            "gla_w_gate": gla_w_gate, "gla_b_gate": gla_b_gate, "gla_norm": gla_norm,
            "s5_lam_re": s5_lam_re, "s5_lam_im": s5_lam_im, "s5_log_step": s5_log_step,
            "s5_b_re": s5_b_re, "s5_b_im": s5_b_im, "s5_c_re": s5_c_re, "s5_c_im": s5_c_im,
            "s5_d": s5_d, "s5_w_glu": s5_w_glu, "s5_b_glu": s5_b_glu,
            "hy_conv_w": hy_conv_w, "hy_conv_b": hy_conv_b, "hy_w1": hy_w1, "hy_b1": hy_b1,
            "hy_f1": hy_f1, "hy_w2": hy_w2, "hy_b2": hy_b2, "hy_f2": hy_f2, "hy_w3": hy_w3,
            "hy_d": hy_d}


def reference(x, c, ctx, c_ctx, w_mod, b_mod, g_pre, g_post, w_in, w_out,
              gla_w_gate, gla_b_gate, gla_norm,
              s5_lam_re, s5_lam_im, s5_log_step, s5_b_re, s5_b_im, s5_c_re, s5_c_im,
              s5_d, s5_w_glu, s5_b_glu,
              hy_conv_w, hy_conv_b, hy_w1, hy_b1, hy_f1, hy_w2, hy_b2, hy_f2, hy_w3, hy_d):
    f32 = jnp.float32
    L = x.shape[1]
    Lc = ctx.shape[1]
    rows = L // GRID_W
    silu_c = jax.nn.silu(c)
    silu_cc = jax.nn.silu(c_ctx)
    xc = ctx
    for l in range(DEPTH):
        last = l == DEPTH - 1
        ctx_out = not last
        col_major = l % 2 == 1
        shift, scale, gate = jnp.split(silu_c @ w_mod[l] + b_mod[l], 3, axis=-1)
        h = rmsnorm(x, g_pre[l]) * (1 + scale[:, None]) + shift[:, None]
        if col_major:
            h = grid_transpose(h, rows, GRID_W)
        cshift, cscale, cgate = jnp.split(silu_cc @ w_mod[l] + b_mod[l], 3, axis=-1)
        hc = rmsnorm(xc, g_pre[l]) * (1 + cscale) + cshift
        lat = split_cols(h @ w_in[l], IN_SPLITS)
        if last:
            cp = split_cols(hc @ w_in[l][:, :STATE_WIDTH], IN_SPLITS[:N_STATE_SPLITS])
        else:
            cp = split_cols(hc @ w_in[l], IN_SPLITS)
        gla_l, gla_c = gla_branch(cp[:4], lat[:4], gla_w_gate[l], gla_b_gate[l], gla_norm[l], ctx_out)
        s5_l, s5_c = s5_branch(cp[4], lat[4], s5_lam_re[l], s5_lam_im[l], s5_log_step[l],
                               s5_b_re[l], s5_b_im[l], s5_c_re[l], s5_c_im[l],
                               s5_d[l], s5_w_glu[l], s5_b_glu[l], ctx_out)
        hyp = (hy_w1[l], hy_b1[l], hy_f1[l], hy_w2[l], hy_b2[l], hy_f2[l], hy_w3[l])
        hy_l = hyena_seq(lat[7], hy_conv_w[l], hy_conv_b[l], hyena_spectra(L, *hyp), hy_d[l])
        sg = lambda t: jax.nn.silu(t.astype(f32))
        y = jnp.concatenate([gla_l * sg(lat[5]), s5_l * sg(lat[6]), hy_l * sg(lat[8])], axis=-1).astype(x.dtype)
        if col_major:
            y = grid_transpose(y, GRID_W, rows)
        x_new = x + gate[:, None] * rmsnorm(y @ w_out[l], g_post[l])
        if ctx_out:
            hy_c = hyena_seq(cp[7], hy_conv_w[l], hy_conv_b[l], hyena_spectra(Lc, *hyp), hy_d[l])
            yc = jnp.concatenate([gla_c * sg(cp[5]), s5_c * sg(cp[6]), hy_c * sg(cp[8])], axis=-1).astype(xc.dtype)
            xc = xc + cgate * rmsnorm(yc @ w_out[l], g_post[l])
        x = x_new
    return x
```

```python
import math
import numpy as np
import ml_dtypes
from contextlib import ExitStack
import concourse.bass as bass
import concourse.mybir as mybir
from concourse.bass_utils import run_bass_kernel_spmd

F32 = mybir.dt.float32
BF16 = mybir.dt.bfloat16
AF = mybir.ActivationFunctionType
ALU = mybir.AluOpType
AX = mybir.AxisListType

CFG = dict(L=4096, LC=256, D=2048, W=64, DEPTH=4)
EPS = 1e-6
C_Q, C_K, C_V, C_LR, C_U, C_GG, C_SG, C_HP, C_HG = 0, 512, 1024, 2048, 2080, 2592, 3616, 4128, 5664
NCOL = 6176
PI = math.pi


class Prog:
    ENGS = ('pe', 'act', 'dve', 'pool', 'sp')
    NDSEM = 16

    def __init__(s, nc, es):
        s.nc = nc
        s.es = es
        s.sem = {e: es.enter_context(nc.semaphore('s_' + e)) for e in ('pe', 'act', 'dve', 'pool')}
        s.dsem = [es.enter_context(nc.semaphore('d%d' % i)) for i in range(s.NDSEM)]
        s.barA = es.enter_context(nc.semaphore('barA'))
        s.barB = es.enter_context(nc.semaphore('barB'))
        s.nbar = 0
        s.cnt = {e: 0 for e in s.ENGS}
        s.dcnt = 0
        s.ops = {e: [] for e in s.ENGS}
        s.waited = {e: {} for e in s.ENGS}
        s.bufs = {}
        s.uid = 0
        s.rr = 0

    def dram(s, name, shape, dt=F32, out=False):
        if out:
            return s.nc.dram_tensor(name, list(shape), dt, kind="ExternalOutput").ap()
        return s.nc.dram_tensor(name, list(shape), dt).ap()

    def _need(s, eng, tok, waits):
        if tok is None:
            return
        kind, a, v = tok
        if kind == 'c' and a == 'pe' and eng == 'pe':
            return
        key = (kind, a)
        if s.waited[eng].get(key, 0) >= v:
            return
        s.waited[eng][key] = v
        waits.append((s.sem[a] if kind == 'c' else s.dsem[a], v))

    def _deps(s, eng, reads, writes, waits, grp=None):
        for b in reads:
            st = s.bufs.setdefault(b, [[], {}, None])
            for t in st[0]:
                s._need(eng, t, waits)
        for b in writes:
            st = s.bufs.setdefault(b, [[], {}, None])
            if grp is not None and st[2] == grp:
                continue
            for t in st[0]:
                s._need(eng, t, waits)
            for (k, a), v in st[1].items():
                s._need(eng, (k, a, v), waits)

    def _commit(s, tok, reads, writes, grp=None):
        for b in reads:
            d = s.bufs[b][1]
            key = (tok[0], tok[1])
            if d.get(key, 0) < tok[2]:
                d[key] = tok[2]
        for b in writes:
            st = s.bufs[b]
            if grp is not None and st[2] == grp:
                st[0].append(tok)
            else:
                s.bufs[b] = [[tok], {}, grp]

    def op(s, eng, fn, reads=(), writes=()):
        waits = []
        s._deps(eng, reads, writes, waits)
        s.cnt[eng] += 1
        tok = ('c', eng, s.cnt[eng])
        s.ops[eng].append((waits, fn, s.sem[eng], 1))
        s._commit(tok, reads, writes)
        return tok

    def dma(s, out, in_, reads=(), writes=(), q='sp', grp=None, **kw):
        i = s.dcnt
        s.dcnt += 1
        si = i % s.NDSEM
        val = 16 * (i // s.NDSEM + 1)
        waits = []
        if val > 16:
            s._need(q, ('d', si, val - 16), waits)
        s._deps(q, reads, writes, waits, grp)
        tok = ('d', si, val)
        s.ops[q].append((waits, (lambda e: e.dma_start(out=out, in_=in_, **kw)), s.dsem[si], 16))
        s._commit(tok, reads, writes, grp)
        return tok

    def dma_k(s, out, in_, nk, reads=(), writes=(), kstep=8):
        s.uid += 1
        g = 'g%d' % s.uid
        for k0 in range(0, nk, kstep):
            k1 = min(nk, k0 + kstep)
            s.dma(out[:, k0:k1, :], in_[:, k0:k1, :], reads=reads, writes=writes, grp=g)

    def barrier(s):
        n = s.dcnt
        s.nbar += 1
        m = s.nbar
        for eng in s.ENGS:
            waits = []
            for si in range(min(n, s.NDSEM)):
                c = (n - si + s.NDSEM - 1) // s.NDSEM
                s._need(eng, ('d', si, 16 * c), waits)
            for e in ('pe', 'act', 'dve', 'pool'):
                if s.cnt[e] and e != eng:
                    s._need(eng, ('c', e, s.cnt[e]), waits)
            s.ops[eng].append((waits, 'arrive', None, 0))
        s.ops['sp'].append(([(s.barA, 5 * m)], 'clear', None, 0))
        for eng in s.ENGS:
            s.ops[eng].append(([(s.barB, m)], None, None, 0))
        s.bufs = {}
        s.cnt = {e: 0 for e in s.ENGS}
        s.dcnt = 0
        s.waited = {e: {} for e in s.ENGS}

    def emit(s):
        s.barrier()
        nc = s.nc

        def run(name, e):
            for waits, fn, sem, inc in s.ops[name]:
                for (sm, v) in waits:
                    e.wait_ge(sm, v)
                if fn is None:
                    continue
                if fn == 'arrive':
                    e.sem_inc(s.barA, 1)
                elif fn == 'clear':
                    for sm in list(s.sem.values()) + s.dsem:
                        e.sem_clear(sm)
                    e.sem_inc(s.barB, 1)
                else:
                    fn(e).then_inc(sem, inc)
            s.ops[name] = []

        with nc.Block() as block:
            @block.sync
            def _(e):
                run('sp', e)

            @block.tensor
            def _(e):
                run('pe', e)

            @block.scalar
            def _(e):
                run('act', e)

            @block.vector
            def _(e):
                run('dve', e)

            @block.gpsimd
            def _(e):
                run('pool', e)

    def ew(s):
        s.rr += 1
        return ('dve', 'pool')[s.rr % 2]


class Stage:
    def __init__(s, P, name):
        s.P = P
        s.name = name
        s.es = ExitStack()
        s.n = 0

    def __enter__(s):
        s.es.__enter__()
        return s

    def __exit__(s, *a):
        if a[0] is None:
            s.P.emit()
        return s.es.__exit__(*a)

    def sb(s, shape, dt=F32, tag='t'):
        s.n += 1
        nm = '%s_%s%d' % (s.name, tag, s.n)
        t = s.es.enter_context(s.P.nc.sbuf_tensor(nm, list(shape), dt))
        return t, nm

    def ps(s, shape, dt=F32, tag='p'):
        s.n += 1
        nm = '%s_%s%d' % (s.name, tag, s.n)
        t = s.es.enter_context(s.P.nc.psum_tensor(nm, list(shape), dt))
        return t, nm


def tiles_of(n, step):
    return [(i, min(step, n - i)) for i in range(0, n, step)]


def stage_mod(P, cfg, l, io, rep):
    D = cfg['D']
    KD = D // 128
    NW = 256
    with Stage(P, 'mod%d' % l) as st:
        cc, n_cc = st.sb([128, KD, 2])
        sc, n_sc = st.sb([128, KD, 2])
        mod, n_mod = st.sb([2, 3 * D])
        bm, n_bm = st.sb([2, 3 * D])
        sel, n_sel = st.sb([2, 2, 128])
        wt = [st.sb([128, KD, NW], tag='w') for _ in range(2)]
        pm = [st.ps([2, NW], tag='pm') for _ in range(2)]
        pr = [st.ps([128, 512], tag='pr') for _ in range(2)]
        gp, n_gp = st.sb([128, D])
        go, n_go = st.sb([128, D])
        rt = [st.sb([128, 512], tag='rt') for _ in range(3)]
        P.dma(cc[:], io['ccT'], writes=[n_cc])
        P.dma(bm[:], io['b_mod2'][l], writes=[n_bm])
        P.dma(sel[:], io['sel2'], writes=[n_sel])
        P.dma(gp[:], io['g_pre_rep'][l], writes=[n_gp])
        P.dma(go[:], io['g_post_rep'][l], writes=[n_go])
        P.op('act', lambda e: e.activation(out=sc[:], in_=cc[:], func=AF.Silu), reads=[n_cc], writes=[n_sc])
        wv = io['w_mod'][l].rearrange("(k p) n -> p k n", p=128)
        for j, (n0, nn) in enumerate(tiles_of(3 * D, NW)):
            w, n_w = wt[j % 2]
            p, n_p = pm[j % 2]
            P.dma(w[:, :, :nn], wv[:, :, n0:n0 + nn], writes=[n_w])
            for k in range(KD):
                P.op('pe', (lambda e, w=w, p=p, k=k, nn=nn: e.matmul(p[:, :nn], lhsT=sc[:, k, :], rhs=w[:, k, :nn],
                                                                      start=(k == 0), stop=(k == KD - 1))),
                     reads=[n_w, n_sc], writes=[n_p])
            P.op('dve', (lambda e, p=p, n0=n0, nn=nn: e.tensor_tensor(out=mod[:, n0:n0 + nn], in0=p[:, :nn],
                                                                      in1=bm[:, n0:n0 + nn], op=ALU.add)),
                 reads=[n_p, n_bm], writes=[n_mod])
        for r in range(2):
            for j, (n0, nn) in enumerate(tiles_of(D, 512)):
                outs = []
                for which in range(3):
                    p, n_p = pr[(j * 3 + which) % 2]
                    t, n_t = rt[which]
                    P.op('pe', (lambda e, p=p, which=which, n0=n0, nn=nn, r=r: e.matmul(
                        p[:, :nn], lhsT=sel[:, r, :], rhs=mod[:, which * D + n0: which * D + n0 + nn],
                        start=True, stop=True)), reads=[n_mod, n_sel], writes=[n_p])
                    if which == 0:
                        P.op('act', (lambda e, p=p, t=t, nn=nn: e.activation(out=t[:, :nn], in_=p[:, :nn], func=AF.Copy)),
                             reads=[n_p], writes=[n_t])
                    elif which == 1:
                        P.op('dve', (lambda e, p=p, t=t, n0=n0, nn=nn: e.scalar_tensor_tensor(
                            out=t[:, :nn], in0=p[:, :nn], scalar=1.0, in1=gp[:, n0:n0 + nn], op0=ALU.add, op1=ALU.mult)),
                             reads=[n_p, n_gp], writes=[n_t])
                    else:
                        P.op('dve', (lambda e, p=p, t=t, n0=n0, nn=nn: e.tensor_tensor(
                            out=t[:, :nn], in0=p[:, :nn], in1=go[:, n0:n0 + nn], op=ALU.mult)),
                             reads=[n_p, n_go], writes=[n_t])
                    dst = {0: 1, 1: 0, 2: 2}[which] + 3 * r
                    P.dma(rep[dst][:, n0:n0 + nn], t[:, :nn], reads=[n_t], writes=['rep%d' % dst])


def pos_row_dmas(cfg, l, i):
    L, LC, W = cfg['L'], cfg['LC'], cfg['W']
    nct = LC // 128
    if i < nct:
        return [(0, 128, 'c', (i * 128, 1, 128))]
    p0 = (i - nct) * 128
    if l % 2 == 0:
        return [(0, 128, 'l', (p0, 1, 128))]
    R = L // W
    out = []
    p = p0
    while p < p0 + 128:
        c, r = p // R, p % R
        n = min(R - r, p0 + 128 - p)
        out.append((p - p0, n, 'l', (r * W + c, W, n)))
        p += n
    return out


def row_ap(t2d, sel):
    start, step, n = sel
    if step == 1:
        return t2d[start:start + n, :]
    return t2d[start:start + (n - 1) * step + 1:step, :]


def stage_inproj(P, cfg, l, io, rep, src_lat, src_ctx, projT, projK):
    L, LC, D = cfg['L'], cfg['LC'], cfg['D']
    LT = L + LC
    NT = LT // 128
    KD = D // 128
    nct = LC // 128
    TBT = 12
    with Stage(P, 'inp%d' % l) as st:
        ident, n_id = st.sb([128, 128], BF16)
        P.dma(ident[:], io['ident'], writes=[n_id])
        reps = [st.sb([128, D], tag='rep') for _ in range(4)]
        for j, r in enumerate((0, 1, 3, 4)):
            P.dma(reps[j][0][:], rep[r], reads=['rep%d' % r], writes=[reps[j][1]])
        hT, n_hT = st.sb([128, KD, TBT * 128], BF16)
        xts = [st.sb([128, D], tag='x') for _ in range(2)]
        junk, n_junk = st.sb([128, D])
        t1s = [st.sb([128, D], tag='t1') for _ in range(2)]
        hbs = [st.sb([128, D], BF16, tag='hb') for _ in range(2)]
        sss = [st.sb([128, 2], tag='ss') for _ in range(2)]
        tps = [st.ps([128, 8, 128], BF16, tag='tp') for _ in range(2)]
        wst = [st.sb([128, KD, 128], tag='wst') for _ in range(2)]
        wbs = [st.sb([128, KD, 128], BF16, tag='wb') for _ in range(2)]
        wkv, n_wkv = st.sb([128, KD, 512], BF16)
        pms = [st.ps([128, 512], tag='pm') for _ in range(4)]
        o32 = [st.sb([128, TBT * 128], tag='o') for _ in range(2)]
        ok32 = [st.sb([128, 512], tag='ok') for _ in range(2)]
        wv = io['w_in'][l].rearrange("(k p) n -> p k n", p=128)
        npm = 0
        for b0 in range(0, NT, TBT):
            btiles = list(range(b0, min(NT, b0 + TBT)))
            nb = len(btiles)
            for ti, i in enumerate(btiles):
                xt, n_x = xts[i % 2]
                t1, n_t1 = t1s[i % 2]
                hb, n_hb = hbs[i % 2]
                ss, n_ss = sss[i % 2]
                for (pp, npp, kind, sel) in pos_row_dmas(cfg, l, i):
                    src = src_ctx if kind == 'c' else src_lat
                    P.dma(xt[pp:pp + npp, :], row_ap(src, sel), reads=['res_' + kind], writes=[n_x])
                G, n_G = reps[0] if i >= nct else reps[2]
                S, n_S = reps[1] if i >= nct else reps[3]
                P.op('dve', (lambda e, ss=ss: e.memset(ss[:], 0.0)), writes=[n_ss])
                P.op('act', (lambda e, xt=xt, ss=ss: e.activation(out=junk[:], in_=xt[:], func=AF.Square,
                                                                    accum_out=ss[:, 0:1])),
                     reads=[n_x], writes=[n_junk, n_ss])
                P.op('dve', (lambda e, ss=ss: e.tensor_scalar(out=ss[:, 1:2], in0=ss[:, 0:1], scalar1=1.0 / D,
                                                               scalar2=EPS, op0=ALU.mult, op1=ALU.add)),
                     reads=[n_ss], writes=[n_ss])
                P.op('act', (lambda e, ss=ss: e.activation(out=ss[:, 1:2], in_=ss[:, 1:2], func=AF.Sqrt)),
                     reads=[n_ss], writes=[n_ss])
                P.op('dve', (lambda e, ss=ss: e.reciprocal(out=ss[:, 1:2], in_=ss[:, 1:2])),
                     reads=[n_ss], writes=[n_ss])
                P.op('dve', (lambda e, xt=xt, ss=ss, t1=t1, G=G: e.scalar_tensor_tensor(
                    out=t1[:], in0=xt[:], scalar=ss[:, 1:2], in1=G[:], op0=ALU.mult, op1=ALU.mult)),
                     reads=[n_x, n_ss, n_G], writes=[n_t1])
                P.op('pool', (lambda e, t1=t1, hb=hb, S=S: e.tensor_tensor(out=hb[:], in0=t1[:], in1=S[:], op=ALU.add)),
                     reads=[n_t1, n_S], writes=[n_hb])
                for k8 in range(0, KD, 8):
                    kk = min(8, KD - k8)
                    tp, n_tp = tps[(k8 // 8 + i) % 2]
                    for k in range(kk):
                        P.op('pe', (lambda e, tp=tp, hb=hb, k=k, k8=k8: e.transpose(
                            out=tp[:, k, :], in_=hb[:, (k8 + k) * 128:(k8 + k + 1) * 128], identity=ident[:])),
                             reads=[n_hb, n_id], writes=[n_tp])
                    eng = 'act' if (k8 // 8) % 2 == 0 else 'dve'
                    if eng == 'act':
                        P.op('act', (lambda e, tp=tp, k8=k8, kk=kk, ti=ti: e.activation(
                            out=hT[:, k8:k8 + kk, ti * 128:(ti + 1) * 128], in_=tp[:, :kk, :], func=AF.Copy)),
                             reads=[n_tp], writes=[n_hT])
                    else:
                        P.op('dve', (lambda e, tp=tp, k8=k8, kk=kk, ti=ti: e.tensor_copy(
                            out=hT[:, k8:k8 + kk, ti * 128:(ti + 1) * 128], in_=tp[:, :kk, :])),
                             reads=[n_tp], writes=[n_hT])
            for ci, (c0, cn) in enumerate(tiles_of(NCOL, 128)):
                ws, n_ws = wst[ci % 2]
                wb, n_wb = wbs[ci % 2]
                o, n_o = o32[ci % 2]
                P.dma(ws[:, :, :cn], wv[:, :, c0:c0 + cn], writes=[n_ws])
                if ci % 2 == 0:
                    P.op('act', (lambda e, ws=ws, wb=wb, cn=cn: e.activation(out=wb[:, :, :cn], in_=ws[:, :, :cn], func=AF.Copy)),
                         reads=[n_ws], writes=[n_wb])
                else:
                    P.op('pool', (lambda e, ws=ws, wb=wb, cn=cn: e.tensor_copy(out=wb[:, :, :cn], in_=ws[:, :, :cn])),
                         reads=[n_ws], writes=[n_wb])
                for (g0, gn) in tiles_of(nb * 128, 512):
                    pm, n_pm = pms[npm % 4]
                    npm += 1
                    for k in range(KD):
                        P.op('pe', (lambda e, pm=pm, wb=wb, k=k, cn=cn, g0=g0, gn=gn: e.matmul(
                            pm[:cn, :gn], lhsT=wb[:, k, :cn], rhs=hT[:, k, g0:g0 + gn], start=(k == 0), stop=(k == KD - 1))),
                             reads=[n_wb, n_hT], writes=[n_pm])
                    if npm % 2 == 0:
                        P.op('act', (lambda e, pm=pm, o=o, cn=cn, g0=g0, gn=gn: e.activation(
                            out=o[:cn, g0:g0 + gn], in_=pm[:cn, :gn], func=AF.Copy)), reads=[n_pm], writes=[n_o])
                    else:
                        P.op('dve', (lambda e, pm=pm, o=o, cn=cn, g0=g0, gn=gn: e.tensor_copy(
                            out=o[:cn, g0:g0 + gn], in_=pm[:cn, :gn])), reads=[n_pm], writes=[n_o])
                P.dma(projT[c0:c0 + cn, b0 * 128:(b0 + nb) * 128], o[:cn, :nb * 128], reads=[n_o], writes=['projT'])
            for gi in range(3):
                for q4 in range(4):
                    c0 = C_K + gi * 512 + q4 * 128
                    ws, n_ws = wst[q4 % 2]
                    P.dma(ws[:], wv[:, :, c0:c0 + 128], writes=[n_ws])
                    P.op('act' if q4 % 2 == 0 else 'pool',
                         (lambda e, ws=ws, q4=q4: e.activation(out=wkv[:, :, q4 * 128:(q4 + 1) * 128], in_=ws[:], func=AF.Copy)
                          if q4 % 2 == 0 else e.tensor_copy(out=wkv[:, :, q4 * 128:(q4 + 1) * 128], in_=ws[:])),
                         reads=[n_ws], writes=[n_wkv])
                for ti, i in enumerate(btiles):
                    pm, n_pm = pms[npm % 4]
                    npm += 1
                    ok, n_ok = ok32[npm % 2]
                    for k in range(KD):
                        P.op('pe', (lambda e, pm=pm, k=k, ti=ti: e.matmul(
                            pm[:, :], lhsT=hT[:, k, ti * 128:(ti + 1) * 128], rhs=wkv[:, k, :], start=(k == 0), stop=(k == KD - 1))),
                             reads=[n_wkv, n_hT], writes=[n_pm])
                    if npm % 2 == 0:
                        P.op('act', (lambda e, pm=pm, ok=ok: e.activation(out=ok[:], in_=pm[:], func=AF.Copy)),
                             reads=[n_pm], writes=[n_ok])
                    else:
                        P.op('dve', (lambda e, pm=pm, ok=ok: e.tensor_copy(out=ok[:], in_=pm[:])),
                             reads=[n_pm], writes=[n_ok])
                    P.dma(projK[i * 128:(i + 1) * 128, gi * 512:(gi + 1) * 512], ok[:], reads=[n_ok], writes=['projK'])


def stage_outproj(P, cfg, l, io, rep, yT, src_lat, src_ctx, dst_lat, dst_ctx):
    L, LC, D = cfg['L'], cfg['LC'], cfg['D']
    LT = L + LC
    NT = LT // 128
    nct = LC // 128
    KM = 2048 // 128
    with Stage(P, 'outp%d' % l) as st:
        wo, n_wo = st.sb([128, KM, D], BF16)
        wst = [st.sb([128, KM, 128], tag='wst') for _ in range(2)]
        ggs = [st.sb([128, D], tag='gg') for _ in range(2)]
        P.dma(ggs[0][0][:], rep[2], reads=['rep2'], writes=[ggs[0][1]])
        P.dma(ggs[1][0][:], rep[5], reads=['rep5'], writes=[ggs[1][1]])
        wv = io['w_out'][l].rearrange("(k p) n -> p k n", p=128)
        for ci, (c0, cn) in enumerate(tiles_of(D, 128)):
            ws, n_ws = wst[ci % 2]
            P.dma(ws[:, :, :cn], wv[:, :, c0:c0 + cn], writes=[n_ws])
            if ci % 2 == 0:
                P.op('act', (lambda e, ws=ws, c0=c0, cn=cn: e.activation(out=wo[:, :, c0:c0 + cn], in_=ws[:, :, :cn], func=AF.Copy)),
                     reads=[n_ws], writes=[n_wo])
            else:
                P.op('pool', (lambda e, ws=ws, c0=c0, cn=cn: e.tensor_copy(out=wo[:, :, c0:c0 + cn], in_=ws[:, :, :cn])),
                     reads=[n_ws], writes=[n_wo])
        yts = [st.sb([128, KM, 128], BF16, tag='y') for _ in range(2)]
        xts = [st.sb([128, D], tag='x') for _ in range(2)]
        rs = [st.sb([128, D], tag='r') for _ in range(2)]
        junk, n_junk = st.sb([128, 512])
        sss = [st.sb([128, 8], tag='ss') for _ in range(2)]
        ngrp = len(tiles_of(D, 512))
        pms = [st.ps([128, 512], tag='pm') for _ in range(8)]
        yv = yT.rearrange("(k p) t -> p k t", p=128)
        if l == cfg['DEPTH'] - 1:
            tile_list = list(range(nct, NT))
        else:
            tile_list = list(range(NT))
        for n, i in enumerate(tile_list):
            yt, n_y = yts[n % 2]
            xt, n_x = xts[n % 2]
            r, n_r = rs[n % 2]
            ss, n_ss = sss[n % 2]
            P.dma(yt[:], yv[:, :, i * 128:(i + 1) * 128], reads=['yT'], writes=[n_y])
            rows = pos_row_dmas(cfg, l, i)
            for (pp, npp, kind, sel) in rows:
                src = src_ctx if kind == 'c' else src_lat
                P.dma(xt[pp:pp + npp, :], row_ap(src, sel), reads=['res_' + kind], writes=[n_x])
            GG, n_GG = ggs[0] if i >= nct else ggs[1]
            P.op('dve', (lambda e, ss=ss: e.memset(ss[:], 0.0)), writes=[n_ss])
            for j, (n0, nn) in enumerate(tiles_of(D, 512)):
                pm, n_pm = pms[(n % 2) * 4 + j % 4]
                for k in range(KM):
                    P.op('pe', (lambda e, pm=pm, yt=yt, k=k, n0=n0, nn=nn: e.matmul(
                        pm[:, :nn], lhsT=yt[:, k, :], rhs=wo[:, k, n0:n0 + nn], start=(k == 0), stop=(k == KM - 1))),
                         reads=[n_y, n_wo], writes=[n_pm])
                P.op('act', (lambda e, pm=pm, ss=ss, j=j, nn=nn: e.activation(
                    out=junk[:, :nn], in_=pm[:, :nn], func=AF.Square, accum_out=ss[:, j:j + 1])),
                     reads=[n_pm], writes=[n_junk, n_ss])
            P.op('dve', (lambda e, ss=ss: e.tensor_reduce(out=ss[:, 4:5], in_=ss[:, 0:4], axis=AX.X, op=ALU.add)),
                 reads=[n_ss], writes=[n_ss])
            P.op('dve', (lambda e, ss=ss: e.tensor_scalar(out=ss[:, 5:6], in0=ss[:, 4:5], scalar1=1.0 / D,
                                                           scalar2=EPS, op0=ALU.mult, op1=ALU.add)),
                 reads=[n_ss], writes=[n_ss])
            P.op('act', (lambda e, ss=ss: e.activation(out=ss[:, 5:6], in_=ss[:, 5:6], func=AF.Sqrt)),
                 reads=[n_ss], writes=[n_ss])
            P.op('dve', (lambda e, ss=ss: e.reciprocal(out=ss[:, 5:6], in_=ss[:, 5:6])),
                 reads=[n_ss], writes=[n_ss])
            for j, (n0, nn) in enumerate(tiles_of(D, 512)):
                pm, n_pm = pms[(n % 2) * 4 + j % 4]
                P.op('dve', (lambda e, pm=pm, ss=ss, r=r, GG=GG, n0=n0, nn=nn: e.scalar_tensor_tensor(
                    out=r[:, n0:n0 + nn], in0=pm[:, :nn], scalar=ss[:, 5:6], in1=GG[:, n0:n0 + nn],
                    op0=ALU.mult, op1=ALU.mult)), reads=[n_pm, n_ss, n_GG], writes=[n_r])
            P.op('pool', (lambda e, r=r, xt=xt: e.tensor_tensor(out=r[:], in0=r[:], in1=xt[:], op=ALU.add)),
                 reads=[n_r, n_x], writes=[n_r])
            for (pp, npp, kind, sel) in rows:
                dst = dst_ctx if kind == 'c' else dst_lat
                P.dma(row_ap(dst, sel), r[pp:pp + npp, :], reads=[n_r], writes=['dst_' + kind])


def stage_stub_mixer(P, cfg, l, projT, yT):
    LT = cfg['L'] + cfg['LC']
    with Stage(P, 'stub%d' % l) as st:
        a = [st.sb([128, LT], tag='a') for _ in range(2)]
        b = [st.sb([128, LT], BF16, tag='b') for _ in range(2)]
        for k in range(16):
            t, n_t = a[k % 2]
            u, n_u = b[k % 2]
            P.dma(t[:], projT[k * 128:(k + 1) * 128, :], reads=['projT'], writes=[n_t])
            P.op('dve', (lambda e, t=t, u=u: e.tensor_copy(out=u[:], in_=t[:])), reads=[n_t], writes=[n_u])
            P.dma(yT[k * 128:(k + 1) * 128, :], u[:], reads=[n_u], writes=['yT'])


def pcols(X, d, j0, n, seg):
    s0, s1 = seg
    if d == 0:
        return X[:, j0:j0 + n]
    lo = s0 + s1 - j0 - n
    return X[:, lo:lo + n][:, ::-1]


def stage_gla(P, cfg, l, io, projT, projK, yT):
    L, LC = cfg['L'], cfg['LC']
    LT = L + LC
    NT = LT // 128
    nct = LC // 128
    segs = [(0, LC), (LC, LT)]
    DKS = 128.0 ** -0.5
    with Stage(P, 'gla%d' % l) as st:
        ident, n_id = st.sb([128, 128], BF16)
        maskf, n_mf = st.sb([128, 128], BF16)
        maskb, n_mb = st.sb([128, 128], BF16)
        ones, n_ones = st.sb([128, 128])
        wg, n_wg = st.sb([16, 2, 512])
        bT, n_bT = st.sb([128, 8])
        nbT, n_nbT = st.sb([128, 8])
        gnT, n_gn = st.sb([128, 2])
        P.dma(ident[:], io['ident'], writes=[n_id])
        P.dma(maskf[:], io['maskf'], writes=[n_mf])
        P.dma(maskb[:], io['maskb'], writes=[n_mb])
        P.dma(wg[:], io['gla_wg'][l], writes=[n_wg])
        P.dma(bT[:], io['gla_bT'][l], writes=[n_bT])
        P.dma(gnT[:], io['gla_nT'][l], writes=[n_gn])
        P.op('dve', lambda e: e.memset(ones[:], 1.0), writes=[n_ones])
        P.op('dve', lambda e: e.tensor_scalar(out=nbT[:], in0=bT[:], scalar1=-1.0, scalar2=None, op0=ALU.mult),
             reads=[n_bT], writes=[n_nbT])
        qT, n_q = st.sb([128, LT])
        kT, n_k = st.sb([128, LT])
        lr, n_lr = st.sb([16, LT])
        LP, n_LP = st.sb([128, LT])
        Gc, n_Gc = st.sb([128, LT])
        EG, n_EG = st.sb([128, LT])
        qg, n_qg = st.sb([128, LT], BF16)
        kg, n_kg = st.sb([128, LT], BF16)
        kgr, n_kgr = st.sb([128, LT], BF16)
        vtok, n_v = st.sb([128, NT, 256], BF16)
        O, n_O = st.sb([128, 2, LT])
        vst = [st.sb([128, 256], tag='vst') for _ in range(2)]
        S, n_S = st.sb([128, 256])
        Sb, n_Sb = st.sb([128, 256], BF16)
        attm = [st.sb([128, 128], BF16, tag='attm') for _ in range(2)]
        kdT = [st.sb([128, 128], BF16, tag='kdT') for _ in range(2)]
        kd = [st.sb([128, 128], BF16, tag='kd') for _ in range(2)]
        qk, n_qk = st.sb([128, 256])
        vt, n_vt = st.sb([128, 2, 256])
        gt, n_gt = st.sb([128, 2, 256])
        tmp, n_tmp = st.sb([128, 2, 256])
        rs, n_rs = st.sb([128, 256])
        yb = [st.sb([128, 2, 256], BF16, tag='yb') for _ in range(2)]
        pz = [st.ps([128, 512], tag='pz') for _ in range(2)]
        patt, n_patt = st.ps([128, 128], tag='patt')
        po = [st.ps([128, 2, 128], tag='po') for _ in range(2)]
        ptr, n_ptr = st.ps([128, 128], BF16, tag='ptr')
        pds, n_pds = st.ps([128, 256], tag='pds')
        pq, n_pq = st.ps([128, 512], tag='pq')
        gcount = 0
        for h in range(4):
            P.dma(qT[:], projT[C_Q + h * 128:C_Q + (h + 1) * 128, :], reads=['projT'], writes=[n_q])
            P.dma(kT[:], projT[C_K + h * 128:C_K + (h + 1) * 128, :], reads=['projT'], writes=[n_k])
            for pcn in range(NT):
                vs, n_vs = vst[pcn % 2]
                P.dma(vs[:], projK[pcn * 128:(pcn + 1) * 128, 512 + h * 256:512 + (h + 1) * 256], reads=['projK'], writes=[n_vs])
                if pcn % 2 == 0:
                    P.op('act', (lambda e, vs=vs, pcn=pcn: e.activation(out=vtok[:, pcn, :], in_=vs[:], func=AF.Copy)),
                         reads=[n_vs], writes=[n_v])
                else:
                    P.op('pool', (lambda e, vs=vs, pcn=pcn: e.tensor_copy(out=vtok[:, pcn, :], in_=vs[:])),
                         reads=[n_vs], writes=[n_v])
            for d in range(2):
                P.dma(lr[:], projT[C_LR + d * 16:C_LR + (d + 1) * 16, :], reads=['projT'], writes=[n_lr])
                col = d * 4 + h
                for seg in segs:
                    for (j0, n) in [(seg[0] + a, b) for (a, b) in tiles_of(seg[1] - seg[0], 512)]:
                        p, n_p = pz[gcount % 2]
                        gcount += 1
                        P.op('pe', (lambda e, p=p, d=d, h=h, j0=j0, n=n, seg=seg: e.matmul(
                            p[:, :n], lhsT=wg[:, d, h * 128:(h + 1) * 128], rhs=pcols(lr, d, j0, n, seg), start=True, stop=True)),
                             reads=[n_wg, n_lr], writes=[n_p])
                        P.op('act', (lambda e, p=p, j0=j0, n=n, col=col: e.activation(
                            out=LP[:, j0:j0 + n], in_=p[:, :n], func=AF.Exp, scale=-1.0, bias=nbT[:, col:col + 1])),
                             reads=[n_p, n_nbT], writes=[n_LP])
                P.op('act', lambda e: e.activation(out=LP[:], in_=LP[:], func=AF.Ln, bias=1.0, scale=1.0),
                     reads=[n_LP], writes=[n_LP])
                for n in range(NT):
                    P.op('dve', (lambda e, n=n: e.tensor_tensor_scan(
                        out=Gc[:, n * 128:(n + 1) * 128], data0=ones[:], data1=LP[:, n * 128:(n + 1) * 128],
                        initial=0.0, op0=ALU.mult, op1=ALU.add)), reads=[n_LP, n_ones], writes=[n_Gc])
                P.op('act', lambda e: e.activation(out=EG[:], in_=Gc[:], func=AF.Exp, scale=-1.0 / 16.0),
                     reads=[n_Gc], writes=[n_EG])
                P.op('act', lambda e: e.activation(out=Gc[:], in_=Gc[:], func=AF.Exp, scale=1.0 / 16.0),
                     reads=[n_Gc], writes=[n_Gc])
                for seg in segs:
                    s0, s1 = seg
                    P.op('dve', (lambda e, d=d, seg=seg, s0=s0, s1=s1: e.scalar_tensor_tensor(
                        out=qg[:, s0:s1], in0=pcols(qT, d, s0, s1 - s0, seg), scalar=DKS, in1=EG[:, s0:s1],
                        op0=ALU.mult, op1=ALU.mult)), reads=[n_q, n_EG], writes=[n_qg])
                    P.op('pool', (lambda e, d=d, seg=seg, s0=s0, s1=s1: e.tensor_tensor(
                        out=kg[:, s0:s1], in0=pcols(kT, d, s0, s1 - s0, seg), in1=Gc[:, s0:s1], op=ALU.mult)),
                         reads=[n_k, n_Gc], writes=[n_kg])
                if d == 1:
                    P.op('pool', lambda e: e.tensor_copy(
                        out=kgr[:].rearrange("p (n i) -> p n i", i=128),
                        in_=kg[:].rearrange("p (n i) -> p n i", i=128)[:, :, ::-1]), reads=[n_kg], writes=[n_kgr])
                kgP, n_kgP = (kg, n_kg) if d == 0 else (kgr, n_kgr)
                P.op('dve', lambda e: e.memset(S[:], 0.0), writes=[n_S])
                P.op('dve', lambda e: e.memset(Sb[:], 0.0), writes=[n_Sb])
                mask, n_mask = (maskf, n_mf) if d == 0 else (maskb, n_mb)
                for n in range(NT):
                    if d == 0:
                        pcn = n
                    else:
                        pcn = (nct - 1 - n) if n < nct else (NT - 1 - n + nct)
                    c0 = n * 128
                    am, n_am = attm[n % 2]
                    kt, n_kt = kdT[n % 2]
                    kdd, n_kd = kd[n % 2]
                    pp, n_po = po[n % 2]
                    P.op('pe', (lambda e, c0=c0, kgP=kgP: e.matmul(patt[:], lhsT=kgP[:, c0:c0 + 128], rhs=qg[:, c0:c0 + 128],
                                                                    start=True, stop=True)),
                         reads=[n_kgP, n_qg], writes=[n_patt])
                    P.op('dve', (lambda e, am=am, mask=mask: e.tensor_tensor(out=am[:], in0=patt[:], in1=mask[:], op=ALU.mult)),
                         reads=[n_patt, n_mask], writes=[n_am])
                    for half in range(2):
                        P.op('pe', (lambda e, pp=pp, half=half, pcn=pcn, am=am: e.matmul(
                            pp[:, half, :], lhsT=vtok[:, pcn, half * 128:(half + 1) * 128], rhs=am[:], start=True, stop=False)),
                             reads=[n_v, n_am], writes=[n_po])
                        P.op('pe', (lambda e, pp=pp, half=half, c0=c0: e.matmul(
                            pp[:, half, :], lhsT=Sb[:, half * 128:(half + 1) * 128], rhs=qg[:, c0:c0 + 128], start=False, stop=True)),
                             reads=[n_Sb, n_qg], writes=[n_po])
                    pb = pcn * 128
                    if d == 0:
                        P.op('act', (lambda e, pp=pp, pb=pb: e.activation(out=O[:, :, pb:pb + 128], in_=pp[:, :, :], func=AF.Copy)),
                             reads=[n_po], writes=[n_O])
                    else:
                        for half in range(2):
                            P.op('dve', (lambda e, pp=pp, pb=pb, half=half: e.tensor_tensor(
                                out=O[:, half, pb:pb + 128][:, ::-1], in0=O[:, half, pb:pb + 128][:, ::-1],
                                in1=pp[:, half, :], op=ALU.add)), reads=[n_po, n_O], writes=[n_O])
                    if n == NT - 1:
                        continue
                    a_n = EG[:, c0 + 127:c0 + 128]
                    P.op('dve', (lambda e, kt=kt, c0=c0, a_n=a_n, kgP=kgP: e.tensor_scalar(
                        out=kt[:], in0=kgP[:, c0:c0 + 128], scalar1=a_n, scalar2=None, op0=ALU.mult)),
                         reads=[n_kgP, n_EG], writes=[n_kt])
                    P.op('pe', (lambda e, kt=kt: e.transpose(out=ptr[:], in_=kt[:], identity=ident[:])),
                         reads=[n_kt, n_id], writes=[n_ptr])
                    P.op('act', (lambda e, kdd=kdd: e.activation(out=kdd[:], in_=ptr[:], func=AF.Copy)),
                         reads=[n_ptr], writes=[n_kd])
                    P.op('pe', (lambda e, kdd=kdd, pcn=pcn: e.matmul(pds[:], lhsT=kdd[:], rhs=vtok[:, pcn, :], start=True, stop=True)),
                         reads=[n_kd, n_v], writes=[n_pds])
                    P.op('dve', (lambda e, a_n=a_n: e.scalar_tensor_tensor(
                        out=S[:], in0=S[:], scalar=a_n, in1=pds[:], op0=ALU.mult, op1=ALU.add)),
                         reads=[n_pds, n_EG, n_S], writes=[n_S])
                    P.op('act', lambda e: e.activation(out=Sb[:], in_=S[:], func=AF.Copy), reads=[n_S], writes=[n_Sb])
            for gi, (g0, gn) in enumerate(tiles_of(LT, 256)):
                y, n_y = yb[gi % 2]
                P.op('dve', (lambda e, g0=g0, gn=gn: e.scalar_tensor_tensor(
                    out=qk[:, :gn], in0=qT[:, g0:g0 + gn], scalar=DKS, in1=kT[:, g0:g0 + gn], op0=ALU.mult, op1=ALU.mult)),
                     reads=[n_q, n_k], writes=[n_qk])
                P.op('pe', (lambda e, gn=gn: e.matmul(pq[:, :gn], lhsT=ones[:], rhs=qk[:, :gn], start=True, stop=True)),
                     reads=[n_ones, n_qk], writes=[n_pq])
                for half in range(2):
                    r0 = C_V + h * 256 + half * 128
                    P.dma(vt[:, half, :gn], projT[r0:r0 + 128, g0:g0 + gn], reads=['projT'], writes=[n_vt])
                    r1 = C_GG + h * 256 + half * 128
                    P.dma(gt[:, half, :gn], projT[r1:r1 + 128, g0:g0 + gn], reads=['projT'], writes=[n_gt])
                for half in range(2):
                    P.op('dve', (lambda e, half=half, gn=gn: e.tensor_tensor(
                        out=tmp[:, half, :gn], in0=pq[:, :gn], in1=vt[:, half, :gn], op=ALU.mult)),
                         reads=[n_pq, n_vt], writes=[n_tmp])
                    P.op('pool', (lambda e, half=half, g0=g0, gn=gn: e.tensor_tensor(
                        out=O[:, half, g0:g0 + gn], in0=O[:, half, g0:g0 + gn], in1=tmp[:, half, :gn], op=ALU.subtract)),
                         reads=[n_tmp, n_O], writes=[n_O])
                P.op('act', (lambda e, g0=g0, gn=gn: e.activation(out=tmp[:, :, :gn], in_=O[:, :, g0:g0 + gn], func=AF.Square)),
                     reads=[n_O], writes=[n_tmp])
                for half in range(2):
                    P.op('pe', (lambda e, half=half, gn=gn: e.matmul(pq[:, :gn], lhsT=ones[:], rhs=tmp[:, half, :gn],
                                                                      start=(half == 0), stop=(half == 1))),
                         reads=[n_ones, n_tmp], writes=[n_pq])
                P.op('dve', (lambda e, gn=gn: e.tensor_scalar(out=rs[:, :gn], in0=pq[:, :gn], scalar1=1.0 / 256.0, scalar2=EPS,
                                                              op0=ALU.mult, op1=ALU.add)), reads=[n_pq], writes=[n_rs])
                P.op('act', (lambda e, gn=gn: e.activation(out=rs[:, :gn], in_=rs[:, :gn], func=AF.Sqrt)),
                     reads=[n_rs], writes=[n_rs])
                P.op('dve', (lambda e, gn=gn: e.reciprocal(out=rs[:, :gn], in_=rs[:, :gn])), reads=[n_rs], writes=[n_rs])
                P.op('act', (lambda e, gn=gn: e.activation(out=gt[:, :, :gn], in_=gt[:, :, :gn], func=AF.Silu)),
                     reads=[n_gt], writes=[n_gt])
                for half in range(2):
                    P.op('dve', (lambda e, half=half, g0=g0, gn=gn: e.scalar_tensor_tensor(
                        out=tmp[:, half, :gn], in0=O[:, half, g0:g0 + gn], scalar=gnT[:, half:half + 1], in1=rs[:, :gn],
                        op0=ALU.mult, op1=ALU.mult)), reads=[n_O, n_gn, n_rs], writes=[n_tmp])
                    P.op('pool', (lambda e, half=half, gn=gn, y=y: e.tensor_tensor(
                        out=y[:, half, :gn], in0=tmp[:, half, :gn], in1=gt[:, half, :gn], op=ALU.mult)),
                         reads=[n_tmp, n_gt], writes=[n_y])
                    r2 = h * 256 + half * 128
                    P.dma(yT[r2:r2 + 128, g0:g0 + gn], y[:, half, :gn], reads=[n_y], writes=['yT'])


MAGIC = 12582912.0


def stage_s5(P, cfg, l, io, projT, ygT, yT):
    L, LC = cfg['L'], cfg['LC']
    LT = L + LC
    segs = [(0, LC), (LC, LT)]
    BW = 1024
    blocks = []
    for seg in segs:
        for (a, n) in tiles_of(seg[1] - seg[0], BW):
            blocks.append((seg[0] + a, n, seg))
    with Stage(P, 's5a%d' % l) as st:
        ident, n_id = st.sb([128, 128])
        P.dma(ident[:], io['ident32'], writes=[n_id])
        sc, n_sc = st.sb([128, 2, 16, 3])
        P.dma(sc[:], io['s5_sc'][l], writes=[n_sc])
        prm = {}
        for nm in ('Dl', 'AR', 'TH', 'Rr', 'k1', 'red', 'sn', 'ab', 'cs', 'LBr', 'LBi', 'den', 'kr', 'ki', 'nki', 't'):
            prm[nm] = st.sb([128, 2, 16], tag=nm)
        LRv, LIv, LSv = sc[:, :, :, 0], sc[:, :, :, 1], sc[:, :, :, 2]

        def pw(eng, fn, r, w):
            P.op(eng, fn, reads=[prm[x][1] if x in prm else x for x in r], writes=[prm[x][1] for x in w])
        T = lambda nm: prm[nm][0]
        pw('act', lambda e: e.activation(out=T('Dl')[:], in_=LSv, func=AF.Exp), [n_sc], ['Dl'])
        pw('dve', lambda e: e.tensor_tensor(out=T('AR')[:], in0=LRv, in1=T('Dl')[:], op=ALU.mult), [n_sc, 'Dl'], ['AR'])
        pw('dve', lambda e: e.tensor_tensor(out=T('TH')[:], in0=LIv, in1=T('Dl')[:], op=ALU.mult), [n_sc, 'Dl'], ['TH'])
        pw('act', lambda e: e.activation(out=T('Rr')[:], in_=T('AR')[:], func=AF.Exp), ['AR'], ['Rr'])
        pw('dve', lambda e: e.tensor_scalar(out=T('k1')[:], in0=T('TH')[:], scalar1=1.0 / (2 * PI), scalar2=MAGIC,
                                            op0=ALU.mult, op1=ALU.add), ['TH'], ['k1'])
        pw('dve', lambda e: e.tensor_scalar(out=T('k1')[:], in0=T('k1')[:], scalar1=MAGIC, scalar2=-2 * PI,
                                            op0=ALU.subtract, op1=ALU.mult), ['k1'], ['k1'])
        pw('dve', lambda e: e.tensor_tensor(out=T('red')[:], in0=T('TH')[:], in1=T('k1')[:], op=ALU.add), ['TH', 'k1'], ['red'])
        pw('act', lambda e: e.activation(out=T('sn')[:], in_=T('red')[:], func=AF.Sin), ['red'], ['sn'])
        pw('act', lambda e: e.activation(out=T('ab')[:], in_=T('red')[:], func=AF.Abs), ['red'], ['ab'])
        pw('act', lambda e: e.activation(out=T('cs')[:], in_=T('ab')[:], func=AF.Sin, scale=-1.0, bias=PI / 2), ['ab'], ['cs'])
        pw('dve', lambda e: e.tensor_tensor(out=T('LBr')[:], in0=T('Rr')[:], in1=T('cs')[:], op=ALU.mult), ['Rr', 'cs'], ['LBr'])
        pw('dve', lambda e: e.tensor_tensor(out=T('LBi')[:], in0=T('Rr')[:], in1=T('sn')[:], op=ALU.mult), ['Rr', 'sn'], ['LBi'])
        pw('dve', lambda e: e.tensor_scalar(out=T('LBr')[:], in0=T('LBr')[:], scalar1=-1.0, scalar2=None, op0=ALU.add), ['LBr'], ['LBr'])
        pw('dve', lambda e: e.tensor_tensor(out=T('den')[:], in0=LRv, in1=LRv, op=ALU.mult), [n_sc], ['den'])
        pw('dve', lambda e: e.tensor_tensor(out=T('t')[:], in0=LIv, in1=LIv, op=ALU.mult), [n_sc], ['t'])
        pw('dve', lambda e: e.tensor_tensor(out=T('den')[:], in0=T('den')[:], in1=T('t')[:], op=ALU.add), ['den', 't'], ['den'])
        pw('dve', lambda e: e.reciprocal(out=T('den')[:], in_=T('den')[:]), ['den'], ['den'])
        pw('dve', lambda e: e.tensor_tensor(out=T('kr')[:], in0=T('LBr')[:], in1=LRv, op=ALU.mult), ['LBr', n_sc], ['kr'])
        pw('dve', lambda e: e.tensor_tensor(out=T('t')[:], in0=T('LBi')[:], in1=LIv, op=ALU.mult), ['LBi', n_sc], ['t'])
        pw('dve', lambda e: e.tensor_tensor(out=T('kr')[:], in0=T('kr')[:], in1=T('t')[:], op=ALU.add), ['kr', 't'], ['kr'])
        pw('dve', lambda e: e.tensor_tensor(out=T('kr')[:], in0=T('kr')[:], in1=T('den')[:], op=ALU.mult), ['kr', 'den'], ['kr'])
        pw('dve', lambda e: e.tensor_tensor(out=T('ki')[:], in0=T('LBi')[:], in1=LRv, op=ALU.mult), ['LBi', n_sc], ['ki'])
        pw('dve', lambda e: e.tensor_tensor(out=T('t')[:], in0=T('LBr')[:], in1=LIv, op=ALU.mult), ['LBr', n_sc], ['t'])
        pw('dve', lambda e: e.tensor_tensor(out=T('ki')[:], in0=T('ki')[:], in1=T('t')[:], op=ALU.subtract), ['ki', 't'], ['ki'])
        pw('dve', lambda e: e.tensor_tensor(out=T('ki')[:], in0=T('ki')[:], in1=T('den')[:], op=ALU.mult), ['ki', 'den'], ['ki'])
        pw('dve', lambda e: e.tensor_scalar(out=T('nki')[:], in0=T('ki')[:], scalar1=-1.0, scalar2=None, op0=ALU.mult), ['ki'], ['nki'])
        cpad, n_cpad = st.sb([128, 2, 16, 2, 128])
        P.dma(cpad[:], io['s5_cpad'][l], writes=[n_cpad])
        P.op('pool', lambda e: e.tensor_scalar(out=cpad[:, :, :, 1, :], in0=cpad[:, :, :, 1, :], scalar1=-1.0, scalar2=None,
                                               op0=ALU.mult), reads=[n_cpad], writes=[n_cpad])
        bpad, n_bpad = st.sb([128, 2, 16, 2, 128])
        bsts = [st.sb([128, 2, 128], tag='bst') for _ in range(2)]
        bb = [st.sb([128, 2, 128], tag='bb') for _ in range(2)]
        tt = [st.sb([128, 2, 128], tag='tt') for _ in range(2)]
        ptr = [st.ps([128, 2, 128], tag='ptr') for _ in range(2)]
        i = 0
        for d in range(2):
            for s_ in range(16):
                bs, n_bs = bsts[i % 2]
                b2, n_b2 = bb[i % 2]
                t2, n_t2 = tt[i % 2]
                pt, n_pt = ptr[i % 2]
                i += 1
                P.dma(bs[:], io['s5_bpad'][l][:, d, s_, :, :], writes=[n_bs])
                kr = T('kr')[:, d, s_:s_ + 1]
                ki = T('ki')[:, d, s_:s_ + 1]
                nki = T('nki')[:, d, s_:s_ + 1]
                rk = [prm['kr'][1], prm['ki'][1], prm['nki'][1]]
                P.op('dve', (lambda e, bs=bs, t2=t2, kr=kr: e.tensor_scalar(out=t2[:], in0=bs[:], scalar1=kr, scalar2=None, op0=ALU.mult)),
                     reads=[n_bs] + rk, writes=[n_t2])
                P.op('dve', (lambda e, bs=bs, t2=t2, b2=b2, nki=nki: e.scalar_tensor_tensor(
                    out=b2[:, 0, :], in0=bs[:, 1, :], scalar=nki, in1=t2[:, 0, :], op0=ALU.mult, op1=ALU.add)),
                     reads=[n_bs, n_t2] + rk, writes=[n_b2])
                P.op('dve', (lambda e, bs=bs, t2=t2, b2=b2, ki=ki: e.scalar_tensor_tensor(
                    out=b2[:, 1, :], in0=bs[:, 0, :], scalar=ki, in1=t2[:, 1, :], op0=ALU.mult, op1=ALU.add)),
                     reads=[n_bs, n_t2] + rk, writes=[n_b2])
                for c in range(2):
                    P.op('pe', (lambda e, pt=pt, b2=b2, c=c: e.transpose(out=pt[:, c, :], in_=b2[:, c, :], identity=ident[:])),
                         reads=[n_b2, n_id], writes=[n_pt])
                P.op('act', (lambda e, pt=pt, d=d, s_=s_: e.activation(out=bpad[:, d, s_, :, :], in_=pt[:], func=AF.Copy)),
                     reads=[n_pt], writes=[n_bpad])
        dT, n_dT = st.sb([128, 4])
        P.dma(dT[:], io['s5_dT'][l], writes=[n_dT])
        iot, n_iot = st.sb([128, BW])
        P.dma(iot[:], io['iota'][:, :BW], writes=[n_iot])
        ones, n_ones = st.sb([128, BW])
        P.op('pool', lambda e: e.memset(ones[:], 1.0), writes=[n_ones])
        uT, n_u = st.sb([128, LT])
        Y, n_Y = st.sb([128, LT])
        rt, n_rt = st.sb([128, BW])
        carry, n_carry = st.sb([128, 2])
        names = ('bre', 'bim', 'ang', 'k1', 'sn', 'cs', 'ta', 'tb', 'btr', 'bti', 'wre', 'wim', 'xre', 'xim')
        B = {nm: st.sb([128, BW], tag=nm) for nm in names}
        pb = [st.ps([128, 512], tag='pb') for _ in range(4)]
        py = [st.ps([128, 512], tag='py') for _ in range(2)]
        npb = 0
        npy = 0

        def bop(eng, fn, r, w):
            P.op(eng, fn, reads=[B[x][1] if x in B else x for x in r], writes=[B[x][1] if x in B else x for x in w])
        X = lambda nm: B[nm][0]
        for ct in range(4):
            P.dma(uT[:], projT[C_U + ct * 128:C_U + (ct + 1) * 128, :], reads=['projT'], writes=[n_u])
            P.op('dve', (lambda e, ct=ct: e.tensor_scalar(out=Y[:], in0=uT[:], scalar1=dT[:, ct:ct + 1], scalar2=None, op0=ALU.mult)),
                 reads=[n_u, n_dT], writes=[n_Y])
            for s_ in range(4 * ct, 4 * ct + 4):
                for d in range(2):
                    rcol = T('Rr')[:, d, s_:s_ + 1]
                    thcol = T('red')[:, d, s_:s_ + 1]
                    P.op('pool', (lambda e, rcol=rcol: e.tensor_scalar(out=rt[:], in0=ones[:], scalar1=rcol, scalar2=None, op0=ALU.mult)),
                         reads=[n_ones, prm['Rr'][1]], writes=[n_rt])
                    P.op('dve', lambda e: e.memset(carry[:], 0.0), writes=[n_carry])
                    for (j0, n, seg) in blocks:
                        for (a, m) in tiles_of(n, 512):
                            for c, nm in ((0, 'bre'), (1, 'bim')):
                                p, n_p = pb[npb % 4]
                                npb += 1
                                P.op('pe', (lambda e, p=p, d=d, s_=s_, c=c, j0=j0, a=a, m=m, seg=seg: e.matmul(
                                    p[:, :m], lhsT=bpad[:, d, s_, c, :], rhs=pcols(uT, d, j0 + a, m, seg), start=True, stop=True)),
                                     reads=[n_bpad, n_u], writes=[n_p])
                                if c == 0:
                                    bop('act', (lambda e, p=p, a=a, m=m, nm=nm: e.activation(out=X(nm)[:, a:a + m], in_=p[:, :m], func=AF.Copy)),
                                        [n_p], [nm])
                                else:
                                    bop('dve', (lambda e, p=p, a=a, m=m, nm=nm: e.tensor_copy(out=X(nm)[:, a:a + m], in_=p[:, :m])),
                                        [n_p], [nm])
                        bop('dve', (lambda e, j0=j0, n=n, thcol=thcol: e.tensor_scalar(
                            out=X('ang')[:, :n], in0=iot[:, :n], scalar1=float(j0), scalar2=thcol, op0=ALU.add, op1=ALU.mult)),
                            [n_iot, prm['red'][1]], ['ang'])
                        bop('dve', (lambda e, n=n: e.tensor_scalar(out=X('k1')[:, :n], in0=X('ang')[:, :n], scalar1=1.0 / (2 * PI),
                                                                   scalar2=MAGIC, op0=ALU.mult, op1=ALU.add)), ['ang'], ['k1'])
                        bop('pool', (lambda e, n=n: e.tensor_scalar(out=X('k1')[:, :n], in0=X('k1')[:, :n], scalar1=MAGIC,
                                                                    scalar2=-2 * PI, op0=ALU.subtract, op1=ALU.mult)), ['k1'], ['k1'])
                        bop('pool', (lambda e, n=n: e.tensor_tensor(out=X('ang')[:, :n], in0=X('ang')[:, :n], in1=X('k1')[:, :n], op=ALU.add)),
                            ['ang', 'k1'], ['ang'])
                        bop('act', (lambda e, n=n: e.activation(out=X('sn')[:, :n], in_=X('ang')[:, :n], func=AF.Sin)), ['ang'], ['sn'])
                        bop('act', (lambda e, n=n: e.activation(out=X('k1')[:, :n], in_=X('ang')[:, :n], func=AF.Abs)), ['ang'], ['k1'])
                        bop('act', (lambda e, n=n: e.activation(out=X('cs')[:, :n], in_=X('k1')[:, :n], func=AF.Sin, scale=-1.0, bias=PI / 2)),
                            ['k1'], ['cs'])
                        bop('dve', (lambda e, n=n: e.tensor_tensor(out=X('ta')[:, :n], in0=X('cs')[:, :n], in1=X('bre')[:, :n], op=ALU.mult)),
                            ['cs', 'bre'], ['ta'])
                        bop('pool', (lambda e, n=n: e.tensor_tensor(out=X('tb')[:, :n], in0=X('sn')[:, :n], in1=X('bim')[:, :n], op=ALU.mult)),
                            ['sn', 'bim'], ['tb'])
                        bop('dve', (lambda e, n=n: e.tensor_tensor(out=X('btr')[:, :n], in0=X('ta')[:, :n], in1=X('tb')[:, :n], op=ALU.add)),
                            ['ta', 'tb'], ['btr'])
                        bop('pool', (lambda e, n=n: e.tensor_tensor(out=X('ta')[:, :n], in0=X('cs')[:, :n], in1=X('bim')[:, :n], op=ALU.mult)),
                            ['cs', 'bim'], ['ta'])
                        bop('dve', (lambda e, n=n: e.tensor_tensor(out=X('tb')[:, :n], in0=X('sn')[:, :n], in1=X('bre')[:, :n], op=ALU.mult)),
                            ['sn', 'bre'], ['tb'])
                        bop('pool', (lambda e, n=n: e.tensor_tensor(out=X('bti')[:, :n], in0=X('ta')[:, :n], in1=X('tb')[:, :n], op=ALU.subtract)),
                            ['ta', 'tb'], ['bti'])
                        bop('dve', (lambda e, n=n: e.tensor_tensor_scan(out=X('wre')[:, :n], data0=rt[:, :n], data1=X('btr')[:, :n],
                                                                        initial=carry[:, 0:1], op0=ALU.mult, op1=ALU.add)),
                            [n_rt, 'btr', n_carry], ['wre'])
                        bop('dve', (lambda e, n=n: e.tensor_tensor_scan(out=X('wim')[:, :n], data0=rt[:, :n], data1=X('bti')[:, :n],
                                                                        initial=carry[:, 1:2], op0=ALU.mult, op1=ALU.add)),
                            [n_rt, 'bti', n_carry], ['wim'])
                        bop('act', (lambda e, n=n: e.activation(out=carry[:, 0:1], in_=X('wre')[:, n - 1:n], func=AF.Copy)), ['wre'], [n_carry])
                        bop('act', (lambda e, n=n: e.activation(out=carry[:, 1:2], in_=X('wim')[:, n - 1:n], func=AF.Copy)), ['wim'], [n_carry])
                        bop('dve', (lambda e, n=n: e.tensor_tensor(out=X('ta')[:, :n], in0=X('cs')[:, :n], in1=X('wre')[:, :n], op=ALU.mult)),
                            ['cs', 'wre'], ['ta'])
                        bop('pool', (lambda e, n=n: e.tensor_tensor(out=X('tb')[:, :n], in0=X('sn')[:, :n], in1=X('wim')[:, :n], op=ALU.mult)),
                            ['sn', 'wim'], ['tb'])
                        bop('dve', (lambda e, n=n: e.tensor_tensor(out=X('xre')[:, :n], in0=X('ta')[:, :n], in1=X('tb')[:, :n], op=ALU.subtract)),
                            ['ta', 'tb'], ['xre'])
                        bop('pool', (lambda e, n=n: e.tensor_tensor(out=X('ta')[:, :n], in0=X('sn')[:, :n], in1=X('wre')[:, :n], op=ALU.mult)),
                            ['sn', 'wre'], ['ta'])
                        bop('dve', (lambda e, n=n: e.tensor_tensor(out=X('tb')[:, :n], in0=X('cs')[:, :n], in1=X('wim')[:, :n], op=ALU.mult)),
                            ['cs', 'wim'], ['tb'])
                        bop('pool', (lambda e, n=n: e.tensor_tensor(out=X('xim')[:, :n], in0=X('ta')[:, :n], in1=X('tb')[:, :n], op=ALU.add)),
                            ['ta', 'tb'], ['xim'])
                        s0, s1 = seg
                        p_lo = j0 if d == 0 else s0 + s1 - j0 - n
                        for (a, m) in tiles_of(n, 512):
                            p, n_p = py[npy % 2]
                            npy += 1
                            if d == 0:
                                cols = lambda Z, a=a, m=m: Z[:, a:a + m]
                            else:
                                cols = lambda Z, a=a, m=m, n=n: Z[:, n - a - m:n - a][:, ::-1]
                            P.op('pe', (lambda e, p=p, d=d, s_=s_, m=m, cols=cols: e.matmul(
                                p[:, :m], lhsT=cpad[:, d, s_, 0, :], rhs=cols(X('xre')), start=True, stop=False)),
                                 reads=[n_cpad, B['xre'][1]], writes=[n_p])
                            P.op('pe', (lambda e, p=p, d=d, s_=s_, m=m, cols=cols: e.matmul(
                                p[:, :m], lhsT=cpad[:, d, s_, 1, :], rhs=cols(X('xim')), start=False, stop=True)),
                                 reads=[n_cpad, B['xim'][1]], writes=[n_p])
                            P.op('dve', (lambda e, p=p, p_lo=p_lo, a=a, m=m: e.tensor_tensor(
                                out=Y[:, p_lo + a:p_lo + a + m], in0=Y[:, p_lo + a:p_lo + a + m], in1=p[:, :m], op=ALU.add)),
                                 reads=[n_p, n_Y], writes=[n_Y])
            P.op('act', lambda e: e.activation(out=Y[:], in_=Y[:], func=AF.Gelu_apprx_tanh), reads=[n_Y], writes=[n_Y])
            P.dma(ygT[ct * 128:(ct + 1) * 128, :], Y[:], reads=[n_Y], writes=['ygT'])
    with Stage(P, 's5b%d' % l) as st:
        wst, n_wst = st.sb([128, 4, 512])
        wgb, n_wgb = st.sb([128, 4, 512], BF16)
        bgT, n_bgT = st.sb([128, 4])
        P.dma(wst[:], io['s5_w_glu'][l].rearrange("(k p) n -> p k n", p=128), writes=[n_wst])
        P.dma(bgT[:], io['s5_bgT'][l], writes=[n_bgT])
        P.op('dve', lambda e: e.tensor_copy(out=wgb[:], in_=wst[:]), reads=[n_wst], writes=[n_wgb])
        ygs = [st.sb([128, 4, 512], tag='yg') for _ in range(2)]
        ygb = [st.sb([128, 4, 512], BF16, tag='ygb') for _ in range(2)]
        gts = [st.sb([128, 4, 512], tag='gt') for _ in range(2)]
        sig = [st.sb([128, 512], tag='sig') for _ in range(2)]
        ob = [st.sb([128, 4, 512], BF16, tag='ob') for _ in range(2)]
        pg = [st.ps([128, 512], tag='pg') for _ in range(4)]
        ygv = ygT.rearrange("(k p) t -> p k t", p=128)
        gv = projT[C_SG:C_SG + 512, :].rearrange("(k p) t -> p k t", p=128)
        ov = yT[1024:1536, :].rearrange("(k p) t -> p k t", p=128)
        for gi, (g0, gn) in enumerate(tiles_of(LT, 512)):
            yg, n_yg = ygs[gi % 2]
            yb, n_yb = ygb[gi % 2]
            gt, n_gt = gts[gi % 2]
            o, n_o = ob[gi % 2]
            P.dma(yg[:, :, :gn], ygv[:, :, g0:g0 + gn], reads=['ygT'], writes=[n_yg])
            P.dma(gt[:, :, :gn], gv[:, :, g0:g0 + gn], reads=['projT'], writes=[n_gt])
            P.op('pool', (lambda e, yg=yg, yb=yb, gn=gn: e.tensor_copy(out=yb[:, :, :gn], in_=yg[:, :, :gn])), reads=[n_yg], writes=[n_yb])
            P.op('act', (lambda e, gt=gt, gn=gn: e.activation(out=gt[:, :, :gn], in_=gt[:, :, :gn], func=AF.Silu)), reads=[n_gt], writes=[n_gt])
            for co in range(4):
                p, n_p = pg[co]
                sg_, n_sg = sig[co % 2]
                for k in range(4):
                    P.op('pe', (lambda e, p=p, yb=yb, k=k, co=co, gn=gn: e.matmul(
                        p[:, :gn], lhsT=wgb[:, k, co * 128:(co + 1) * 128], rhs=yb[:, k, :gn], start=(k == 0), stop=(k == 3))),
                         reads=[n_wgb, n_yb], writes=[n_p])
                P.op('act', (lambda e, p=p, sg_=sg_, co=co, gn=gn: e.activation(out=sg_[:, :gn], in_=p[:, :gn], func=AF.Sigmoid,
                                                                               bias=bgT[:, co:co + 1], scale=1.0)),
                     reads=[n_p, n_bgT], writes=[n_sg])
                P.op('dve', (lambda e, sg_=sg_, yg=yg, co=co, gn=gn: e.tensor_tensor(out=sg_[:, :gn], in0=sg_[:, :gn], in1=yg[:, co, :gn], op=ALU.mult)),
                     reads=[n_sg, n_yg], writes=[n_sg])
                P.op('pool', (lambda e, sg_=sg_, gt=gt, o=o, co=co, gn=gn: e.tensor_tensor(out=o[:, co, :gn], in0=sg_[:, :gn], in1=gt[:, co, :gn], op=ALU.mult)),
                     reads=[n_sg, n_gt], writes=[n_o])
            P.dma(ov[:, :, g0:g0 + gn], o[:, :, :gn], reads=[n_o], writes=['yT'])


def hyena_run(P, cfg, l, io, projT, yT, scr, Lh, pos0, sfx):
    NTl = Lh // 128
    NF = NTl + 1
    FP = NF * 128
    Cm, Sm = io['dftC_' + sfx], io['dftS_' + sfx]
    Cv = Cm.rearrange("(k p) f -> p k f", p=128)
    Sv = Sm.rearrange("(k p) f -> p k f", p=128)
    Sr_d, Si_d = scr['Sr'], scr['Si']
    hv_d, hx1_d, hx2_d = scr['hv'], scr['hx1'], scr['hx2T']
    with Stage(P, 'hy1%s%d' % (sfx, l)) as st:
        w1, n_w1 = st.sb([33, 64])
        w2, n_w2 = st.sb([64, 64])
        w3, n_w3 = st.sb([64, 2048])
        fb, n_fb = st.sb([64, 6])
        ft, n_ft = st.sb([33, Lh])
        drep, n_drep = st.sb([128, 1024])
        wgt, n_wgt = st.sb([128, NF])
        P.dma(w1[:], io['hy_w1'][l], writes=[n_w1])
        P.dma(w2[:], io['hy_w2'][l], writes=[n_w2])
        P.dma(w3[:], io['hy_w3'][l], writes=[n_w3])
        P.dma(fb[:, 0:4], io['hy_fb'][l], writes=[n_fb])
        P.dma(ft[:], io['featsT_' + sfx], writes=[n_ft])
        P.dma(drep[:], io['hy_d_rep'][l], writes=[n_drep])
        P.dma(wgt[:], io['wgt_' + sfx], writes=[n_wgt])
        P.op('dve', lambda e: e.tensor_tensor(out=fb[:, 4:5], in0=fb[:, 0:1], in1=fb[:, 1:2], op=ALU.mult), reads=[n_fb], writes=[n_fb])
        P.op('dve', lambda e: e.tensor_tensor(out=fb[:, 5:6], in0=fb[:, 2:3], in1=fb[:, 3:4], op=ALU.mult), reads=[n_fb], writes=[n_fb])
        h1, n_h1 = st.sb([64, Lh])
        h2, n_h2 = st.sb([64, Lh])
        a_, n_a = st.sb([64, 512])
        k_, n_k = st.sb([64, 512])
        pm = [st.ps([128, 512], tag='pm') for _ in range(2)]
        cnt = 0
        for (wm, n_wm, src, n_src, dst, n_dst, fc, bc) in ((w1, n_w1, ft, n_ft, h1, n_h1, 0, 4), (w2, n_w2, h1, n_h1, h2, n_h2, 2, 5)):
            for (g0, gn) in tiles_of(Lh, 512):
                p, n_p = pm[cnt % 2]
                cnt += 1
                P.op('pe', (lambda e, p=p, wm=wm, src=src, g0=g0, gn=gn: e.matmul(p[:64, :gn], lhsT=wm[:], rhs=src[:, g0:g0 + gn],
                                                                                   start=True, stop=True)),
                     reads=[n_wm, n_src], writes=[n_p])
                P.op('dve', (lambda e, p=p, gn=gn, fc=fc, bc=bc: e.tensor_scalar(out=a_[:, :gn], in0=p[:64, :gn], scalar1=fb[:, fc:fc + 1],
                                                                                 scalar2=fb[:, bc:bc + 1], op0=ALU.mult, op1=ALU.add)),
                     reads=[n_p, n_fb], writes=[n_a])
                P.op('dve', (lambda e, gn=gn: e.tensor_scalar(out=k_[:, :gn], in0=a_[:, :gn], scalar1=1.0 / (2 * PI), scalar2=MAGIC,
                                                              op0=ALU.mult, op1=ALU.add)), reads=[n_a], writes=[n_k])
                P.op('dve', (lambda e, gn=gn: e.tensor_scalar(out=k_[:, :gn], in0=k_[:, :gn], scalar1=MAGIC, scalar2=-2 * PI,
                                                              op0=ALU.subtract, op1=ALU.mult)), reads=[n_k], writes=[n_k])
                P.op('dve', (lambda e, gn=gn: e.tensor_tensor(out=a_[:, :gn], in0=a_[:, :gn], in1=k_[:, :gn], op=ALU.add)),
                     reads=[n_a, n_k], writes=[n_a])
                P.op('act', (lambda e, dst=dst, g0=g0, gn=gn: e.activation(out=dst[:, g0:g0 + gn], in_=a_[:, :gn], func=AF.Sin)),
                     reads=[n_a], writes=[n_dst])
        Eb, n_Eb = st.sb([128, NTl, 512], BF16)
        Ob, n_Ob = st.sb([128, NTl, 512], BF16)
        decs = [st.sb([128, 512], tag='dec') for _ in range(2)]
        hf, n_hf = st.sb([128, 512])
        hb, n_hb = st.sb([128, 512])
        cts = [st.sb([128, NTl, 128], BF16, tag='ct') for _ in range(2)]
        sts = [st.sb([128, NTl, 128], BF16, tag='st') for _ in range(2)]
        ps2 = [st.ps([128, 512], tag='ps2') for _ in range(4)]
        t_, n_t = st.sb([128, 512])
        so = [st.sb([128, 512], tag='so') for _ in range(2)]
        nso = 0
        nld = 0
        for o in range(2):
            for kt in range(NTl):
                dec, n_dec = decs[kt % 2]
                P.dma(dec[:], io['dec_' + sfx][kt * 128:(kt + 1) * 128, :], writes=[n_dec])
                for d in range(2):
                    p, n_p = pm[cnt % 2]
                    cnt += 1
                    c0 = o * 1024 + d * 512
                    P.op('pe', (lambda e, p=p, kt=kt, c0=c0: e.matmul(p[:, :], lhsT=h2[:, kt * 128:(kt + 1) * 128], rhs=w3[:, c0:c0 + 512],
                                                                      start=True, stop=True)), reads=[n_h2, n_w3], writes=[n_p])
                    dst, n_dst = (hf, n_hf) if d == 0 else (hb, n_hb)
                    P.op('dve', (lambda e, p=p, dst=dst, dec=dec: e.tensor_tensor(out=dst[:], in0=p[:], in1=dec[:], op=ALU.mult)),
                         reads=[n_p, n_dec], writes=[n_dst])
                if kt == 0:
                    P.op('dve', lambda e: e.memset(hb[0:1, :], 0.0), reads=[n_hb], writes=[n_hb])
                P.op('pool', (lambda e, kt=kt: e.tensor_tensor(out=Eb[:, kt, :], in0=hf[:], in1=hb[:], op=ALU.add)),
                     reads=[n_hf, n_hb], writes=[n_Eb])
                P.op('pool', (lambda e, kt=kt: e.tensor_tensor(out=Ob[:, kt, :], in0=hb[:], in1=hf[:], op=ALU.subtract)),
                     reads=[n_hf, n_hb], writes=[n_Ob])
            for kf in range(NF):
                ct, n_ct = cts[nld % 2]
                stt, n_st = sts[nld % 2]
                nld += 1
                P.dma_k(ct, Cv[:, 0:NTl, kf * 128:(kf + 1) * 128], NTl, writes=[n_ct])
                P.dma_k(stt, Sv[:, 0:NTl, kf * 128:(kf + 1) * 128], NTl, writes=[n_st])
                for c, (mt, n_mt, src, n_src) in enumerate(((ct, n_ct, Eb, n_Eb), (stt, n_st, Ob, n_Ob))):
                    p, n_p = ps2[(kf * 2 + c) % 4]
                    for kt in range(NTl):
                        P.op('pe', (lambda e, p=p, mt=mt, src=src, kt=kt: e.matmul(
                            p[:], lhsT=mt[:, kt, :], rhs=src[:, kt, :], start=(kt == 0), stop=(kt == NTl - 1))),
                             reads=[n_mt, n_src], writes=[n_p])
                    s_o, n_so = so[nso % 2]
                    nso += 1
                    if c == 0:
                        P.op('dve', (lambda e, p=p, o=o: e.tensor_tensor(out=t_[:], in0=p[:], in1=drep[:, o * 512:(o + 1) * 512], op=ALU.add)),
                             reads=[n_p, n_drep], writes=[n_t])
                        P.op('pool', (lambda e, s_o=s_o, kf=kf: e.tensor_scalar(out=s_o[:], in0=t_[:], scalar1=wgt[:, kf:kf + 1], scalar2=None,
                                                                                op0=ALU.mult)), reads=[n_t, n_wgt], writes=[n_so])
                        P.dma(Sr_d[kf * 128:(kf + 1) * 128, o * 512:(o + 1) * 512], s_o[:], reads=[n_so], writes=['Sr'])
                    else:
                        P.op('dve', (lambda e, p=p, s_o=s_o, kf=kf: e.tensor_scalar(out=s_o[:], in0=p[:], scalar1=wgt[:, kf:kf + 1], scalar2=None,
                                                                                    op0=ALU.mult)), reads=[n_p, n_wgt], writes=[n_so])
                        P.dma(Si_d[kf * 128:(kf + 1) * 128, o * 512:(o + 1) * 512], s_o[:], reads=[n_so], writes=['Si'])
    with Stage(P, 'hy2%s%d' % (sfx, l)) as st:
        ident, n_id = st.sb([128, 128])
        P.dma(ident[:], io['ident32'], writes=[n_id])
        cw, n_cw = st.sb([128, 12, 4])
        P.dma(cw[:], io['hy_cw'][l], writes=[n_cw])
        pts = [st.sb([128, Lh + 2], tag='pt') for _ in range(2)]
        ss = [st.sb([128, Lh], tag='s') for _ in range(2)]
        ptp = [st.ps([128, 4, 128], tag='ptp') for _ in range(2)]
        ovb = [st.sb([128, 4, 128], BF16, tag='ovb') for _ in range(2)]
        ovf = [st.sb([128, 4, 128], tag='ovf') for _ in range(2)]
        ntp = 0
        for k in range(12):
            pt, n_pt = pts[k % 2]
            sv, n_sv = ss[k % 2]
            P.op('pool', (lambda e, pt=pt: e.memset(pt[:, 0:1], 0.0)), writes=[n_pt])
            P.op('pool', (lambda e, pt=pt: e.memset(pt[:, Lh + 1:Lh + 2], 0.0)), writes=[n_pt])
            P.dma(pt[:, 1:Lh + 1], projT[C_HP + k * 128:C_HP + (k + 1) * 128, pos0:pos0 + Lh], reads=['projT'], writes=[n_pt])
            P.op('dve', (lambda e, pt=pt, sv=sv, k=k: e.tensor_scalar(out=sv[:], in0=pt[:, 0:Lh], scalar1=cw[:, k, 0:1], scalar2=cw[:, k, 3:4],
                                                                        op0=ALU.mult, op1=ALU.add)), reads=[n_pt, n_cw], writes=[n_sv])
            P.op('dve', (lambda e, pt=pt, sv=sv, k=k: e.scalar_tensor_tensor(out=sv[:], in0=pt[:, 1:Lh + 1], scalar=cw[:, k, 1:2], in1=sv[:],
                                                                               op0=ALU.mult, op1=ALU.add)), reads=[n_pt, n_cw, n_sv], writes=[n_sv])
            P.op('dve', (lambda e, pt=pt, sv=sv, k=k: e.scalar_tensor_tensor(out=sv[:], in0=pt[:, 2:Lh + 2], scalar=cw[:, k, 2:3], in1=sv[:],
                                                                               op0=ALU.mult, op1=ALU.add)), reads=[n_pt, n_cw, n_sv], writes=[n_sv])
            if k >= 8:
                P.dma(hx2_d[(k - 8) * 128:(k - 7) * 128, pos0:pos0 + Lh], sv[:], reads=[n_sv], writes=['hx2T'])
                continue
            for t4 in range(0, NTl, 4):
                nn = min(4, NTl - t4)
                pp, n_pp = ptp[ntp % 2]
                for i in range(nn):
                    P.op('pe', (lambda e, pp=pp, sv=sv, i=i, t4=t4: e.transpose(out=pp[:, i, :], in_=sv[:, (t4 + i) * 128:(t4 + i + 1) * 128],
                                                                                 identity=ident[:])), reads=[n_sv, n_id], writes=[n_pp])
                if k < 4:
                    ob_, n_ob = ovb[ntp % 2]
                    P.op('act', (lambda e, pp=pp, ob_=ob_, nn=nn: e.activation(out=ob_[:, :nn, :], in_=pp[:, :nn, :], func=AF.Copy)),
                         reads=[n_pp], writes=[n_ob])
                    dstv = hv_d[pos0 + t4 * 128:pos0 + (t4 + nn) * 128, k * 128:(k + 1) * 128].rearrange("(i p) c -> p i c", p=128)
                    P.dma(dstv, ob_[:, :nn, :], reads=[n_ob], writes=['hv'])
                else:
                    of_, n_of = ovf[ntp % 2]
                    P.op('act', (lambda e, pp=pp, of_=of_, nn=nn: e.activation(out=of_[:, :nn, :], in_=pp[:, :nn, :], func=AF.Copy)),
                         reads=[n_pp], writes=[n_of])
                    dstv = hx1_d[pos0 + t4 * 128:pos0 + (t4 + nn) * 128, (k - 4) * 128:(k - 3) * 128].rearrange("(i p) c -> p i c", p=128)
                    P.dma(dstv, of_[:, :nn, :], reads=[n_of], writes=['hx1'])
                ntp += 1
    with Stage(P, 'hy3%s%d' % (sfx, l)) as st:
        sig, n_sig = st.sb([128, NTl, 512], BF16)
        A_, n_A = st.sb([128, NF, 512], BF16)
        B_, n_B = st.sb([128, NF, 512], BF16)
        TW = 256 if Lh >= 256 else Lh
        dbuf = [st.sb([128, NF * TW], BF16, tag='dft') for _ in range(4)]
        srs = [st.sb([128, 512], tag='sr') for _ in range(2)]
        sis = [st.sb([128, 512], tag='si') for _ in range(2)]
        x1s = [st.sb([128, 512], tag='x1') for _ in range(2)]
        tq = [st.sb([128, 512], tag='tq') for _ in range(4)]
        pU = [st.ps([128, 512], tag='pU') for _ in range(2)]
        pV = [st.ps([128, 512], tag='pV') for _ in range(2)]
        pZ = [st.ps([128, 512], tag='pZ') for _ in range(4)]
        P.dma_k(sig, hv_d[pos0:pos0 + Lh, :].rearrange("(k p) c -> p k c", p=128), NTl, reads=['hv'], writes=[n_sig])
        for order in range(2):
            for kf in range(NF):
                cb, n_cb = dbuf[(kf % 2) * 2]
                sb_, n_sb = dbuf[(kf % 2) * 2 + 1]
                ct = cb[:, 0:NTl * 128].rearrange("p (k f) -> p k f", f=128)
                stt = sb_[:, 0:NTl * 128].rearrange("p (k f) -> p k f", f=128)
                P.dma_k(ct, Cv[:, 0:NTl, kf * 128:(kf + 1) * 128], NTl, writes=[n_cb])
                P.dma_k(stt, Sv[:, 0:NTl, kf * 128:(kf + 1) * 128], NTl, writes=[n_sb])
                sr, n_sr = srs[kf % 2]
                si, n_si = sis[kf % 2]
                P.dma(sr[:], Sr_d[kf * 128:(kf + 1) * 128, order * 512:(order + 1) * 512], reads=['Sr'], writes=[n_sr])
                P.dma(si[:], Si_d[kf * 128:(kf + 1) * 128, order * 512:(order + 1) * 512], reads=['Si'], writes=[n_si])
                pu, n_pu = pU[kf % 2]
                pv, n_pv = pV[kf % 2]
                for kt in range(NTl):
                    P.op('pe', (lambda e, pu=pu, ct=ct, kt=kt: e.matmul(pu[:], lhsT=ct[:, kt, :], rhs=sig[:, kt, :], start=(kt == 0), stop=(kt == NTl - 1))),
                         reads=[n_cb, n_sig], writes=[n_pu])
                for kt in range(NTl):
                    P.op('pe', (lambda e, pv=pv, stt=stt, kt=kt: e.matmul(pv[:], lhsT=stt[:, kt, :], rhs=sig[:, kt, :], start=(kt == 0), stop=(kt == NTl - 1))),
                         reads=[n_sb, n_sig], writes=[n_pv])
                t1, t2, t3, t4 = tq
                P.op('dve', (lambda e, pu=pu, sr=sr: e.tensor_tensor(out=t1[0][:], in0=pu[:], in1=sr[:], op=ALU.mult)), reads=[n_pu, n_sr], writes=[t1[1]])
                P.op('dve', (lambda e, pv=pv, si=si: e.tensor_tensor(out=t2[0][:], in0=pv[:], in1=si[:], op=ALU.mult)), reads=[n_pv, n_si], writes=[t2[1]])
                P.op('pool', (lambda e, kf=kf: e.tensor_tensor(out=A_[:, kf, :], in0=t1[0][:], in1=t2[0][:], op=ALU.add)), reads=[t1[1], t2[1]], writes=[n_A])
                P.op('dve', (lambda e, pv=pv, sr=sr: e.tensor_tensor(out=t3[0][:], in0=pv[:], in1=sr[:], op=ALU.mult)), reads=[n_pv, n_sr], writes=[t3[1]])
                P.op('dve', (lambda e, pu=pu, si=si: e.tensor_tensor(out=t4[0][:], in0=pu[:], in1=si[:], op=ALU.mult)), reads=[n_pu, n_si], writes=[t4[1]])
                P.op('pool', (lambda e, kf=kf: e.tensor_tensor(out=B_[:, kf, :], in0=t3[0][:], in1=t4[0][:], op=ALU.subtract)), reads=[t3[1], t4[1]], writes=[n_B])
            if order == 0:
                for kt in range(NTl):
                    cb, n_cb = dbuf[(kt % 2) * 2]
                    sb_, n_sb = dbuf[(kt % 2) * 2 + 1]
                    ct = cb[:, 0:NF * 128].rearrange("p (k f) -> p k f", f=128)
                    stt = sb_[:, 0:NF * 128].rearrange("p (k f) -> p k f", f=128)
                    P.dma_k(ct, Cv[:, 0:NF, kt * 128:(kt + 1) * 128], NF, writes=[n_cb])
                    P.dma_k(stt, Sv[:, 0:NF, kt * 128:(kt + 1) * 128], NF, writes=[n_sb])
                    x1, n_x1 = x1s[kt % 2]
                    P.dma(x1[:], hx1_d[pos0 + kt * 128:pos0 + (kt + 1) * 128, :], reads=['hx1'], writes=[n_x1])
                    pz, n_pz = pZ[kt % 4]
                    for kf in range(NF):
                        P.op('pe', (lambda e, pz=pz, ct=ct, kf=kf: e.matmul(pz[:], lhsT=ct[:, kf, :], rhs=A_[:, kf, :], start=(kf == 0), stop=False)),
                             reads=[n_cb, n_A], writes=[n_pz])
                    for kf in range(NF):
                        P.op('pe', (lambda e, pz=pz, stt=stt, kf=kf: e.matmul(pz[:], lhsT=stt[:, kf, :], rhs=B_[:, kf, :], start=False, stop=(kf == NF - 1))),
                             reads=[n_sb, n_B], writes=[n_pz])
                    P.op('dve', (lambda e, pz=pz, x1=x1, kt=kt: e.tensor_tensor(out=sig[:, kt, :], in0=pz[:], in1=x1[:], op=ALU.mult)),
                         reads=[n_pz, n_x1], writes=[n_sig])
            else:
                x2s = [st.sb([128, 4, TW], tag='x2') for _ in range(2)]
                gts = [st.sb([128, 4, TW], tag='g') for _ in range(2)]
                obs = [st.sb([128, 4, TW], BF16, tag='ob') for _ in range(2)]
                x2v = hx2_d.rearrange("(k p) t -> p k t", p=128)
                gv = projT[C_HG:C_HG + 512, :].rearrange("(k p) t -> p k t", p=128)
                ov = yT[1536:2048, :].rearrange("(k p) t -> p k t", p=128)
                for gi, (g0, gn) in enumerate(tiles_of(Lh, TW)):
                    cb, n_cb = dbuf[(gi % 2) * 2]
                    sb_, n_sb = dbuf[(gi % 2) * 2 + 1]
                    ct = cb[:, 0:NF * gn].rearrange("p (k t) -> p k t", t=gn)
                    stt = sb_[:, 0:NF * gn].rearrange("p (k t) -> p k t", t=gn)
                    P.dma_k(ct, Cv[:, 0:NF, g0:g0 + gn], NF, writes=[n_cb])
                    P.dma_k(stt, Sv[:, 0:NF, g0:g0 + gn], NF, writes=[n_sb])
                    x2, n_x2 = x2s[gi % 2]
                    gt, n_gt = gts[gi % 2]
                    ob_, n_ob = obs[gi % 2]
                    P.dma(x2[:, :, :gn], x2v[:, :, pos0 + g0:pos0 + g0 + gn], reads=['hx2T'], writes=[n_x2])
                    P.dma(gt[:, :, :gn], gv[:, :, pos0 + g0:pos0 + g0 + gn], reads=['projT'], writes=[n_gt])
                    P.op('act', (lambda e, gt=gt, gn=gn: e.activation(out=gt[:, :, :gn], in_=gt[:, :, :gn], func=AF.Silu)), reads=[n_gt], writes=[n_gt])
                    P.op('pool', (lambda e, gt=gt, x2=x2, gn=gn: e.tensor_tensor(out=gt[:, :, :gn], in0=gt[:, :, :gn], in1=x2[:, :, :gn], op=ALU.mult)),
                         reads=[n_gt, n_x2], writes=[n_gt])
                    for cq in range(4):
                        pz, n_pz = pZ[cq]
                        for kf in range(NF):
                            P.op('pe', (lambda e, pz=pz, ct=ct, kf=kf, cq=cq, gn=gn: e.matmul(
                                pz[:, :gn], lhsT=A_[:, kf, cq * 128:(cq + 1) * 128], rhs=ct[:, kf, :], start=(kf == 0), stop=False)),
                                 reads=[n_cb, n_A], writes=[n_pz])
                        for kf in range(NF):
                            P.op('pe', (lambda e, pz=pz, stt=stt, kf=kf, cq=cq, gn=gn: e.matmul(
                                pz[:, :gn], lhsT=B_[:, kf, cq * 128:(cq + 1) * 128], rhs=stt[:, kf, :], start=False, stop=(kf == NF - 1))),
                                 reads=[n_sb, n_B], writes=[n_pz])
                        P.op('dve', (lambda e, pz=pz, gt=gt, ob_=ob_, cq=cq, gn=gn: e.tensor_tensor(
                            out=ob_[:, cq, :gn], in0=pz[:, :gn], in1=gt[:, cq, :gn], op=ALU.mult)), reads=[n_pz, n_gt], writes=[n_ob])
                    P.dma(ov[:, :, pos0 + g0:pos0 + g0 + gn], ob_[:, :, :gn], reads=[n_ob], writes=['yT'])


def stage_hyena(P, cfg, l, io, projT, yT, scr):
    L, LC = cfg['L'], cfg['LC']
    if l < cfg['DEPTH'] - 1:
        hyena_run(P, cfg, l, io, projT, yT, scr, LC, 0, 'C')
    hyena_run(P, cfg, l, io, projT, yT, scr, L, LC, 'L')


INPUT_SPECS = None


def input_shapes(cfg):
    L, LC, D, DEPTH = cfg['L'], cfg['LC'], cfg['D'], cfg['DEPTH']
    return {
        'x': ([L, D], F32), 'ctx': ([LC, D], F32), 'ccT': ([128, D // 128, 2], F32),
        'w_mod': ([DEPTH, D, 3 * D], F32), 'b_mod2': ([DEPTH, 2, 3 * D], F32),
        'g_pre_rep': ([DEPTH, 128, D], F32), 'g_post_rep': ([DEPTH, 128, D], F32),
        'w_in': ([DEPTH, D, NCOL], F32), 'w_out': ([DEPTH, 2048, D], F32),
        'ident': ([128, 128], BF16), 'sel2': ([2, 2, 128], F32),
        'maskf': ([128, 128], BF16), 'maskb': ([128, 128], BF16),
        'gla_wg': ([DEPTH, 16, 2, 512], F32), 'gla_bT': ([DEPTH, 128, 8], F32), 'gla_nT': ([DEPTH, 128, 2], F32),
        'ident32': ([128, 128], F32), 'iota': ([128, 1024], F32),
        's5_sc': ([DEPTH, 128, 2, 16, 3], F32), 's5_cpad': ([DEPTH, 128, 2, 16, 2, 128], F32),
        's5_bpad': ([DEPTH, 128, 2, 16, 2, 128], F32), 's5_dT': ([DEPTH, 128, 4], F32), 's5_bgT': ([DEPTH, 128, 4], F32),
        's5_w_glu': ([DEPTH, 512, 512], F32),
        'hy_w1': ([DEPTH, 33, 64], F32), 'hy_w2': ([DEPTH, 64, 64], F32), 'hy_w3': ([DEPTH, 64, 2048], F32),
        'hy_fb': ([DEPTH, 64, 4], F32), 'hy_cw': ([DEPTH, 128, 12, 4], F32), 'hy_d_rep': ([DEPTH, 128, 1024], F32),
        'featsT_L': ([33, L], F32), 'featsT_C': ([33, LC], F32), 'dec_L': ([L, 512], F32), 'dec_C': ([LC, 512], F32),
        'dftC_L': ([L + 128, L + 128], BF16), 'dftS_L': ([L + 128, L + 128], BF16),
        'dftC_C': ([LC + 128, LC + 128], BF16), 'dftS_C': ([LC + 128, LC + 128], BF16),
        'wgt_L': ([128, L // 128 + 1], F32), 'wgt_C': ([128, LC // 128 + 1], F32),
    }


def build_program(cfg, mixers=('gla', 's5', 'hy'), debug=False):
    L, LC, D, DEPTH = cfg['L'], cfg['LC'], cfg['D'], cfg['DEPTH']
    LT = L + LC
    nc = bass.Bass("TRN2", target_bir_lowering=False)
    io = {}
    for name, (shape, dt) in input_shapes(cfg).items():
        io[name] = nc.dram_tensor(name, list(shape), dt, kind="ExternalInput").ap()
    out = nc.dram_tensor("out", [L, D], F32, kind="ExternalOutput").ap()
    with ExitStack() as es:
        P = Prog(nc, es)
        res = [P.dram('res%d' % i, [L, D]) for i in range(2)]
        cres = [P.dram('cres%d' % i, [LC, D]) for i in range(2)]
        rep = [P.dram('rep%d' % i, [128, D]) for i in range(6)]
        projT = P.dram('projT', [NCOL, LT], out=debug)
        projK = P.dram('projK', [LT, 1536], out=debug)
        yT = P.dram('yT', [2048, LT], BF16, out=debug)
        ygT = P.dram('ygT', [512, LT], F32, out=debug)
        scr = {'Sr': P.dram('hSr', [L + 128, 1024]), 'Si': P.dram('hSi', [L + 128, 1024]),
               'hv': P.dram('hv', [LT, 512], BF16), 'hx1': P.dram('hx1', [LT, 512]), 'hx2T': P.dram('hx2T', [512, LT])}
        for l in range(DEPTH):
            src_lat = io['x'] if l == 0 else res[(l - 1) % 2]
            src_ctx = io['ctx'] if l == 0 else cres[(l - 1) % 2]
            dst_lat = out if l == DEPTH - 1 else res[l % 2]
            dst_ctx = cres[l % 2]
            stage_mod(P, cfg, l, io, rep)
            stage_inproj(P, cfg, l, io, rep, src_lat, src_ctx, projT, projK)
            if mixers == 'stub':
                stage_stub_mixer(P, cfg, l, projT, yT)
            else:
                if 'gla' in mixers:
                    stage_gla(P, cfg, l, io, projT, projK, yT)
                if 's5' in mixers:
                    stage_s5(P, cfg, l, io, projT, ygT, yT)
                if 'hy' in mixers:
                    stage_hyena(P, cfg, l, io, projT, yT, scr)
            stage_outproj(P, cfg, l, io, rep, yT, src_lat, src_ctx, dst_lat, dst_ctx)
    return nc


_HC = {}


def hyena_consts(cfg):
    key = (cfg['L'], cfg['LC'])
    if key in _HC:
        return _HC[key]
    out = {}
    f32 = np.float32
    for sfx, Lh in (('L', cfg['L']), ('C', cfg['LC'])):
        t = np.linspace(0.0, 1.0, Lh, dtype=f32)[:, None]
        ang = (f32(2.0 * math.pi / Lh) * np.arange(Lh, dtype=f32))[:, None]
        bands = np.linspace(1e-4, 15.0, 16, dtype=f32)[None, :]
        feats = np.concatenate([t, np.cos(bands * ang), -np.sin(bands * ang)], axis=-1).astype(f32)
        out['featsT_' + sfx] = np.ascontiguousarray(feats.T)
        deltas = np.abs(np.linspace(math.log(1e-2) / 0.3, math.log(1e-2) / 1.5, 512, dtype=f32))
        out['dec_' + sfx] = np.exp(-t * deltas[None, :]).astype(f32)
        N = 2 * Lh
        FP = Lh + 128
        a = np.arange(Lh + 1, dtype=np.int64)
        prod = (a[:, None] * a[None, :]) % N
        angm = prod.astype(np.float64) * (2.0 * math.pi / N)
        C = np.zeros((FP, FP), np.float32)
        S = np.zeros((FP, FP), np.float32)
        C[:Lh + 1, :Lh + 1] = np.cos(angm)
        S[:Lh + 1, :Lh + 1] = np.sin(angm)
        out['dftC_' + sfx] = C.astype(ml_dtypes.bfloat16)
        out['dftS_' + sfx] = S.astype(ml_dtypes.bfloat16)
        w = np.zeros(FP, np.float32)
        w[:Lh + 1] = 2.0 / N
        w[0] = 1.0 / N
        w[Lh] = 1.0 / N
        out['wgt_' + sfx] = np.ascontiguousarray(w.reshape(FP // 128, 128).T)
    _HC[key] = out
    return out


def prep_inputs(cfg, b, inp):
    D, DEPTH = cfg['D'], cfg['DEPTH']
    f = lambda a: np.ascontiguousarray(np.asarray(a), dtype=np.float32)
    m = {}
    m['x'] = f(inp['x'][b])
    m['ctx'] = f(inp['ctx'][b])
    cc = np.stack([np.asarray(inp['c'][b]), np.asarray(inp['c_ctx'])], axis=-1)
    m['ccT'] = f(cc.reshape(D // 128, 128, 2).transpose(1, 0, 2))
    m['w_mod'] = f(inp['w_mod'])
    m['b_mod2'] = f(np.repeat(np.asarray(inp['b_mod'])[:, None, :], 2, axis=1))
    m['g_pre_rep'] = f(np.repeat(np.asarray(inp['g_pre'])[:, None, :], 128, axis=1))
    m['g_post_rep'] = f(np.repeat(np.asarray(inp['g_post'])[:, None, :], 128, axis=1))
    m['w_in'] = f(inp['w_in'])
    m['w_out'] = f(inp['w_out'])
    m['ident'] = np.eye(128, dtype=np.float32).astype(ml_dtypes.bfloat16)
    sel = np.zeros((2, 2, 128), np.float32)
    sel[0, 0, :] = 1.0
    sel[1, 1, :] = 1.0
    m['sel2'] = sel
    ii = np.arange(128)
    m['maskf'] = (ii[:, None] <= ii[None, :]).astype(np.float32).astype(ml_dtypes.bfloat16)
    m['maskb'] = (ii[:, None] + ii[None, :] >= 127).astype(np.float32).astype(ml_dtypes.bfloat16)
    m['gla_wg'] = f(np.asarray(inp['gla_w_gate']).transpose(0, 2, 1, 3))
    bg = np.asarray(inp['gla_b_gate']).reshape(DEPTH, 2, 4, 128)
    m['gla_bT'] = f(bg.transpose(0, 3, 1, 2).reshape(DEPTH, 128, 8))
    m['gla_nT'] = f(np.asarray(inp['gla_norm']).reshape(DEPTH, 2, 128).transpose(0, 2, 1))
    m['ident32'] = np.eye(128, dtype=np.float32)
    m['iota'] = f(np.repeat(np.arange(1024, dtype=np.float32)[None, :], 128, axis=0))

    def st_layout(a):
        a = np.asarray(a)
        sh = a.shape
        a = a.reshape(sh[0], 2, 16, 2, 64, *sh[4:])
        nd = a.ndim
        a = a.transpose(0, 3, 4, 1, 2, *range(5, nd))
        return a.reshape(sh[0], 128, 2, 16, *sh[4:])
    ls = np.broadcast_to(np.asarray(inp['s5_log_step'])[..., None], np.asarray(inp['s5_lam_re']).shape)
    m['s5_sc'] = f(np.stack([st_layout(inp['s5_lam_re']), st_layout(inp['s5_lam_im']), st_layout(ls)], axis=-1))

    def pad_layout(re, im):
        a = np.stack([st_layout(re), st_layout(im)], axis=4)
        out = np.zeros((DEPTH, 128, 2, 16, 2, 128), np.float32)
        for s_ in range(16):
            for gl in range(2):
                cb = 16 * (2 * (s_ % 4) + gl)
                out[:, gl * 64:(gl + 1) * 64, :, s_, :, cb:cb + 16] = a[:, gl * 64:(gl + 1) * 64, :, s_, :, :]
        return out
    m['s5_bpad'] = pad_layout(inp['s5_b_re'], inp['s5_b_im'])
    m['s5_cpad'] = pad_layout(np.asarray(inp['s5_c_re']).transpose(0, 1, 2, 4, 3), np.asarray(inp['s5_c_im']).transpose(0, 1, 2, 4, 3))
    m['s5_dT'] = f(np.asarray(inp['s5_d']).reshape(DEPTH, 4, 128).transpose(0, 2, 1))
    m['s5_bgT'] = f(np.asarray(inp['s5_b_glu']).reshape(DEPTH, 4, 128).transpose(0, 2, 1))
    m['s5_w_glu'] = f(inp['s5_w_glu'])
    m['hy_w1'] = f(inp['hy_w1'])
    m['hy_w2'] = f(inp['hy_w2'])
    m['hy_w3'] = f(inp['hy_w3'])
    m['hy_fb'] = f(np.stack([np.asarray(inp['hy_f1']), np.asarray(inp['hy_b1']), np.asarray(inp['hy_f2']), np.asarray(inp['hy_b2'])], axis=-1))
    cw = np.concatenate([np.asarray(inp['hy_conv_w']), np.asarray(inp['hy_conv_b'])[:, None, :]], axis=1)
    m['hy_cw'] = f(cw.reshape(DEPTH, 4, 12, 128).transpose(0, 3, 2, 1))
    m['hy_d_rep'] = f(np.repeat(np.asarray(inp['hy_d']).reshape(DEPTH, 1, 1024), 128, axis=1))
    m.update(hyena_consts(cfg))
    return m


def kernel(**inputs):
    cfg = CFG
    nc = build_program(cfg)
    B = inputs['x'].shape[0]
    outs = []
    for b in range(B):
        res = run_bass_kernel_spmd(nc, [prep_inputs(cfg, b, inputs)], core_ids=[0])
        outs.append(np.asarray(res.results[0]['out']))
    return np.stack(outs, axis=0).astype(np.float32)
```

```python
import math
import numpy as np
import ml_dtypes
from contextlib import ExitStack
import concourse.bass as bass
import concourse.mybir as mybir
from concourse.bass_utils import run_bass_kernel_spmd

F32 = mybir.dt.float32
BF16 = mybir.dt.bfloat16
AF = mybir.ActivationFunctionType
ALU = mybir.AluOpType
AX = mybir.AxisListType

CFG = dict(L=4096, LC=256, D=2048, W=64, DEPTH=4)
EPS = 1e-6
C_Q, C_K, C_V, C_LR, C_U, C_GG, C_SG, C_HP, C_HG = 0, 512, 1024, 2048, 2080, 2592, 3616, 4128, 5664
NCOL = 6176
PI = math.pi


class Prog:
    ENGS = ('pe', 'act', 'dve', 'pool', 'sp')
    NDSEM = 16

    def __init__(s, nc, es):
        s.nc = nc
        s.es = es
        s.sem = {e: es.enter_context(nc.semaphore('s_' + e)) for e in ('pe', 'act', 'dve', 'pool')}
        s.dsem = [es.enter_context(nc.semaphore('d%d' % i)) for i in range(s.NDSEM)]
        s.barA = es.enter_context(nc.semaphore('barA'))
        s.barB = es.enter_context(nc.semaphore('barB'))
        s.nbar = 0
        s.cnt = {e: 0 for e in s.ENGS}
        s.dcnt = 0
        s.ops = {e: [] for e in s.ENGS}
        s.waited = {e: {} for e in s.ENGS}
        s.bufs = {}
        s.uid = 0
        s.rr = 0

    def dram(s, name, shape, dt=F32, out=False):
        if out:
            return s.nc.dram_tensor(name, list(shape), dt, kind="ExternalOutput").ap()
        return s.nc.dram_tensor(name, list(shape), dt).ap()

    def _need(s, eng, tok, waits):
        if tok is None:
            return
        kind, a, v = tok
        if kind == 'c' and a == 'pe' and eng == 'pe':
            return
        key = (kind, a)
        if s.waited[eng].get(key, 0) >= v:
            return
        s.waited[eng][key] = v
        waits.append((s.sem[a] if kind == 'c' else s.dsem[a], v))

    def _deps(s, eng, reads, writes, waits, grp=None):
        for b in reads:
            st = s.bufs.setdefault(b, [[], {}, None])
            for t in st[0]:
                s._need(eng, t, waits)
        for b in writes:
            st = s.bufs.setdefault(b, [[], {}, None])
            if grp is not None and st[2] == grp:
                continue
            for t in st[0]:
                s._need(eng, t, waits)
            for (k, a), v in st[1].items():
                s._need(eng, (k, a, v), waits)

    def _commit(s, tok, reads, writes, grp=None):
        for b in reads:
            d = s.bufs[b][1]
            key = (tok[0], tok[1])
            if d.get(key, 0) < tok[2]:
                d[key] = tok[2]
        for b in writes:
            st = s.bufs[b]
            if grp is not None and st[2] == grp:
                st[0].append(tok)
            else:
                s.bufs[b] = [[tok], {}, grp]

    def op(s, eng, fn, reads=(), writes=()):
        waits = []
        s._deps(eng, reads, writes, waits)
        s.cnt[eng] += 1
        tok = ('c', eng, s.cnt[eng])
        s.ops[eng].append((waits, fn, s.sem[eng], 1))
        s._commit(tok, reads, writes)
        return tok

    def dma(s, out, in_, reads=(), writes=(), q='sp', grp=None, **kw):
        i = s.dcnt
        s.dcnt += 1
        si = i % s.NDSEM
        val = 16 * (i // s.NDSEM + 1)
        waits = []
        if val > 16:
            s._need(q, ('d', si, val - 16), waits)
        s._deps(q, reads, writes, waits, grp)
        tok = ('d', si, val)
        s.ops[q].append((waits, (lambda e: e.dma_start(out=out, in_=in_, **kw)), s.dsem[si], 16))
        s._commit(tok, reads, writes, grp)
        return tok

    def dma_k(s, out, in_, nk, reads=(), writes=(), kstep=8):
        s.uid += 1
        g = 'g%d' % s.uid
        for k0 in range(0, nk, kstep):
            k1 = min(nk, k0 + kstep)
            s.dma(out[:, k0:k1, :], in_[:, k0:k1, :], reads=reads, writes=writes, grp=g)

    def barrier(s):
        n = s.dcnt
        s.nbar += 1
        m = s.nbar
        for eng in s.ENGS:
            waits = []
            for si in range(min(n, s.NDSEM)):
                c = (n - si + s.NDSEM - 1) // s.NDSEM
                s._need(eng, ('d', si, 16 * c), waits)
            for e in ('pe', 'act', 'dve', 'pool'):
                if s.cnt[e] and e != eng:
                    s._need(eng, ('c', e, s.cnt[e]), waits)
            s.ops[eng].append((waits, 'arrive', None, 0))
        s.ops['sp'].append(([(s.barA, 5 * m)], 'clear', None, 0))
        for eng in s.ENGS:
            s.ops[eng].append(([(s.barB, m)], None, None, 0))
        s.bufs = {}
        s.cnt = {e: 0 for e in s.ENGS}
        s.dcnt = 0
        s.waited = {e: {} for e in s.ENGS}

    def emit(s):
        s.barrier()
        nc = s.nc

        def run(name, e):
            for waits, fn, sem, inc in s.ops[name]:
                for (sm, v) in waits:
                    e.wait_ge(sm, v)
                if fn is None:
                    continue
                if fn == 'arrive':
                    e.sem_inc(s.barA, 1)
                elif fn == 'clear':
                    for sm in list(s.sem.values()) + s.dsem:
                        e.sem_clear(sm)
                    e.sem_inc(s.barB, 1)
                else:
                    fn(e).then_inc(sem, inc)
            s.ops[name] = []

        with nc.Block() as block:
            @block.sync
            def _(e):
                run('sp', e)

            @block.tensor
            def _(e):
                run('pe', e)

            @block.scalar
            def _(e):
                run('act', e)

            @block.vector
            def _(e):
                run('dve', e)

            @block.gpsimd
            def _(e):
                run('pool', e)

    def ew(s):
        s.rr += 1
        return ('dve', 'pool')[s.rr % 2]


class Stage:
    def __init__(s, P, name):
        s.P = P
        P.uid += 1
        s.name = '%s_%d' % (name, P.uid)
        s.es = ExitStack()
        s.n = 0

    def __enter__(s):
        s.es.__enter__()
        return s

    def __exit__(s, *a):
        if a[0] is None:
            s.P.emit()
        return s.es.__exit__(*a)

    def sb(s, shape, dt=F32, tag='t'):
        s.n += 1
        nm = '%s_%s%d' % (s.name, tag, s.n)
        t = s.es.enter_context(s.P.nc.sbuf_tensor(nm, list(shape), dt))
        return t, nm

    def ps(s, shape, dt=F32, tag='p'):
        s.n += 1
        nm = '%s_%s%d' % (s.name, tag, s.n)
        t = s.es.enter_context(s.P.nc.psum_tensor(nm, list(shape), dt))
        return t, nm


def tiles_of(n, step):
    return [(i, min(step, n - i)) for i in range(0, n, step)]


def stage_mod(P, cfg, l, io, rep):
    D = cfg['D']
    KD = D // 128
    NW = 256
    with Stage(P, 'mod%d' % l) as st:
        cc, n_cc = st.sb([128, KD, 2])
        sc, n_sc = st.sb([128, KD, 2])
        mod, n_mod = st.sb([2, 3 * D])
        bm, n_bm = st.sb([2, 3 * D])
        sel, n_sel = st.sb([2, 2, 128])
        wt = [st.sb([128, KD, NW], tag='w') for _ in range(2)]
        pm = [st.ps([2, NW], tag='pm') for _ in range(2)]
        pr = [st.ps([128, 512], tag='pr') for _ in range(2)]
        gp, n_gp = st.sb([128, D])
        go, n_go = st.sb([128, D])
        rt = [st.sb([128, 512], tag='rt') for _ in range(3)]
        P.dma(cc[:], io['ccT'], writes=[n_cc])
        P.dma(bm[:], io['b_mod2'][l], writes=[n_bm])
        P.dma(sel[:], io['sel2'], writes=[n_sel])
        P.dma(gp[:], io['g_pre_rep'][l], writes=[n_gp])
        P.dma(go[:], io['g_post_rep'][l], writes=[n_go])
        P.op('act', lambda e: e.activation(out=sc[:], in_=cc[:], func=AF.Silu), reads=[n_cc], writes=[n_sc])
        wv = io['w_mod'][l].rearrange("(k p) n -> p k n", p=128)
        for j, (n0, nn) in enumerate(tiles_of(3 * D, NW)):
            w, n_w = wt[j % 2]
            p, n_p = pm[j % 2]
            P.dma(w[:, :, :nn], wv[:, :, n0:n0 + nn], writes=[n_w])
            for k in range(KD):
                P.op('pe', (lambda e, w=w, p=p, k=k, nn=nn: e.matmul(p[:, :nn], lhsT=sc[:, k, :], rhs=w[:, k, :nn],
                                                                      start=(k == 0), stop=(k == KD - 1))),
                     reads=[n_w, n_sc], writes=[n_p])
            P.op('dve', (lambda e, p=p, n0=n0, nn=nn: e.tensor_tensor(out=mod[:, n0:n0 + nn], in0=p[:, :nn],
                                                                      in1=bm[:, n0:n0 + nn], op=ALU.add)),
                 reads=[n_p, n_bm], writes=[n_mod])
        for r in range(2):
            for j, (n0, nn) in enumerate(tiles_of(D, 512)):
                outs = []
                for which in range(3):
                    p, n_p = pr[(j * 3 + which) % 2]
                    t, n_t = rt[which]
                    P.op('pe', (lambda e, p=p, which=which, n0=n0, nn=nn, r=r: e.matmul(
                        p[:, :nn], lhsT=sel[:, r, :], rhs=mod[:, which * D + n0: which * D + n0 + nn],
                        start=True, stop=True)), reads=[n_mod, n_sel], writes=[n_p])
                    if which == 0:
                        P.op('act', (lambda e, p=p, t=t, nn=nn: e.activation(out=t[:, :nn], in_=p[:, :nn], func=AF.Copy)),
                             reads=[n_p], writes=[n_t])
                    elif which == 1:
                        P.op('dve', (lambda e, p=p, t=t, n0=n0, nn=nn: e.scalar_tensor_tensor(
                            out=t[:, :nn], in0=p[:, :nn], scalar=1.0, in1=gp[:, n0:n0 + nn], op0=ALU.add, op1=ALU.mult)),
                             reads=[n_p, n_gp], writes=[n_t])
                    else:
                        P.op('dve', (lambda e, p=p, t=t, n0=n0, nn=nn: e.tensor_tensor(
                            out=t[:, :nn], in0=p[:, :nn], in1=go[:, n0:n0 + nn], op=ALU.mult)),
                             reads=[n_p, n_go], writes=[n_t])
                    dst = {0: 1, 1: 0, 2: 2}[which] + 3 * r
                    P.dma(rep[dst][:, n0:n0 + nn], t[:, :nn], reads=[n_t], writes=['rep%d' % dst])


def pos_row_dmas(cfg, l, i):
    L, LC, W = cfg['L'], cfg['LC'], cfg['W']
    nct = LC // 128
    if i < nct:
        return [(0, 128, 'c', (i * 128, 1, 128))]
    p0 = (i - nct) * 128
    if l % 2 == 0:
        return [(0, 128, 'l', (p0, 1, 128))]
    R = L // W
    out = []
    p = p0
    while p < p0 + 128:
        c, r = p // R, p % R
        n = min(R - r, p0 + 128 - p)
        out.append((p - p0, n, 'l', (r * W + c, W, n)))
        p += n
    return out


def row_ap(t2d, sel):
    start, step, n = sel
    if step == 1:
        return t2d[start:start + n, :]
    return t2d[start:start + (n - 1) * step + 1:step, :]


def stage_inproj(P, cfg, l, io, rep, src_lat, src_ctx, projT, projK):
    L, LC, D = cfg['L'], cfg['LC'], cfg['D']
    LT = L + LC
    NT = LT // 128
    KD = D // 128
    nct = LC // 128
    TBT = 12
    with Stage(P, 'inp%d' % l) as st:
        ident, n_id = st.sb([128, 128], BF16)
        P.dma(ident[:], io['ident'], writes=[n_id])
        reps = [st.sb([128, D], tag='rep') for _ in range(4)]
        for j, r in enumerate((0, 1, 3, 4)):
            P.dma(reps[j][0][:], rep[r], reads=['rep%d' % r], writes=[reps[j][1]])
        hT, n_hT = st.sb([128, KD, TBT * 128], BF16)
        xts = [st.sb([128, D], tag='x') for _ in range(2)]
        junk, n_junk = st.sb([128, D])
        t1s = [st.sb([128, D], tag='t1') for _ in range(2)]
        hbs = [st.sb([128, D], BF16, tag='hb') for _ in range(2)]
        sss = [st.sb([128, 2], tag='ss') for _ in range(2)]
        tps = [st.ps([128, 8, 128], BF16, tag='tp') for _ in range(2)]
        wst = [st.sb([128, KD, 128], tag='wst') for _ in range(2)]
        wbs = [st.sb([128, KD, 128], BF16, tag='wb') for _ in range(2)]
        wkv, n_wkv = st.sb([128, KD, 512], BF16)
        pms = [st.ps([128, 512], tag='pm') for _ in range(4)]
        o32 = [st.sb([128, TBT * 128], tag='o') for _ in range(2)]
        ok32 = [st.sb([128, 512], tag='ok') for _ in range(2)]
        wv = io['w_in'][l].rearrange("(k p) n -> p k n", p=128)
        npm = 0
        for b0 in range(0, NT, TBT):
            btiles = list(range(b0, min(NT, b0 + TBT)))
            nb = len(btiles)
            for ti, i in enumerate(btiles):
                xt, n_x = xts[i % 2]
                t1, n_t1 = t1s[i % 2]
                hb, n_hb = hbs[i % 2]
                ss, n_ss = sss[i % 2]
                for (pp, npp, kind, sel) in pos_row_dmas(cfg, l, i):
                    src = src_ctx if kind == 'c' else src_lat
                    P.dma(xt[pp:pp + npp, :], row_ap(src, sel), reads=['res_' + kind], writes=[n_x])
                G, n_G = reps[0] if i >= nct else reps[2]
                S, n_S = reps[1] if i >= nct else reps[3]
                P.op('dve', (lambda e, ss=ss: e.memset(ss[:], 0.0)), writes=[n_ss])
                P.op('act', (lambda e, xt=xt, ss=ss: e.activation(out=junk[:], in_=xt[:], func=AF.Square,
                                                                    accum_out=ss[:, 0:1])),
                     reads=[n_x], writes=[n_junk, n_ss])
                P.op('dve', (lambda e, ss=ss: e.tensor_scalar(out=ss[:, 1:2], in0=ss[:, 0:1], scalar1=1.0 / D,
                                                               scalar2=EPS, op0=ALU.mult, op1=ALU.add)),
                     reads=[n_ss], writes=[n_ss])
                P.op('act', (lambda e, ss=ss: e.activation(out=ss[:, 1:2], in_=ss[:, 1:2], func=AF.Sqrt)),
                     reads=[n_ss], writes=[n_ss])
                P.op('dve', (lambda e, ss=ss: e.reciprocal(out=ss[:, 1:2], in_=ss[:, 1:2])),
                     reads=[n_ss], writes=[n_ss])
                P.op('dve', (lambda e, xt=xt, ss=ss, t1=t1, G=G: e.scalar_tensor_tensor(
                    out=t1[:], in0=xt[:], scalar=ss[:, 1:2], in1=G[:], op0=ALU.mult, op1=ALU.mult)),
                     reads=[n_x, n_ss, n_G], writes=[n_t1])
                P.op('pool', (lambda e, t1=t1, hb=hb, S=S: e.tensor_tensor(out=hb[:], in0=t1[:], in1=S[:], op=ALU.add)),
                     reads=[n_t1, n_S], writes=[n_hb])
                for k8 in range(0, KD, 8):
                    kk = min(8, KD - k8)
                    tp, n_tp = tps[(k8 // 8 + i) % 2]
                    for k in range(kk):
                        P.op('pe', (lambda e, tp=tp, hb=hb, k=k, k8=k8: e.transpose(
                            out=tp[:, k, :], in_=hb[:, (k8 + k) * 128:(k8 + k + 1) * 128], identity=ident[:])),
                             reads=[n_hb, n_id], writes=[n_tp])
                    eng = 'act' if (k8 // 8) % 2 == 0 else 'dve'
                    if eng == 'act':
                        P.op('act', (lambda e, tp=tp, k8=k8, kk=kk, ti=ti: e.activation(
                            out=hT[:, k8:k8 + kk, ti * 128:(ti + 1) * 128], in_=tp[:, :kk, :], func=AF.Copy)),
                             reads=[n_tp], writes=[n_hT])
                    else:
                        P.op('dve', (lambda e, tp=tp, k8=k8, kk=kk, ti=ti: e.tensor_copy(
                            out=hT[:, k8:k8 + kk, ti * 128:(ti + 1) * 128], in_=tp[:, :kk, :])),
                             reads=[n_tp], writes=[n_hT])
            for ci, (c0, cn) in enumerate(tiles_of(NCOL, 128)):
                ws, n_ws = wst[ci % 2]
                wb, n_wb = wbs[ci % 2]
                o, n_o = o32[ci % 2]
                P.dma(ws[:, :, :cn], wv[:, :, c0:c0 + cn], writes=[n_ws])
                if ci % 2 == 0:
                    P.op('act', (lambda e, ws=ws, wb=wb, cn=cn: e.activation(out=wb[:, :, :cn], in_=ws[:, :, :cn], func=AF.Copy)),
                         reads=[n_ws], writes=[n_wb])
                else:
                    P.op('pool', (lambda e, ws=ws, wb=wb, cn=cn: e.tensor_copy(out=wb[:, :, :cn], in_=ws[:, :, :cn])),
                         reads=[n_ws], writes=[n_wb])
                for (g0, gn) in tiles_of(nb * 128, 512):
                    pm, n_pm = pms[npm % 4]
                    npm += 1
                    for k in range(KD):
                        P.op('pe', (lambda e, pm=pm, wb=wb, k=k, cn=cn, g0=g0, gn=gn: e.matmul(
                            pm[:cn, :gn], lhsT=wb[:, k, :cn], rhs=hT[:, k, g0:g0 + gn], start=(k == 0), stop=(k == KD - 1))),
                             reads=[n_wb, n_hT], writes=[n_pm])
                    if npm % 2 == 0:
                        P.op('act', (lambda e, pm=pm, o=o, cn=cn, g0=g0, gn=gn: e.activation(
                            out=o[:cn, g0:g0 + gn], in_=pm[:cn, :gn], func=AF.Copy)), reads=[n_pm], writes=[n_o])
                    else:
                        P.op('dve', (lambda e, pm=pm, o=o, cn=cn, g0=g0, gn=gn: e.tensor_copy(
                            out=o[:cn, g0:g0 + gn], in_=pm[:cn, :gn])), reads=[n_pm], writes=[n_o])
                P.dma(projT[c0:c0 + cn, b0 * 128:(b0 + nb) * 128], o[:cn, :nb * 128], reads=[n_o], writes=['projT'])
            for gi in range(3):
                for q4 in range(4):
                    c0 = C_K + gi * 512 + q4 * 128
                    ws, n_ws = wst[q4 % 2]
                    P.dma(ws[:], wv[:, :, c0:c0 + 128], writes=[n_ws])
                    P.op('act' if q4 % 2 == 0 else 'pool',
                         (lambda e, ws=ws, q4=q4: e.activation(out=wkv[:, :, q4 * 128:(q4 + 1) * 128], in_=ws[:], func=AF.Copy)
                          if q4 % 2 == 0 else e.tensor_copy(out=wkv[:, :, q4 * 128:(q4 + 1) * 128], in_=ws[:])),
                         reads=[n_ws], writes=[n_wkv])
                for ti, i in enumerate(btiles):
                    pm, n_pm = pms[npm % 4]
                    npm += 1
                    ok, n_ok = ok32[npm % 2]
                    for k in range(KD):
                        P.op('pe', (lambda e, pm=pm, k=k, ti=ti: e.matmul(
                            pm[:, :], lhsT=hT[:, k, ti * 128:(ti + 1) * 128], rhs=wkv[:, k, :], start=(k == 0), stop=(k == KD - 1))),
                             reads=[n_wkv, n_hT], writes=[n_pm])
                    if npm % 2 == 0:
                        P.op('act', (lambda e, pm=pm, ok=ok: e.activation(out=ok[:], in_=pm[:], func=AF.Copy)),
                             reads=[n_pm], writes=[n_ok])
                    else:
                        P.op('dve', (lambda e, pm=pm, ok=ok: e.tensor_copy(out=ok[:], in_=pm[:])),
                             reads=[n_pm], writes=[n_ok])
                    P.dma(projK[i * 128:(i + 1) * 128, gi * 512:(gi + 1) * 512], ok[:], reads=[n_ok], writes=['projK'])


def stage_outproj(P, cfg, l, io, rep, yT, src_lat, src_ctx, dst_lat, dst_ctx):
    L, LC, D = cfg['L'], cfg['LC'], cfg['D']
    LT = L + LC
    NT = LT // 128
    nct = LC // 128
    KM = 2048 // 128
    with Stage(P, 'outp%d' % l) as st:
        wo, n_wo = st.sb([128, KM, D], BF16)
        wst = [st.sb([128, KM, 128], tag='wst') for _ in range(2)]
        ggs = [st.sb([128, D], tag='gg') for _ in range(2)]
        P.dma(ggs[0][0][:], rep[2], reads=['rep2'], writes=[ggs[0][1]])
        P.dma(ggs[1][0][:], rep[5], reads=['rep5'], writes=[ggs[1][1]])
        wv = io['w_out'][l].rearrange("(k p) n -> p k n", p=128)
        for ci, (c0, cn) in enumerate(tiles_of(D, 128)):
            ws, n_ws = wst[ci % 2]
            P.dma(ws[:, :, :cn], wv[:, :, c0:c0 + cn], writes=[n_ws])
            if ci % 2 == 0:
                P.op('act', (lambda e, ws=ws, c0=c0, cn=cn: e.activation(out=wo[:, :, c0:c0 + cn], in_=ws[:, :, :cn], func=AF.Copy)),
                     reads=[n_ws], writes=[n_wo])
            else:
                P.op('pool', (lambda e, ws=ws, c0=c0, cn=cn: e.tensor_copy(out=wo[:, :, c0:c0 + cn], in_=ws[:, :, :cn])),
                     reads=[n_ws], writes=[n_wo])
        yts = [st.sb([128, KM, 128], BF16, tag='y') for _ in range(2)]
        xts = [st.sb([128, D], tag='x') for _ in range(2)]
        rs = [st.sb([128, D], tag='r') for _ in range(2)]
        junk, n_junk = st.sb([128, 512])
        sss = [st.sb([128, 8], tag='ss') for _ in range(2)]
        ngrp = len(tiles_of(D, 512))
        pms = [st.ps([128, 512], tag='pm') for _ in range(8)]
        yv = yT.rearrange("(k p) t -> p k t", p=128)
        if l == cfg['DEPTH'] - 1:
            tile_list = list(range(nct, NT))
        else:
            tile_list = list(range(NT))
        for n, i in enumerate(tile_list):
            yt, n_y = yts[n % 2]
            xt, n_x = xts[n % 2]
            r, n_r = rs[n % 2]
            ss, n_ss = sss[n % 2]
            P.dma(yt[:], yv[:, :, i * 128:(i + 1) * 128], reads=['yT'], writes=[n_y])
            rows = pos_row_dmas(cfg, l, i)
            for (pp, npp, kind, sel) in rows:
                src = src_ctx if kind == 'c' else src_lat
                P.dma(xt[pp:pp + npp, :], row_ap(src, sel), reads=['res_' + kind], writes=[n_x])
            GG, n_GG = ggs[0] if i >= nct else ggs[1]
            P.op('dve', (lambda e, ss=ss: e.memset(ss[:], 0.0)), writes=[n_ss])
            for j, (n0, nn) in enumerate(tiles_of(D, 512)):
                pm, n_pm = pms[(n % 2) * 4 + j % 4]
                for k in range(KM):
                    P.op('pe', (lambda e, pm=pm, yt=yt, k=k, n0=n0, nn=nn: e.matmul(
                        pm[:, :nn], lhsT=yt[:, k, :], rhs=wo[:, k, n0:n0 + nn], start=(k == 0), stop=(k == KM - 1))),
                         reads=[n_y, n_wo], writes=[n_pm])
                P.op('act', (lambda e, pm=pm, ss=ss, j=j, nn=nn: e.activation(
                    out=junk[:, :nn], in_=pm[:, :nn], func=AF.Square, accum_out=ss[:, j:j + 1])),
                     reads=[n_pm], writes=[n_junk, n_ss])
            P.op('dve', (lambda e, ss=ss: e.tensor_reduce(out=ss[:, 4:5], in_=ss[:, 0:4], axis=AX.X, op=ALU.add)),
                 reads=[n_ss], writes=[n_ss])
            P.op('dve', (lambda e, ss=ss: e.tensor_scalar(out=ss[:, 5:6], in0=ss[:, 4:5], scalar1=1.0 / D,
                                                           scalar2=EPS, op0=ALU.mult, op1=ALU.add)),
                 reads=[n_ss], writes=[n_ss])
            P.op('act', (lambda e, ss=ss: e.activation(out=ss[:, 5:6], in_=ss[:, 5:6], func=AF.Sqrt)),
                 reads=[n_ss], writes=[n_ss])
            P.op('dve', (lambda e, ss=ss: e.reciprocal(out=ss[:, 5:6], in_=ss[:, 5:6])),
                 reads=[n_ss], writes=[n_ss])
            for j, (n0, nn) in enumerate(tiles_of(D, 512)):
                pm, n_pm = pms[(n % 2) * 4 + j % 4]
                P.op('dve', (lambda e, pm=pm, ss=ss, r=r, GG=GG, n0=n0, nn=nn: e.scalar_tensor_tensor(
                    out=r[:, n0:n0 + nn], in0=pm[:, :nn], scalar=ss[:, 5:6], in1=GG[:, n0:n0 + nn],
                    op0=ALU.mult, op1=ALU.mult)), reads=[n_pm, n_ss, n_GG], writes=[n_r])
            P.op('pool', (lambda e, r=r, xt=xt: e.tensor_tensor(out=r[:], in0=r[:], in1=xt[:], op=ALU.add)),
                 reads=[n_r, n_x], writes=[n_r])
            for (pp, npp, kind, sel) in rows:
                dst = dst_ctx if kind == 'c' else dst_lat
                P.dma(row_ap(dst, sel), r[pp:pp + npp, :], reads=[n_r], writes=['dst_' + kind])


def stage_stub_mixer(P, cfg, l, projT, yT):
    LT = cfg['L'] + cfg['LC']
    with Stage(P, 'stub%d' % l) as st:
        a = [st.sb([128, LT], tag='a') for _ in range(2)]
        b = [st.sb([128, LT], BF16, tag='b') for _ in range(2)]
        for k in range(16):
            t, n_t = a[k % 2]
            u, n_u = b[k % 2]
            P.dma(t[:], projT[k * 128:(k + 1) * 128, :], reads=['projT'], writes=[n_t])
            P.op('dve', (lambda e, t=t, u=u: e.tensor_copy(out=u[:], in_=t[:])), reads=[n_t], writes=[n_u])
            P.dma(yT[k * 128:(k + 1) * 128, :], u[:], reads=[n_u], writes=['yT'])


def pcols(X, d, j0, n, seg):
    s0, s1 = seg
    if d == 0:
        return X[:, j0:j0 + n]
    lo = s0 + s1 - j0 - n
    return X[:, lo:lo + n][:, ::-1]


def stage_gla(P, cfg, l, io, projT, projK, yT):
    L, LC = cfg['L'], cfg['LC']
    LT = L + LC
    NT = LT // 128
    nct = LC // 128
    segs = [(0, LC), (LC, LT)]
    DKS = 128.0 ** -0.5
    with Stage(P, 'gla%d' % l) as st:
        ident, n_id = st.sb([128, 128], BF16)
        maskf, n_mf = st.sb([128, 128], BF16)
        maskb, n_mb = st.sb([128, 128], BF16)
        ones, n_ones = st.sb([128, 128])
        wg, n_wg = st.sb([16, 2, 512])
        bT, n_bT = st.sb([128, 8])
        nbT, n_nbT = st.sb([128, 8])
        gnT, n_gn = st.sb([128, 2])
        P.dma(ident[:], io['ident'], writes=[n_id])
        P.dma(maskf[:], io['maskf'], writes=[n_mf])
        P.dma(maskb[:], io['maskb'], writes=[n_mb])
        P.dma(wg[:], io['gla_wg'][l], writes=[n_wg])
        P.dma(bT[:], io['gla_bT'][l], writes=[n_bT])
        P.dma(gnT[:], io['gla_nT'][l], writes=[n_gn])
        P.op('dve', lambda e: e.memset(ones[:], 1.0), writes=[n_ones])
        P.op('dve', lambda e: e.tensor_scalar(out=nbT[:], in0=bT[:], scalar1=-1.0, scalar2=None, op0=ALU.mult),
             reads=[n_bT], writes=[n_nbT])
        qT, n_q = st.sb([128, LT])
        kT, n_k = st.sb([128, LT])
        lr, n_lr = st.sb([16, LT])
        LP, n_LP = st.sb([128, LT])
        Gc, n_Gc = st.sb([128, LT])
        EG, n_EG = st.sb([128, LT])
        qg, n_qg = st.sb([128, LT], BF16)
        kg, n_kg = st.sb([128, LT], BF16)
        kgr, n_kgr = st.sb([128, LT], BF16)
        vtok, n_v = st.sb([128, NT, 256], BF16)
        O, n_O = st.sb([128, 2, LT])
        vst = [st.sb([128, 256], tag='vst') for _ in range(2)]
        S, n_S = st.sb([128, 256])
        Sb, n_Sb = st.sb([128, 256], BF16)
        attm = [st.sb([128, 128], BF16, tag='attm') for _ in range(2)]
        kdT = [st.sb([128, 128], BF16, tag='kdT') for _ in range(2)]
        kd = [st.sb([128, 128], BF16, tag='kd') for _ in range(2)]
        qk, n_qk = st.sb([128, 256])
        vt, n_vt = st.sb([128, 2, 256])
        gt, n_gt = st.sb([128, 2, 256])
        tmp, n_tmp = st.sb([128, 2, 256])
        rs, n_rs = st.sb([128, 256])
        yb = [st.sb([128, 2, 256], BF16, tag='yb') for _ in range(2)]
        pz = [st.ps([128, 512], tag='pz') for _ in range(2)]
        patt, n_patt = st.ps([128, 128], tag='patt')
        po = [st.ps([128, 2, 128], tag='po') for _ in range(2)]
        ptr, n_ptr = st.ps([128, 128], BF16, tag='ptr')
        pds, n_pds = st.ps([128, 256], tag='pds')
        pq, n_pq = st.ps([128, 512], tag='pq')
        gcount = 0
        for h in range(4):
            P.dma(qT[:], projT[C_Q + h * 128:C_Q + (h + 1) * 128, :], reads=['projT'], writes=[n_q])
            P.dma(kT[:], projT[C_K + h * 128:C_K + (h + 1) * 128, :], reads=['projT'], writes=[n_k])
            for pcn in range(NT):
                vs, n_vs = vst[pcn % 2]
                P.dma(vs[:], projK[pcn * 128:(pcn + 1) * 128, 512 + h * 256:512 + (h + 1) * 256], reads=['projK'], writes=[n_vs])
                if pcn % 2 == 0:
                    P.op('act', (lambda e, vs=vs, pcn=pcn: e.activation(out=vtok[:, pcn, :], in_=vs[:], func=AF.Copy)),
                         reads=[n_vs], writes=[n_v])
                else:
                    P.op('pool', (lambda e, vs=vs, pcn=pcn: e.tensor_copy(out=vtok[:, pcn, :], in_=vs[:])),
                         reads=[n_vs], writes=[n_v])
            for d in range(2):
                P.dma(lr[:], projT[C_LR + d * 16:C_LR + (d + 1) * 16, :], reads=['projT'], writes=[n_lr])
                col = d * 4 + h
                for seg in segs:
                    for (j0, n) in [(seg[0] + a, b) for (a, b) in tiles_of(seg[1] - seg[0], 512)]:
                        p, n_p = pz[gcount % 2]
                        gcount += 1
                        P.op('pe', (lambda e, p=p, d=d, h=h, j0=j0, n=n, seg=seg: e.matmul(
                            p[:, :n], lhsT=wg[:, d, h * 128:(h + 1) * 128], rhs=pcols(lr, d, j0, n, seg), start=True, stop=True)),
                             reads=[n_wg, n_lr], writes=[n_p])
                        P.op('act', (lambda e, p=p, j0=j0, n=n, col=col: e.activation(
                            out=LP[:, j0:j0 + n], in_=p[:, :n], func=AF.Exp, scale=-1.0, bias=nbT[:, col:col + 1])),
                             reads=[n_p, n_nbT], writes=[n_LP])
                P.op('act', lambda e: e.activation(out=LP[:], in_=LP[:], func=AF.Ln, bias=1.0, scale=1.0),
                     reads=[n_LP], writes=[n_LP])
                for n in range(NT):
                    P.op('dve', (lambda e, n=n: e.tensor_tensor_scan(
                        out=Gc[:, n * 128:(n + 1) * 128], data0=ones[:], data1=LP[:, n * 128:(n + 1) * 128],
                        initial=0.0, op0=ALU.mult, op1=ALU.add)), reads=[n_LP, n_ones], writes=[n_Gc])
                P.op('act', lambda e: e.activation(out=EG[:], in_=Gc[:], func=AF.Exp, scale=-1.0 / 16.0),
                     reads=[n_Gc], writes=[n_EG])
                P.op('act', lambda e: e.activation(out=Gc[:], in_=Gc[:], func=AF.Exp, scale=1.0 / 16.0),
                     reads=[n_Gc], writes=[n_Gc])
                for seg in segs:
                    s0, s1 = seg
                    P.op('dve', (lambda e, d=d, seg=seg, s0=s0, s1=s1: e.scalar_tensor_tensor(
                        out=qg[:, s0:s1], in0=pcols(qT, d, s0, s1 - s0, seg), scalar=DKS, in1=EG[:, s0:s1],
                        op0=ALU.mult, op1=ALU.mult)), reads=[n_q, n_EG], writes=[n_qg])
                    P.op('pool', (lambda e, d=d, seg=seg, s0=s0, s1=s1: e.tensor_tensor(
                        out=kg[:, s0:s1], in0=pcols(kT, d, s0, s1 - s0, seg), in1=Gc[:, s0:s1], op=ALU.mult)),
                         reads=[n_k, n_Gc], writes=[n_kg])
                if d == 1:
                    P.op('pool', lambda e: e.tensor_copy(
                        out=kgr[:].rearrange("p (n i) -> p n i", i=128),
                        in_=kg[:].rearrange("p (n i) -> p n i", i=128)[:, :, ::-1]), reads=[n_kg], writes=[n_kgr])
                kgP, n_kgP = (kg, n_kg) if d == 0 else (kgr, n_kgr)
                P.op('dve', lambda e: e.memset(S[:], 0.0), writes=[n_S])
                P.op('dve', lambda e: e.memset(Sb[:], 0.0), writes=[n_Sb])
                mask, n_mask = (maskf, n_mf) if d == 0 else (maskb, n_mb)
                for n in range(NT):
                    if d == 0:
                        pcn = n
                    else:
                        pcn = (nct - 1 - n) if n < nct else (NT - 1 - n + nct)
                    c0 = n * 128
                    am, n_am = attm[n % 2]
                    kt, n_kt = kdT[n % 2]
                    kdd, n_kd = kd[n % 2]
                    pp, n_po = po[n % 2]
                    P.op('pe', (lambda e, c0=c0, kgP=kgP: e.matmul(patt[:], lhsT=kgP[:, c0:c0 + 128], rhs=qg[:, c0:c0 + 128],
                                                                    start=True, stop=True)),
                         reads=[n_kgP, n_qg], writes=[n_patt])
                    P.op('dve', (lambda e, am=am, mask=mask: e.tensor_tensor(out=am[:], in0=patt[:], in1=mask[:], op=ALU.mult)),
                         reads=[n_patt, n_mask], writes=[n_am])
                    for half in range(2):
                        P.op('pe', (lambda e, pp=pp, half=half, pcn=pcn, am=am: e.matmul(
                            pp[:, half, :], lhsT=vtok[:, pcn, half * 128:(half + 1) * 128], rhs=am[:], start=True, stop=False)),
                             reads=[n_v, n_am], writes=[n_po])
                        P.op('pe', (lambda e, pp=pp, half=half, c0=c0: e.matmul(
                            pp[:, half, :], lhsT=Sb[:, half * 128:(half + 1) * 128], rhs=qg[:, c0:c0 + 128], start=False, stop=True)),
                             reads=[n_Sb, n_qg], writes=[n_po])
                    pb = pcn * 128
                    if d == 0:
                        P.op('act', (lambda e, pp=pp, pb=pb: e.activation(out=O[:, :, pb:pb + 128], in_=pp[:, :, :], func=AF.Copy)),
                             reads=[n_po], writes=[n_O])
                    else:
                        for half in range(2):
                            P.op('dve', (lambda e, pp=pp, pb=pb, half=half: e.tensor_tensor(
                                out=O[:, half, pb:pb + 128][:, ::-1], in0=O[:, half, pb:pb + 128][:, ::-1],
                                in1=pp[:, half, :], op=ALU.add)), reads=[n_po, n_O], writes=[n_O])
                    if n == NT - 1:
                        continue
                    a_n = EG[:, c0 + 127:c0 + 128]
                    P.op('dve', (lambda e, kt=kt, c0=c0, a_n=a_n, kgP=kgP: e.tensor_scalar(
                        out=kt[:], in0=kgP[:, c0:c0 + 128], scalar1=a_n, scalar2=None, op0=ALU.mult)),
                         reads=[n_kgP, n_EG], writes=[n_kt])
                    P.op('pe', (lambda e, kt=kt: e.transpose(out=ptr[:], in_=kt[:], identity=ident[:])),
                         reads=[n_kt, n_id], writes=[n_ptr])
                    P.op('act', (lambda e, kdd=kdd: e.activation(out=kdd[:], in_=ptr[:], func=AF.Copy)),
                         reads=[n_ptr], writes=[n_kd])
                    P.op('pe', (lambda e, kdd=kdd, pcn=pcn: e.matmul(pds[:], lhsT=kdd[:], rhs=vtok[:, pcn, :], start=True, stop=True)),
                         reads=[n_kd, n_v], writes=[n_pds])
                    P.op('dve', (lambda e, a_n=a_n: e.scalar_tensor_tensor(
                        out=S[:], in0=S[:], scalar=a_n, in1=pds[:], op0=ALU.mult, op1=ALU.add)),
                         reads=[n_pds, n_EG, n_S], writes=[n_S])
                    P.op('act', lambda e: e.activation(out=Sb[:], in_=S[:], func=AF.Copy), reads=[n_S], writes=[n_Sb])
            for gi, (g0, gn) in enumerate(tiles_of(LT, 256)):
                y, n_y = yb[gi % 2]
                P.op('dve', (lambda e, g0=g0, gn=gn: e.scalar_tensor_tensor(
                    out=qk[:, :gn], in0=qT[:, g0:g0 + gn], scalar=DKS, in1=kT[:, g0:g0 + gn], op0=ALU.mult, op1=ALU.mult)),
                     reads=[n_q, n_k], writes=[n_qk])
                P.op('pe', (lambda e, gn=gn: e.matmul(pq[:, :gn], lhsT=ones[:], rhs=qk[:, :gn], start=True, stop=True)),
                     reads=[n_ones, n_qk], writes=[n_pq])
                for half in range(2):
                    r0 = C_V + h * 256 + half * 128
                    P.dma(vt[:, half, :gn], projT[r0:r0 + 128, g0:g0 + gn], reads=['projT'], writes=[n_vt])
                    r1 = C_GG + h * 256 + half * 128
                    P.dma(gt[:, half, :gn], projT[r1:r1 + 128, g0:g0 + gn], reads=['projT'], writes=[n_gt])
                for half in range(2):
                    P.op('dve', (lambda e, half=half, gn=gn: e.tensor_tensor(
                        out=tmp[:, half, :gn], in0=pq[:, :gn], in1=vt[:, half, :gn], op=ALU.mult)),
                         reads=[n_pq, n_vt], writes=[n_tmp])
                    P.op('pool', (lambda e, half=half, g0=g0, gn=gn: e.tensor_tensor(
                        out=O[:, half, g0:g0 + gn], in0=O[:, half, g0:g0 + gn], in1=tmp[:, half, :gn], op=ALU.subtract)),
                         reads=[n_tmp, n_O], writes=[n_O])
                P.op('act', (lambda e, g0=g0, gn=gn: e.activation(out=tmp[:, :, :gn], in_=O[:, :, g0:g0 + gn], func=AF.Square)),
                     reads=[n_O], writes=[n_tmp])
                for half in range(2):
                    P.op('pe', (lambda e, half=half, gn=gn: e.matmul(pq[:, :gn], lhsT=ones[:], rhs=tmp[:, half, :gn],
                                                                      start=(half == 0), stop=(half == 1))),
                         reads=[n_ones, n_tmp], writes=[n_pq])
                P.op('dve', (lambda e, gn=gn: e.tensor_scalar(out=rs[:, :gn], in0=pq[:, :gn], scalar1=1.0 / 256.0, scalar2=EPS,
                                                              op0=ALU.mult, op1=ALU.add)), reads=[n_pq], writes=[n_rs])
                P.op('act', (lambda e, gn=gn: e.activation(out=rs[:, :gn], in_=rs[:, :gn], func=AF.Sqrt)),
                     reads=[n_rs], writes=[n_rs])
                P.op('dve', (lambda e, gn=gn: e.reciprocal(out=rs[:, :gn], in_=rs[:, :gn])), reads=[n_rs], writes=[n_rs])
                P.op('act', (lambda e, gn=gn: e.activation(out=gt[:, :, :gn], in_=gt[:, :, :gn], func=AF.Silu)),
                     reads=[n_gt], writes=[n_gt])
                for half in range(2):
                    P.op('dve', (lambda e, half=half, g0=g0, gn=gn: e.scalar_tensor_tensor(
                        out=tmp[:, half, :gn], in0=O[:, half, g0:g0 + gn], scalar=gnT[:, half:half + 1], in1=rs[:, :gn],
                        op0=ALU.mult, op1=ALU.mult)), reads=[n_O, n_gn, n_rs], writes=[n_tmp])
                    P.op('pool', (lambda e, half=half, gn=gn, y=y: e.tensor_tensor(
                        out=y[:, half, :gn], in0=tmp[:, half, :gn], in1=gt[:, half, :gn], op=ALU.mult)),
                         reads=[n_tmp, n_gt], writes=[n_y])
                    r2 = h * 256 + half * 128
                    P.dma(yT[r2:r2 + 128, g0:g0 + gn], y[:, half, :gn], reads=[n_y], writes=['yT'])


MAGIC = 12582912.0


def stage_s5(P, cfg, l, io, projT, ygT, yT):
    L, LC = cfg['L'], cfg['LC']
    LT = L + LC
    segs = [(0, LC), (LC, LT)]
    BW = 1024
    blocks = []
    for seg in segs:
        for (a, n) in tiles_of(seg[1] - seg[0], BW):
            blocks.append((seg[0] + a, n, seg))
    with Stage(P, 's5a%d' % l) as st:
        ident, n_id = st.sb([128, 128])
        P.dma(ident[:], io['ident32'], writes=[n_id])
        sc, n_sc = st.sb([128, 2, 16, 3])
        P.dma(sc[:], io['s5_sc'][l], writes=[n_sc])
        prm = {}
        for nm in ('Dl', 'AR', 'TH', 'Rr', 'k1', 'red', 'sn', 'ab', 'cs', 'LBr', 'LBi', 'den', 'kr', 'ki', 'nki', 't'):
            prm[nm] = st.sb([128, 2, 16], tag=nm)
        LRv, LIv, LSv = sc[:, :, :, 0], sc[:, :, :, 1], sc[:, :, :, 2]

        def pw(eng, fn, r, w):
            P.op(eng, fn, reads=[prm[x][1] if x in prm else x for x in r], writes=[prm[x][1] for x in w])
        T = lambda nm: prm[nm][0]
        pw('act', lambda e: e.activation(out=T('Dl')[:], in_=LSv, func=AF.Exp), [n_sc], ['Dl'])
        pw('dve', lambda e: e.tensor_tensor(out=T('AR')[:], in0=LRv, in1=T('Dl')[:], op=ALU.mult), [n_sc, 'Dl'], ['AR'])
        pw('dve', lambda e: e.tensor_tensor(out=T('TH')[:], in0=LIv, in1=T('Dl')[:], op=ALU.mult), [n_sc, 'Dl'], ['TH'])
        pw('act', lambda e: e.activation(out=T('Rr')[:], in_=T('AR')[:], func=AF.Exp), ['AR'], ['Rr'])
        pw('dve', lambda e: e.tensor_scalar(out=T('k1')[:], in0=T('TH')[:], scalar1=1.0 / (2 * PI), scalar2=MAGIC,
                                            op0=ALU.mult, op1=ALU.add), ['TH'], ['k1'])
        pw('dve', lambda e: e.tensor_scalar(out=T('k1')[:], in0=T('k1')[:], scalar1=MAGIC, scalar2=-2 * PI,
                                            op0=ALU.subtract, op1=ALU.mult), ['k1'], ['k1'])
        pw('dve', lambda e: e.tensor_tensor(out=T('red')[:], in0=T('TH')[:], in1=T('k1')[:], op=ALU.add), ['TH', 'k1'], ['red'])
        pw('act', lambda e: e.activation(out=T('sn')[:], in_=T('red')[:], func=AF.Sin), ['red'], ['sn'])
        pw('act', lambda e: e.activation(out=T('ab')[:], in_=T('red')[:], func=AF.Abs), ['red'], ['ab'])
        pw('act', lambda e: e.activation(out=T('cs')[:], in_=T('ab')[:], func=AF.Sin, scale=-1.0, bias=PI / 2), ['ab'], ['cs'])
        pw('dve', lambda e: e.tensor_tensor(out=T('LBr')[:], in0=T('Rr')[:], in1=T('cs')[:], op=ALU.mult), ['Rr', 'cs'], ['LBr'])
        pw('dve', lambda e: e.tensor_tensor(out=T('LBi')[:], in0=T('Rr')[:], in1=T('sn')[:], op=ALU.mult), ['Rr', 'sn'], ['LBi'])
        pw('dve', lambda e: e.tensor_scalar(out=T('LBr')[:], in0=T('LBr')[:], scalar1=-1.0, scalar2=None, op0=ALU.add), ['LBr'], ['LBr'])
        pw('dve', lambda e: e.tensor_tensor(out=T('den')[:], in0=LRv, in1=LRv, op=ALU.mult), [n_sc], ['den'])
        pw('dve', lambda e: e.tensor_tensor(out=T('t')[:], in0=LIv, in1=LIv, op=ALU.mult), [n_sc], ['t'])
        pw('dve', lambda e: e.tensor_tensor(out=T('den')[:], in0=T('den')[:], in1=T('t')[:], op=ALU.add), ['den', 't'], ['den'])
        pw('dve', lambda e: e.reciprocal(out=T('den')[:], in_=T('den')[:]), ['den'], ['den'])
        pw('dve', lambda e: e.tensor_tensor(out=T('kr')[:], in0=T('LBr')[:], in1=LRv, op=ALU.mult), ['LBr', n_sc], ['kr'])
        pw('dve', lambda e: e.tensor_tensor(out=T('t')[:], in0=T('LBi')[:], in1=LIv, op=ALU.mult), ['LBi', n_sc], ['t'])
        pw('dve', lambda e: e.tensor_tensor(out=T('kr')[:], in0=T('kr')[:], in1=T('t')[:], op=ALU.add), ['kr', 't'], ['kr'])
        pw('dve', lambda e: e.tensor_tensor(out=T('kr')[:], in0=T('kr')[:], in1=T('den')[:], op=ALU.mult), ['kr', 'den'], ['kr'])
        pw('dve', lambda e: e.tensor_tensor(out=T('ki')[:], in0=T('LBi')[:], in1=LRv, op=ALU.mult), ['LBi', n_sc], ['ki'])
        pw('dve', lambda e: e.tensor_tensor(out=T('t')[:], in0=T('LBr')[:], in1=LIv, op=ALU.mult), ['LBr', n_sc], ['t'])
        pw('dve', lambda e: e.tensor_tensor(out=T('ki')[:], in0=T('ki')[:], in1=T('t')[:], op=ALU.subtract), ['ki', 't'], ['ki'])
        pw('dve', lambda e: e.tensor_tensor(out=T('ki')[:], in0=T('ki')[:], in1=T('den')[:], op=ALU.mult), ['ki', 'den'], ['ki'])
        pw('dve', lambda e: e.tensor_scalar(out=T('nki')[:], in0=T('ki')[:], scalar1=-1.0, scalar2=None, op0=ALU.mult), ['ki'], ['nki'])
        cpad, n_cpad = st.sb([128, 2, 16, 2, 128])
        P.dma(cpad[:], io['s5_cpad'][l], writes=[n_cpad])
        P.op('pool', lambda e: e.tensor_scalar(out=cpad[:, :, :, 1, :], in0=cpad[:, :, :, 1, :], scalar1=-1.0, scalar2=None,
                                               op0=ALU.mult), reads=[n_cpad], writes=[n_cpad])
        bpad, n_bpad = st.sb([128, 2, 16, 2, 128])
        bsts = [st.sb([128, 2, 128], tag='bst') for _ in range(2)]
        bb = [st.sb([128, 2, 128], tag='bb') for _ in range(2)]
        tt = [st.sb([128, 2, 128], tag='tt') for _ in range(2)]
        ptr = [st.ps([128, 2, 128], tag='ptr') for _ in range(2)]
        i = 0
        for d in range(2):
            for s_ in range(16):
                bs, n_bs = bsts[i % 2]
                b2, n_b2 = bb[i % 2]
                t2, n_t2 = tt[i % 2]
                pt, n_pt = ptr[i % 2]
                i += 1
                P.dma(bs[:], io['s5_bpad'][l][:, d, s_, :, :], writes=[n_bs])
                kr = T('kr')[:, d, s_:s_ + 1]
                ki = T('ki')[:, d, s_:s_ + 1]
                nki = T('nki')[:, d, s_:s_ + 1]
                rk = [prm['kr'][1], prm['ki'][1], prm['nki'][1]]
                P.op('dve', (lambda e, bs=bs, t2=t2, kr=kr: e.tensor_scalar(out=t2[:], in0=bs[:], scalar1=kr, scalar2=None, op0=ALU.mult)),
                     reads=[n_bs] + rk, writes=[n_t2])
                P.op('dve', (lambda e, bs=bs, t2=t2, b2=b2, nki=nki: e.scalar_tensor_tensor(
                    out=b2[:, 0, :], in0=bs[:, 1, :], scalar=nki, in1=t2[:, 0, :], op0=ALU.mult, op1=ALU.add)),
                     reads=[n_bs, n_t2] + rk, writes=[n_b2])
                P.op('dve', (lambda e, bs=bs, t2=t2, b2=b2, ki=ki: e.scalar_tensor_tensor(
                    out=b2[:, 1, :], in0=bs[:, 0, :], scalar=ki, in1=t2[:, 1, :], op0=ALU.mult, op1=ALU.add)),
                     reads=[n_bs, n_t2] + rk, writes=[n_b2])
                for c in range(2):
                    P.op('pe', (lambda e, pt=pt, b2=b2, c=c: e.transpose(out=pt[:, c, :], in_=b2[:, c, :], identity=ident[:])),
                         reads=[n_b2, n_id], writes=[n_pt])
                P.op('act', (lambda e, pt=pt, d=d, s_=s_: e.activation(out=bpad[:, d, s_, :, :], in_=pt[:], func=AF.Copy)),
                     reads=[n_pt], writes=[n_bpad])
        dT, n_dT = st.sb([128, 4])
        P.dma(dT[:], io['s5_dT'][l], writes=[n_dT])
        iot, n_iot = st.sb([128, BW])
        P.dma(iot[:], io['iota'][:, :BW], writes=[n_iot])
        ones, n_ones = st.sb([128, BW])
        P.op('pool', lambda e: e.memset(ones[:], 1.0), writes=[n_ones])
        uT, n_u = st.sb([128, LT])
        Y, n_Y = st.sb([128, LT])
        rt, n_rt = st.sb([128, BW])
        carry, n_carry = st.sb([128, 2])
        names = ('bre', 'bim', 'ang', 'k1', 'sn', 'cs', 'ta', 'tb', 'btr', 'bti', 'wre', 'wim', 'xre', 'xim')
        B = {nm: st.sb([128, BW], tag=nm) for nm in names}
        pb = [st.ps([128, 512], tag='pb') for _ in range(4)]
        py = [st.ps([128, 512], tag='py') for _ in range(2)]
        npb = 0
        npy = 0

        def bop(eng, fn, r, w):
            P.op(eng, fn, reads=[B[x][1] if x in B else x for x in r], writes=[B[x][1] if x in B else x for x in w])
        X = lambda nm: B[nm][0]
        for ct in range(4):
            P.dma(uT[:], projT[C_U + ct * 128:C_U + (ct + 1) * 128, :], reads=['projT'], writes=[n_u])
            P.op('dve', (lambda e, ct=ct: e.tensor_scalar(out=Y[:], in0=uT[:], scalar1=dT[:, ct:ct + 1], scalar2=None, op0=ALU.mult)),
                 reads=[n_u, n_dT], writes=[n_Y])
            for s_ in range(4 * ct, 4 * ct + 4):
                for d in range(2):
                    rcol = T('Rr')[:, d, s_:s_ + 1]
                    thcol = T('red')[:, d, s_:s_ + 1]
                    P.op('pool', (lambda e, rcol=rcol: e.tensor_scalar(out=rt[:], in0=ones[:], scalar1=rcol, scalar2=None, op0=ALU.mult)),
                         reads=[n_ones, prm['Rr'][1]], writes=[n_rt])
                    P.op('dve', lambda e: e.memset(carry[:], 0.0), writes=[n_carry])
                    for (j0, n, seg) in blocks:
                        for (a, m) in tiles_of(n, 512):
                            for c, nm in ((0, 'bre'), (1, 'bim')):
                                p, n_p = pb[npb % 4]
                                npb += 1
                                P.op('pe', (lambda e, p=p, d=d, s_=s_, c=c, j0=j0, a=a, m=m, seg=seg: e.matmul(
                                    p[:, :m], lhsT=bpad[:, d, s_, c, :], rhs=pcols(uT, d, j0 + a, m, seg), start=True, stop=True)),
                                     reads=[n_bpad, n_u], writes=[n_p])
                                if c == 0:
                                    bop('act', (lambda e, p=p, a=a, m=m, nm=nm: e.activation(out=X(nm)[:, a:a + m], in_=p[:, :m], func=AF.Copy)),
                                        [n_p], [nm])
                                else:
                                    bop('dve', (lambda e, p=p, a=a, m=m, nm=nm: e.tensor_copy(out=X(nm)[:, a:a + m], in_=p[:, :m])),
                                        [n_p], [nm])
                        bop('dve', (lambda e, j0=j0, n=n, thcol=thcol: e.tensor_scalar(
                            out=X('ang')[:, :n], in0=iot[:, :n], scalar1=float(j0), scalar2=thcol, op0=ALU.add, op1=ALU.mult)),
                            [n_iot, prm['red'][1]], ['ang'])
                        bop('dve', (lambda e, n=n: e.tensor_scalar(out=X('k1')[:, :n], in0=X('ang')[:, :n], scalar1=1.0 / (2 * PI),
                                                                   scalar2=MAGIC, op0=ALU.mult, op1=ALU.add)), ['ang'], ['k1'])
                        bop('pool', (lambda e, n=n: e.tensor_scalar(out=X('k1')[:, :n], in0=X('k1')[:, :n], scalar1=MAGIC,
                                                                    scalar2=-2 * PI, op0=ALU.subtract, op1=ALU.mult)), ['k1'], ['k1'])
                        bop('pool', (lambda e, n=n: e.tensor_tensor(out=X('ang')[:, :n], in0=X('ang')[:, :n], in1=X('k1')[:, :n], op=ALU.add)),
                            ['ang', 'k1'], ['ang'])
                        bop('act', (lambda e, n=n: e.activation(out=X('sn')[:, :n], in_=X('ang')[:, :n], func=AF.Sin)), ['ang'], ['sn'])
                        bop('act', (lambda e, n=n: e.activation(out=X('k1')[:, :n], in_=X('ang')[:, :n], func=AF.Abs)), ['ang'], ['k1'])
                        bop('act', (lambda e, n=n: e.activation(out=X('cs')[:, :n], in_=X('k1')[:, :n], func=AF.Sin, scale=-1.0, bias=PI / 2)),
                            ['k1'], ['cs'])
                        bop('dve', (lambda e, n=n: e.tensor_tensor(out=X('ta')[:, :n], in0=X('cs')[:, :n], in1=X('bre')[:, :n], op=ALU.mult)),
                            ['cs', 'bre'], ['ta'])
                        bop('pool', (lambda e, n=n: e.tensor_tensor(out=X('tb')[:, :n], in0=X('sn')[:, :n], in1=X('bim')[:, :n], op=ALU.mult)),
                            ['sn', 'bim'], ['tb'])
                        bop('dve', (lambda e, n=n: e.tensor_tensor(out=X('btr')[:, :n], in0=X('ta')[:, :n], in1=X('tb')[:, :n], op=ALU.add)),
                            ['ta', 'tb'], ['btr'])
                        bop('pool', (lambda e, n=n: e.tensor_tensor(out=X('ta')[:, :n], in0=X('cs')[:, :n], in1=X('bim')[:, :n], op=ALU.mult)),
                            ['cs', 'bim'], ['ta'])
                        bop('dve', (lambda e, n=n: e.tensor_tensor(out=X('tb')[:, :n], in0=X('sn')[:, :n], in1=X('bre')[:, :n], op=ALU.mult)),
                            ['sn', 'bre'], ['tb'])
                        bop('pool', (lambda e, n=n: e.tensor_tensor(out=X('bti')[:, :n], in0=X('ta')[:, :n], in1=X('tb')[:, :n], op=ALU.subtract)),
                            ['ta', 'tb'], ['bti'])
                        bop('dve', (lambda e, n=n: e.tensor_tensor_scan(out=X('wre')[:, :n], data0=rt[:, :n], data1=X('btr')[:, :n],
                                                                        initial=carry[:, 0:1], op0=ALU.mult, op1=ALU.add)),
                            [n_rt, 'btr', n_carry], ['wre'])
                        bop('dve', (lambda e, n=n: e.tensor_tensor_scan(out=X('wim')[:, :n], data0=rt[:, :n], data1=X('bti')[:, :n],
                                                                        initial=carry[:, 1:2], op0=ALU.mult, op1=ALU.add)),
                            [n_rt, 'bti', n_carry], ['wim'])
                        bop('act', (lambda e, n=n: e.activation(out=carry[:, 0:1], in_=X('wre')[:, n - 1:n], func=AF.Copy)), ['wre'], [n_carry])
                        bop('act', (lambda e, n=n: e.activation(out=carry[:, 1:2], in_=X('wim')[:, n - 1:n], func=AF.Copy)), ['wim'], [n_carry])
                        bop('dve', (lambda e, n=n: e.tensor_tensor(out=X('ta')[:, :n], in0=X('cs')[:, :n], in1=X('wre')[:, :n], op=ALU.mult)),
                            ['cs', 'wre'], ['ta'])
                        bop('pool', (lambda e, n=n: e.tensor_tensor(out=X('tb')[:, :n], in0=X('sn')[:, :n], in1=X('wim')[:, :n], op=ALU.mult)),
                            ['sn', 'wim'], ['tb'])
                        bop('dve', (lambda e, n=n: e.tensor_tensor(out=X('xre')[:, :n], in0=X('ta')[:, :n], in1=X('tb')[:, :n], op=ALU.subtract)),
                            ['ta', 'tb'], ['xre'])
                        bop('pool', (lambda e, n=n: e.tensor_tensor(out=X('ta')[:, :n], in0=X('sn')[:, :n], in1=X('wre')[:, :n], op=ALU.mult)),
                            ['sn', 'wre'], ['ta'])
                        bop('dve', (lambda e, n=n: e.tensor_tensor(out=X('tb')[:, :n], in0=X('cs')[:, :n], in1=X('wim')[:, :n], op=ALU.mult)),
                            ['cs', 'wim'], ['tb'])
                        bop('pool', (lambda e, n=n: e.tensor_tensor(out=X('xim')[:, :n], in0=X('ta')[:, :n], in1=X('tb')[:, :n], op=ALU.add)),
                            ['ta', 'tb'], ['xim'])
                        s0, s1 = seg
                        p_lo = j0 if d == 0 else s0 + s1 - j0 - n
                        for (a, m) in tiles_of(n, 512):
                            p, n_p = py[npy % 2]
                            npy += 1
                            if d == 0:
                                cols = lambda Z, a=a, m=m: Z[:, a:a + m]
                            else:
                                cols = lambda Z, a=a, m=m, n=n: Z[:, n - a - m:n - a][:, ::-1]
                            P.op('pe', (lambda e, p=p, d=d, s_=s_, m=m, cols=cols: e.matmul(
                                p[:, :m], lhsT=cpad[:, d, s_, 0, :], rhs=cols(X('xre')), start=True, stop=False)),
                                 reads=[n_cpad, B['xre'][1]], writes=[n_p])
                            P.op('pe', (lambda e, p=p, d=d, s_=s_, m=m, cols=cols: e.matmul(
                                p[:, :m], lhsT=cpad[:, d, s_, 1, :], rhs=cols(X('xim')), start=False, stop=True)),
                                 reads=[n_cpad, B['xim'][1]], writes=[n_p])
                            P.op('dve', (lambda e, p=p, p_lo=p_lo, a=a, m=m: e.tensor_tensor(
                                out=Y[:, p_lo + a:p_lo + a + m], in0=Y[:, p_lo + a:p_lo + a + m], in1=p[:, :m], op=ALU.add)),
                                 reads=[n_p, n_Y], writes=[n_Y])
            P.op('act', lambda e: e.activation(out=Y[:], in_=Y[:], func=AF.Gelu_apprx_tanh), reads=[n_Y], writes=[n_Y])
            P.dma(ygT[ct * 128:(ct + 1) * 128, :], Y[:], reads=[n_Y], writes=['ygT'])
    with Stage(P, 's5b%d' % l) as st:
        wst, n_wst = st.sb([128, 4, 512])
        wgb, n_wgb = st.sb([128, 4, 512], BF16)
        bgT, n_bgT = st.sb([128, 4])
        P.dma(wst[:], io['s5_w_glu'][l].rearrange("(k p) n -> p k n", p=128), writes=[n_wst])
        P.dma(bgT[:], io['s5_bgT'][l], writes=[n_bgT])
        P.op('dve', lambda e: e.tensor_copy(out=wgb[:], in_=wst[:]), reads=[n_wst], writes=[n_wgb])
        ygs = [st.sb([128, 4, 512], tag='yg') for _ in range(2)]
        ygb = [st.sb([128, 4, 512], BF16, tag='ygb') for _ in range(2)]
        gts = [st.sb([128, 4, 512], tag='gt') for _ in range(2)]
        sig = [st.sb([128, 512], tag='sig') for _ in range(2)]
        ob = [st.sb([128, 4, 512], BF16, tag='ob') for _ in range(2)]
        pg = [st.ps([128, 512], tag='pg') for _ in range(4)]
        ygv = ygT.rearrange("(k p) t -> p k t", p=128)
        gv = projT[C_SG:C_SG + 512, :].rearrange("(k p) t -> p k t", p=128)
        ov = yT[1024:1536, :].rearrange("(k p) t -> p k t", p=128)
        for gi, (g0, gn) in enumerate(tiles_of(LT, 512)):
            yg, n_yg = ygs[gi % 2]
            yb, n_yb = ygb[gi % 2]
            gt, n_gt = gts[gi % 2]
            o, n_o = ob[gi % 2]
            P.dma(yg[:, :, :gn], ygv[:, :, g0:g0 + gn], reads=['ygT'], writes=[n_yg])
            P.dma(gt[:, :, :gn], gv[:, :, g0:g0 + gn], reads=['projT'], writes=[n_gt])
            P.op('pool', (lambda e, yg=yg, yb=yb, gn=gn: e.tensor_copy(out=yb[:, :, :gn], in_=yg[:, :, :gn])), reads=[n_yg], writes=[n_yb])
            P.op('act', (lambda e, gt=gt, gn=gn: e.activation(out=gt[:, :, :gn], in_=gt[:, :, :gn], func=AF.Silu)), reads=[n_gt], writes=[n_gt])
            for co in range(4):
                p, n_p = pg[co]
                sg_, n_sg = sig[co % 2]
                for k in range(4):
                    P.op('pe', (lambda e, p=p, yb=yb, k=k, co=co, gn=gn: e.matmul(
                        p[:, :gn], lhsT=wgb[:, k, co * 128:(co + 1) * 128], rhs=yb[:, k, :gn], start=(k == 0), stop=(k == 3))),
                         reads=[n_wgb, n_yb], writes=[n_p])
                P.op('act', (lambda e, p=p, sg_=sg_, co=co, gn=gn: e.activation(out=sg_[:, :gn], in_=p[:, :gn], func=AF.Sigmoid,
                                                                               bias=bgT[:, co:co + 1], scale=1.0)),
                     reads=[n_p, n_bgT], writes=[n_sg])
                P.op('dve', (lambda e, sg_=sg_, yg=yg, co=co, gn=gn: e.tensor_tensor(out=sg_[:, :gn], in0=sg_[:, :gn], in1=yg[:, co, :gn], op=ALU.mult)),
                     reads=[n_sg, n_yg], writes=[n_sg])
                P.op('pool', (lambda e, sg_=sg_, gt=gt, o=o, co=co, gn=gn: e.tensor_tensor(out=o[:, co, :gn], in0=sg_[:, :gn], in1=gt[:, co, :gn], op=ALU.mult)),
                     reads=[n_sg, n_gt], writes=[n_o])
            P.dma(ov[:, :, g0:g0 + gn], o[:, :, :gn], reads=[n_o], writes=['yT'])


def hyena_run(P, cfg, l, io, projT, yT, scr, Lh, pos0, sfx):
    NTl = Lh // 128
    NF = NTl + 1
    FP = NF * 128
    Cm, Sm = io['dftC_' + sfx], io['dftS_' + sfx]
    Cv = Cm.rearrange("(k p) f -> p k f", p=128)
    Sv = Sm.rearrange("(k p) f -> p k f", p=128)
    Sr_d, Si_d = scr['Sr'], scr['Si']
    hv_d, hx1_d, hx2_d = scr['hv'], scr['hx1'], scr['hx2T']
    with Stage(P, 'hy1%s%d' % (sfx, l)) as st:
        w1, n_w1 = st.sb([33, 64])
        w2, n_w2 = st.sb([64, 64])
        w3, n_w3 = st.sb([64, 2048])
        fb, n_fb = st.sb([64, 6])
        ft, n_ft = st.sb([33, Lh])
        drep, n_drep = st.sb([128, 1024])
        wgt, n_wgt = st.sb([128, NF])
        P.dma(w1[:], io['hy_w1'][l], writes=[n_w1])
        P.dma(w2[:], io['hy_w2'][l], writes=[n_w2])
        P.dma(w3[:], io['hy_w3'][l], writes=[n_w3])
        P.dma(fb[:, 0:4], io['hy_fb'][l], writes=[n_fb])
        P.dma(ft[:], io['featsT_' + sfx], writes=[n_ft])
        P.dma(drep[:], io['hy_d_rep'][l], writes=[n_drep])
        P.dma(wgt[:], io['wgt_' + sfx], writes=[n_wgt])
        P.op('dve', lambda e: e.tensor_tensor(out=fb[:, 4:5], in0=fb[:, 0:1], in1=fb[:, 1:2], op=ALU.mult), reads=[n_fb], writes=[n_fb])
        P.op('dve', lambda e: e.tensor_tensor(out=fb[:, 5:6], in0=fb[:, 2:3], in1=fb[:, 3:4], op=ALU.mult), reads=[n_fb], writes=[n_fb])
        h1, n_h1 = st.sb([64, Lh])
        h2, n_h2 = st.sb([64, Lh])
        a_, n_a = st.sb([64, 512])
        k_, n_k = st.sb([64, 512])
        pm = [st.ps([128, 512], tag='pm') for _ in range(2)]
        cnt = 0
        for (wm, n_wm, src, n_src, dst, n_dst, fc, bc) in ((w1, n_w1, ft, n_ft, h1, n_h1, 0, 4), (w2, n_w2, h1, n_h1, h2, n_h2, 2, 5)):
            for (g0, gn) in tiles_of(Lh, 512):
                p, n_p = pm[cnt % 2]
                cnt += 1
                P.op('pe', (lambda e, p=p, wm=wm, src=src, g0=g0, gn=gn: e.matmul(p[:64, :gn], lhsT=wm[:], rhs=src[:, g0:g0 + gn],
                                                                                   start=True, stop=True)),
                     reads=[n_wm, n_src], writes=[n_p])
                P.op('dve', (lambda e, p=p, gn=gn, fc=fc, bc=bc: e.tensor_scalar(out=a_[:, :gn], in0=p[:64, :gn], scalar1=fb[:, fc:fc + 1],
                                                                                 scalar2=fb[:, bc:bc + 1], op0=ALU.mult, op1=ALU.add)),
                     reads=[n_p, n_fb], writes=[n_a])
                P.op('dve', (lambda e, gn=gn: e.tensor_scalar(out=k_[:, :gn], in0=a_[:, :gn], scalar1=1.0 / (2 * PI), scalar2=MAGIC,
                                                              op0=ALU.mult, op1=ALU.add)), reads=[n_a], writes=[n_k])
                P.op('dve', (lambda e, gn=gn: e.tensor_scalar(out=k_[:, :gn], in0=k_[:, :gn], scalar1=MAGIC, scalar2=-2 * PI,
                                                              op0=ALU.subtract, op1=ALU.mult)), reads=[n_k], writes=[n_k])
                P.op('dve', (lambda e, gn=gn: e.tensor_tensor(out=a_[:, :gn], in0=a_[:, :gn], in1=k_[:, :gn], op=ALU.add)),
                     reads=[n_a, n_k], writes=[n_a])
                P.op('act', (lambda e, dst=dst, g0=g0, gn=gn: e.activation(out=dst[:, g0:g0 + gn], in_=a_[:, :gn], func=AF.Sin)),
                     reads=[n_a], writes=[n_dst])
        Eb, n_Eb = st.sb([128, NTl, 512], BF16)
        Ob, n_Ob = st.sb([128, NTl, 512], BF16)
        decs = [st.sb([128, 512], tag='dec') for _ in range(2)]
        hf, n_hf = st.sb([128, 512])
        hb, n_hb = st.sb([128, 512])
        cts = [st.sb([128, NTl, 128], BF16, tag='ct') for _ in range(2)]
        sts = [st.sb([128, NTl, 128], BF16, tag='st') for _ in range(2)]
        ps2 = [st.ps([128, 512], tag='ps2') for _ in range(4)]
        t_, n_t = st.sb([128, 512])
        so = [st.sb([128, 512], tag='so') for _ in range(2)]
        nso = 0
        nld = 0
        for o in range(2):
            for kt in range(NTl):
                dec, n_dec = decs[kt % 2]
                P.dma(dec[:], io['dec_' + sfx][kt * 128:(kt + 1) * 128, :], writes=[n_dec])
                for d in range(2):
                    p, n_p = pm[cnt % 2]
                    cnt += 1
                    c0 = o * 1024 + d * 512
                    P.op('pe', (lambda e, p=p, kt=kt, c0=c0: e.matmul(p[:, :], lhsT=h2[:, kt * 128:(kt + 1) * 128], rhs=w3[:, c0:c0 + 512],
                                                                      start=True, stop=True)), reads=[n_h2, n_w3], writes=[n_p])
                    dst, n_dst = (hf, n_hf) if d == 0 else (hb, n_hb)
                    P.op('dve', (lambda e, p=p, dst=dst, dec=dec: e.tensor_tensor(out=dst[:], in0=p[:], in1=dec[:], op=ALU.mult)),
                         reads=[n_p, n_dec], writes=[n_dst])
                if kt == 0:
                    P.op('dve', lambda e: e.memset(hb[0:1, :], 0.0), reads=[n_hb], writes=[n_hb])
                P.op('pool', (lambda e, kt=kt: e.tensor_tensor(out=Eb[:, kt, :], in0=hf[:], in1=hb[:], op=ALU.add)),
                     reads=[n_hf, n_hb], writes=[n_Eb])
                P.op('pool', (lambda e, kt=kt: e.tensor_tensor(out=Ob[:, kt, :], in0=hb[:], in1=hf[:], op=ALU.subtract)),
                     reads=[n_hf, n_hb], writes=[n_Ob])
            for kf in range(NF):
                ct, n_ct = cts[nld % 2]
                stt, n_st = sts[nld % 2]
                nld += 1
                P.dma_k(ct, Cv[:, 0:NTl, kf * 128:(kf + 1) * 128], NTl, writes=[n_ct])
                P.dma_k(stt, Sv[:, 0:NTl, kf * 128:(kf + 1) * 128], NTl, writes=[n_st])
                for c, (mt, n_mt, src, n_src) in enumerate(((ct, n_ct, Eb, n_Eb), (stt, n_st, Ob, n_Ob))):
                    p, n_p = ps2[(kf * 2 + c) % 4]
                    for kt in range(NTl):
                        P.op('pe', (lambda e, p=p, mt=mt, src=src, kt=kt: e.matmul(
                            p[:], lhsT=mt[:, kt, :], rhs=src[:, kt, :], start=(kt == 0), stop=(kt == NTl - 1))),
                             reads=[n_mt, n_src], writes=[n_p])
                    s_o, n_so = so[nso % 2]
                    nso += 1
                    if c == 0:
                        P.op('dve', (lambda e, p=p, o=o: e.tensor_tensor(out=t_[:], in0=p[:], in1=drep[:, o * 512:(o + 1) * 512], op=ALU.add)),
                             reads=[n_p, n_drep], writes=[n_t])
                        P.op('pool', (lambda e, s_o=s_o, kf=kf: e.tensor_scalar(out=s_o[:], in0=t_[:], scalar1=wgt[:, kf:kf + 1], scalar2=None,
                                                                                op0=ALU.mult)), reads=[n_t, n_wgt], writes=[n_so])
                        P.dma(Sr_d[kf * 128:(kf + 1) * 128, o * 512:(o + 1) * 512], s_o[:], reads=[n_so], writes=['Sr'])
                    else:
                        P.op('dve', (lambda e, p=p, s_o=s_o, kf=kf: e.tensor_scalar(out=s_o[:], in0=p[:], scalar1=wgt[:, kf:kf + 1], scalar2=None,
                                                                                    op0=ALU.mult)), reads=[n_p, n_wgt], writes=[n_so])
                        P.dma(Si_d[kf * 128:(kf + 1) * 128, o * 512:(o + 1) * 512], s_o[:], reads=[n_so], writes=['Si'])
    with Stage(P, 'hy2%s%d' % (sfx, l)) as st:
        ident, n_id = st.sb([128, 128])
        P.dma(ident[:], io['ident32'], writes=[n_id])
        cw, n_cw = st.sb([128, 12, 4])
        P.dma(cw[:], io['hy_cw'][l], writes=[n_cw])
        pts = [st.sb([128, Lh + 2], tag='pt') for _ in range(2)]
        ss = [st.sb([128, Lh], tag='s') for _ in range(2)]
        ptp = [st.ps([128, 4, 128], tag='ptp') for _ in range(2)]
        ovb = [st.sb([128, 4, 128], BF16, tag='ovb') for _ in range(2)]
        ovf = [st.sb([128, 4, 128], tag='ovf') for _ in range(2)]
        ntp = 0
        for k in range(12):
            pt, n_pt = pts[k % 2]
            sv, n_sv = ss[k % 2]
            P.op('pool', (lambda e, pt=pt: e.memset(pt[:, 0:1], 0.0)), writes=[n_pt])
            P.op('pool', (lambda e, pt=pt: e.memset(pt[:, Lh + 1:Lh + 2], 0.0)), writes=[n_pt])
            P.dma(pt[:, 1:Lh + 1], projT[C_HP + k * 128:C_HP + (k + 1) * 128, pos0:pos0 + Lh], reads=['projT'], writes=[n_pt])
            P.op('dve', (lambda e, pt=pt, sv=sv, k=k: e.tensor_scalar(out=sv[:], in0=pt[:, 0:Lh], scalar1=cw[:, k, 0:1], scalar2=cw[:, k, 3:4],
                                                                        op0=ALU.mult, op1=ALU.add)), reads=[n_pt, n_cw], writes=[n_sv])
            P.op('dve', (lambda e, pt=pt, sv=sv, k=k: e.scalar_tensor_tensor(out=sv[:], in0=pt[:, 1:Lh + 1], scalar=cw[:, k, 1:2], in1=sv[:],
                                                                               op0=ALU.mult, op1=ALU.add)), reads=[n_pt, n_cw, n_sv], writes=[n_sv])
            P.op('dve', (lambda e, pt=pt, sv=sv, k=k: e.scalar_tensor_tensor(out=sv[:], in0=pt[:, 2:Lh + 2], scalar=cw[:, k, 2:3], in1=sv[:],
                                                                               op0=ALU.mult, op1=ALU.add)), reads=[n_pt, n_cw, n_sv], writes=[n_sv])
            if k >= 8:
                P.dma(hx2_d[(k - 8) * 128:(k - 7) * 128, pos0:pos0 + Lh], sv[:], reads=[n_sv], writes=['hx2T'])
                continue
            for t4 in range(0, NTl, 4):
                nn = min(4, NTl - t4)
                pp, n_pp = ptp[ntp % 2]
                for i in range(nn):
                    P.op('pe', (lambda e, pp=pp, sv=sv, i=i, t4=t4: e.transpose(out=pp[:, i, :], in_=sv[:, (t4 + i) * 128:(t4 + i + 1) * 128],
                                                                                 identity=ident[:])), reads=[n_sv, n_id], writes=[n_pp])
                if k < 4:
                    ob_, n_ob = ovb[ntp % 2]
                    P.op('act', (lambda e, pp=pp, ob_=ob_, nn=nn: e.activation(out=ob_[:, :nn, :], in_=pp[:, :nn, :], func=AF.Copy)),
                         reads=[n_pp], writes=[n_ob])
                    dstv = hv_d[pos0 + t4 * 128:pos0 + (t4 + nn) * 128, k * 128:(k + 1) * 128].rearrange("(i p) c -> p i c", p=128)
                    P.dma(dstv, ob_[:, :nn, :], reads=[n_ob], writes=['hv'])
                else:
                    of_, n_of = ovf[ntp % 2]
                    P.op('act', (lambda e, pp=pp, of_=of_, nn=nn: e.activation(out=of_[:, :nn, :], in_=pp[:, :nn, :], func=AF.Copy)),
                         reads=[n_pp], writes=[n_of])
                    dstv = hx1_d[pos0 + t4 * 128:pos0 + (t4 + nn) * 128, (k - 4) * 128:(k - 3) * 128].rearrange("(i p) c -> p i c", p=128)
                    P.dma(dstv, of_[:, :nn, :], reads=[n_of], writes=['hx1'])
                ntp += 1
    with Stage(P, 'hy3%s%d' % (sfx, l)) as st:
        sig, n_sig = st.sb([128, NTl, 512], BF16)
        A_, n_A = st.sb([128, NF, 512], BF16)
        B_, n_B = st.sb([128, NF, 512], BF16)
        TW = 256 if Lh >= 256 else Lh
        dbuf = [st.sb([128, NF * TW], BF16, tag='dft') for _ in range(4)]
        srs = [st.sb([128, 512], tag='sr') for _ in range(2)]
        sis = [st.sb([128, 512], tag='si') for _ in range(2)]
        x1s = [st.sb([128, 512], tag='x1') for _ in range(2)]
        tq = [st.sb([128, 512], tag='tq') for _ in range(4)]
        pU = [st.ps([128, 512], tag='pU') for _ in range(2)]
        pV = [st.ps([128, 512], tag='pV') for _ in range(2)]
        pZ = [st.ps([128, 512], tag='pZ') for _ in range(4)]
        P.dma_k(sig, hv_d[pos0:pos0 + Lh, :].rearrange("(k p) c -> p k c", p=128), NTl, reads=['hv'], writes=[n_sig])
        for order in range(2):
            for kf in range(NF):
                cb, n_cb = dbuf[(kf % 2) * 2]
                sb_, n_sb = dbuf[(kf % 2) * 2 + 1]
                ct = cb[:, 0:NTl * 128].rearrange("p (k f) -> p k f", f=128)
                stt = sb_[:, 0:NTl * 128].rearrange("p (k f) -> p k f", f=128)
                P.dma_k(ct, Cv[:, 0:NTl, kf * 128:(kf + 1) * 128], NTl, writes=[n_cb])
                P.dma_k(stt, Sv[:, 0:NTl, kf * 128:(kf + 1) * 128], NTl, writes=[n_sb])
                sr, n_sr = srs[kf % 2]
                si, n_si = sis[kf % 2]
                P.dma(sr[:], Sr_d[kf * 128:(kf + 1) * 128, order * 512:(order + 1) * 512], reads=['Sr'], writes=[n_sr])
                P.dma(si[:], Si_d[kf * 128:(kf + 1) * 128, order * 512:(order + 1) * 512], reads=['Si'], writes=[n_si])
                pu, n_pu = pU[kf % 2]
                pv, n_pv = pV[kf % 2]
                for kt in range(NTl):
                    P.op('pe', (lambda e, pu=pu, ct=ct, kt=kt: e.matmul(pu[:], lhsT=ct[:, kt, :], rhs=sig[:, kt, :], start=(kt == 0), stop=(kt == NTl - 1))),
                         reads=[n_cb, n_sig], writes=[n_pu])
                for kt in range(NTl):
                    P.op('pe', (lambda e, pv=pv, stt=stt, kt=kt: e.matmul(pv[:], lhsT=stt[:, kt, :], rhs=sig[:, kt, :], start=(kt == 0), stop=(kt == NTl - 1))),
                         reads=[n_sb, n_sig], writes=[n_pv])
                t1, t2, t3, t4 = tq
                P.op('dve', (lambda e, pu=pu, sr=sr: e.tensor_tensor(out=t1[0][:], in0=pu[:], in1=sr[:], op=ALU.mult)), reads=[n_pu, n_sr], writes=[t1[1]])
                P.op('dve', (lambda e, pv=pv, si=si: e.tensor_tensor(out=t2[0][:], in0=pv[:], in1=si[:], op=ALU.mult)), reads=[n_pv, n_si], writes=[t2[1]])
                P.op('pool', (lambda e, kf=kf: e.tensor_tensor(out=A_[:, kf, :], in0=t1[0][:], in1=t2[0][:], op=ALU.add)), reads=[t1[1], t2[1]], writes=[n_A])
                P.op('dve', (lambda e, pv=pv, sr=sr: e.tensor_tensor(out=t3[0][:], in0=pv[:], in1=sr[:], op=ALU.mult)), reads=[n_pv, n_sr], writes=[t3[1]])
                P.op('dve', (lambda e, pu=pu, si=si: e.tensor_tensor(out=t4[0][:], in0=pu[:], in1=si[:], op=ALU.mult)), reads=[n_pu, n_si], writes=[t4[1]])
                P.op('pool', (lambda e, kf=kf: e.tensor_tensor(out=B_[:, kf, :], in0=t3[0][:], in1=t4[0][:], op=ALU.subtract)), reads=[t3[1], t4[1]], writes=[n_B])
            if order == 0:
                for kt in range(NTl):
                    cb, n_cb = dbuf[(kt % 2) * 2]
                    sb_, n_sb = dbuf[(kt % 2) * 2 + 1]
                    ct = cb[:, 0:NF * 128].rearrange("p (k f) -> p k f", f=128)
                    stt = sb_[:, 0:NF * 128].rearrange("p (k f) -> p k f", f=128)
                    P.dma_k(ct, Cv[:, 0:NF, kt * 128:(kt + 1) * 128], NF, writes=[n_cb])
                    P.dma_k(stt, Sv[:, 0:NF, kt * 128:(kt + 1) * 128], NF, writes=[n_sb])
                    x1, n_x1 = x1s[kt % 2]
                    P.dma(x1[:], hx1_d[pos0 + kt * 128:pos0 + (kt + 1) * 128, :], reads=['hx1'], writes=[n_x1])
                    pz, n_pz = pZ[kt % 4]
                    for kf in range(NF):
                        P.op('pe', (lambda e, pz=pz, ct=ct, kf=kf: e.matmul(pz[:], lhsT=ct[:, kf, :], rhs=A_[:, kf, :], start=(kf == 0), stop=False)),
                             reads=[n_cb, n_A], writes=[n_pz])
                    for kf in range(NF):
                        P.op('pe', (lambda e, pz=pz, stt=stt, kf=kf: e.matmul(pz[:], lhsT=stt[:, kf, :], rhs=B_[:, kf, :], start=False, stop=(kf == NF - 1))),
                             reads=[n_sb, n_B], writes=[n_pz])
                    P.op('dve', (lambda e, pz=pz, x1=x1, kt=kt: e.tensor_tensor(out=sig[:, kt, :], in0=pz[:], in1=x1[:], op=ALU.mult)),
                         reads=[n_pz, n_x1], writes=[n_sig])
            else:
                x2s = [st.sb([128, 4, TW], tag='x2') for _ in range(2)]
                gts = [st.sb([128, 4, TW], tag='g') for _ in range(2)]
                obs = [st.sb([128, 4, TW], BF16, tag='ob') for _ in range(2)]
                x2v = hx2_d.rearrange("(k p) t -> p k t", p=128)
                gv = projT[C_HG:C_HG + 512, :].rearrange("(k p) t -> p k t", p=128)
                ov = yT[1536:2048, :].rearrange("(k p) t -> p k t", p=128)
                for gi, (g0, gn) in enumerate(tiles_of(Lh, TW)):
                    cb, n_cb = dbuf[(gi % 2) * 2]
                    sb_, n_sb = dbuf[(gi % 2) * 2 + 1]
                    ct = cb[:, 0:NF * gn].rearrange("p (k t) -> p k t", t=gn)
                    stt = sb_[:, 0:NF * gn].rearrange("p (k t) -> p k t", t=gn)
                    P.dma_k(ct, Cv[:, 0:NF, g0:g0 + gn], NF, writes=[n_cb])
                    P.dma_k(stt, Sv[:, 0:NF, g0:g0 + gn], NF, writes=[n_sb])
                    x2, n_x2 = x2s[gi % 2]
                    gt, n_gt = gts[gi % 2]
                    ob_, n_ob = obs[gi % 2]
                    P.dma(x2[:, :, :gn], x2v[:, :, pos0 + g0:pos0 + g0 + gn], reads=['hx2T'], writes=[n_x2])
                    P.dma(gt[:, :, :gn], gv[:, :, pos0 + g0:pos0 + g0 + gn], reads=['projT'], writes=[n_gt])
                    P.op('act', (lambda e, gt=gt, gn=gn: e.activation(out=gt[:, :, :gn], in_=gt[:, :, :gn], func=AF.Silu)), reads=[n_gt], writes=[n_gt])
                    P.op('pool', (lambda e, gt=gt, x2=x2, gn=gn: e.tensor_tensor(out=gt[:, :, :gn], in0=gt[:, :, :gn], in1=x2[:, :, :gn], op=ALU.mult)),
                         reads=[n_gt, n_x2], writes=[n_gt])
                    for cq in range(4):
                        pz, n_pz = pZ[cq]
                        for kf in range(NF):
                            P.op('pe', (lambda e, pz=pz, ct=ct, kf=kf, cq=cq, gn=gn: e.matmul(
                                pz[:, :gn], lhsT=A_[:, kf, cq * 128:(cq + 1) * 128], rhs=ct[:, kf, :], start=(kf == 0), stop=False)),
                                 reads=[n_cb, n_A], writes=[n_pz])
                        for kf in range(NF):
                            P.op('pe', (lambda e, pz=pz, stt=stt, kf=kf, cq=cq, gn=gn: e.matmul(
                                pz[:, :gn], lhsT=B_[:, kf, cq * 128:(cq + 1) * 128], rhs=stt[:, kf, :], start=False, stop=(kf == NF - 1))),
                                 reads=[n_sb, n_B], writes=[n_pz])
                        P.op('dve', (lambda e, pz=pz, gt=gt, ob_=ob_, cq=cq, gn=gn: e.tensor_tensor(
                            out=ob_[:, cq, :gn], in0=pz[:, :gn], in1=gt[:, cq, :gn], op=ALU.mult)), reads=[n_pz, n_gt], writes=[n_ob])
                    P.dma(ov[:, :, pos0 + g0:pos0 + g0 + gn], ob_[:, :, :gn], reads=[n_ob], writes=['yT'])


def stage_hyena(P, cfg, l, io, projT, yT, scr):
    L, LC = cfg['L'], cfg['LC']
    if l < cfg['DEPTH'] - 1:
        hyena_run(P, cfg, l, io, projT, yT, scr, LC, 0, 'C')
    hyena_run(P, cfg, l, io, projT, yT, scr, L, LC, 'L')


INPUT_SPECS = None


def input_shapes(cfg):
    L, LC, D, DEPTH = cfg['L'], cfg['LC'], cfg['D'], cfg['DEPTH']
    return {
        'x': ([L, D], F32), 'ctx': ([LC, D], F32), 'ccT': ([128, D // 128, 2], F32),
        'w_mod': ([DEPTH, D, 3 * D], F32), 'b_mod2': ([DEPTH, 2, 3 * D], F32),
        'g_pre_rep': ([DEPTH, 128, D], F32), 'g_post_rep': ([DEPTH, 128, D], F32),
        'w_in': ([DEPTH, D, NCOL], F32), 'w_out': ([DEPTH, 2048, D], F32),
        'ident': ([128, 128], BF16), 'sel2': ([2, 2, 128], F32),
        'maskf': ([128, 128], BF16), 'maskb': ([128, 128], BF16),
        'gla_wg': ([DEPTH, 16, 2, 512], F32), 'gla_bT': ([DEPTH, 128, 8], F32), 'gla_nT': ([DEPTH, 128, 2], F32),
        'ident32': ([128, 128], F32), 'iota': ([128, 1024], F32),
        's5_sc': ([DEPTH, 128, 2, 16, 3], F32), 's5_cpad': ([DEPTH, 128, 2, 16, 2, 128], F32),
        's5_bpad': ([DEPTH, 128, 2, 16, 2, 128], F32), 's5_dT': ([DEPTH, 128, 4], F32), 's5_bgT': ([DEPTH, 128, 4], F32),
        's5_w_glu': ([DEPTH, 512, 512], F32),
        'hy_w1': ([DEPTH, 33, 64], F32), 'hy_w2': ([DEPTH, 64, 64], F32), 'hy_w3': ([DEPTH, 64, 2048], F32),
        'hy_fb': ([DEPTH, 64, 4], F32), 'hy_cw': ([DEPTH, 128, 12, 4], F32), 'hy_d_rep': ([DEPTH, 128, 1024], F32),
        'featsT_L': ([33, L], F32), 'featsT_C': ([33, LC], F32), 'dec_L': ([L, 512], F32), 'dec_C': ([LC, 512], F32),
        'dftC_L': ([L + 128, L + 128], BF16), 'dftS_L': ([L + 128, L + 128], BF16),
        'dftC_C': ([LC + 128, LC + 128], BF16), 'dftS_C': ([LC + 128, LC + 128], BF16),
        'wgt_L': ([128, L // 128 + 1], F32), 'wgt_C': ([128, LC // 128 + 1], F32),
    }


PER_BATCH = ('x', 'ctx', 'ccT')


def build_program(cfg, mixers=('gla', 's5', 'hy'), debug=False, nb=1):
    L, LC, D, DEPTH = cfg['L'], cfg['LC'], cfg['D'], cfg['DEPTH']
    LT = L + LC
    nc = bass.Bass("TRN2", target_bir_lowering=False)
    io = {}
    for name, (shape, dt) in input_shapes(cfg).items():
        if name in PER_BATCH:
            for b in range(nb):
                io['%s%d' % (name, b)] = nc.dram_tensor('%s%d' % (name, b), list(shape), dt, kind="ExternalInput").ap()
        else:
            io[name] = nc.dram_tensor(name, list(shape), dt, kind="ExternalInput").ap()
    outs = [nc.dram_tensor("out%d" % b, [L, D], F32, kind="ExternalOutput").ap() for b in range(nb)]
    with ExitStack() as es:
        P = Prog(nc, es)
        res = [P.dram('res%d' % i, [L, D]) for i in range(2)]
        cres = [P.dram('cres%d' % i, [LC, D]) for i in range(2)]
        rep = [P.dram('rep%d' % i, [128, D]) for i in range(6)]
        projT = P.dram('projT', [NCOL, LT], out=debug)
        projK = P.dram('projK', [LT, 1536], out=debug)
        yT = P.dram('yT', [2048, LT], BF16, out=debug)
        ygT = P.dram('ygT', [512, LT], F32, out=debug)
        scr = {'Sr': P.dram('hSr', [L + 128, 1024]), 'Si': P.dram('hSi', [L + 128, 1024]),
               'hv': P.dram('hv', [LT, 512], BF16), 'hx1': P.dram('hx1', [LT, 512]), 'hx2T': P.dram('hx2T', [512, LT])}
        for b in range(nb):
            iob = dict(io)
            for name in PER_BATCH:
                iob[name] = io['%s%d' % (name, b)]
            for l in range(DEPTH):
                src_lat = iob['x'] if l == 0 else res[(l - 1) % 2]
                src_ctx = iob['ctx'] if l == 0 else cres[(l - 1) % 2]
                dst_lat = outs[b] if l == DEPTH - 1 else res[l % 2]
                dst_ctx = cres[l % 2]
                stage_mod(P, cfg, l, iob, rep)
                stage_inproj(P, cfg, l, iob, rep, src_lat, src_ctx, projT, projK)
                if mixers == 'stub':
                    stage_stub_mixer(P, cfg, l, projT, yT)
                else:
                    if 'gla' in mixers:
                        stage_gla(P, cfg, l, iob, projT, projK, yT)
                    if 's5' in mixers:
                        stage_s5(P, cfg, l, iob, projT, ygT, yT)
                    if 'hy' in mixers:
                        stage_hyena(P, cfg, l, iob, projT, yT, scr)
                stage_outproj(P, cfg, l, iob, rep, yT, src_lat, src_ctx, dst_lat, dst_ctx)
    return nc


_HC = {}


def hyena_consts(cfg):
    key = (cfg['L'], cfg['LC'])
    if key in _HC:
        return _HC[key]
    out = {}
    f32 = np.float32
    for sfx, Lh in (('L', cfg['L']), ('C', cfg['LC'])):
        t = np.linspace(0.0, 1.0, Lh, dtype=f32)[:, None]
        ang = (f32(2.0 * math.pi / Lh) * np.arange(Lh, dtype=f32))[:, None]
        bands = np.linspace(1e-4, 15.0, 16, dtype=f32)[None, :]
        feats = np.concatenate([t, np.cos(bands * ang), -np.sin(bands * ang)], axis=-1).astype(f32)
        out['featsT_' + sfx] = np.ascontiguousarray(feats.T)
        deltas = np.abs(np.linspace(math.log(1e-2) / 0.3, math.log(1e-2) / 1.5, 512, dtype=f32))
        out['dec_' + sfx] = np.exp(-t * deltas[None, :]).astype(f32)
        N = 2 * Lh
        FP = Lh + 128
        a = np.arange(Lh + 1, dtype=np.int64)
        prod = (a[:, None] * a[None, :]) % N
        angm = prod.astype(np.float64) * (2.0 * math.pi / N)
        C = np.zeros((FP, FP), np.float32)
        S = np.zeros((FP, FP), np.float32)
        C[:Lh + 1, :Lh + 1] = np.cos(angm)
        S[:Lh + 1, :Lh + 1] = np.sin(angm)
        out['dftC_' + sfx] = C.astype(ml_dtypes.bfloat16)
        out['dftS_' + sfx] = S.astype(ml_dtypes.bfloat16)
        w = np.zeros(FP, np.float32)
        w[:Lh + 1] = 2.0 / N
        w[0] = 1.0 / N
        w[Lh] = 1.0 / N
        out['wgt_' + sfx] = np.ascontiguousarray(w.reshape(FP // 128, 128).T)
    _HC[key] = out
    return out


def prep_inputs(cfg, b, inp):
    D, DEPTH = cfg['D'], cfg['DEPTH']
    f = lambda a: np.ascontiguousarray(np.asarray(a), dtype=np.float32)
    m = {}
    m['x'] = f(inp['x'][b])
    m['ctx'] = f(inp['ctx'][b])
    cc = np.stack([np.asarray(inp['c'][b]), np.asarray(inp['c_ctx'])], axis=-1)
    m['ccT'] = f(cc.reshape(D // 128, 128, 2).transpose(1, 0, 2))
    m['w_mod'] = f(inp['w_mod'])
    m['b_mod2'] = f(np.repeat(np.asarray(inp['b_mod'])[:, None, :], 2, axis=1))
    m['g_pre_rep'] = f(np.repeat(np.asarray(inp['g_pre'])[:, None, :], 128, axis=1))
    m['g_post_rep'] = f(np.repeat(np.asarray(inp['g_post'])[:, None, :], 128, axis=1))
    m['w_in'] = f(inp['w_in'])
    m['w_out'] = f(inp['w_out'])
    m['ident'] = np.eye(128, dtype=np.float32).astype(ml_dtypes.bfloat16)
    sel = np.zeros((2, 2, 128), np.float32)
    sel[0, 0, :] = 1.0
    sel[1, 1, :] = 1.0
    m['sel2'] = sel
    ii = np.arange(128)
    m['maskf'] = (ii[:, None] <= ii[None, :]).astype(np.float32).astype(ml_dtypes.bfloat16)
    m['maskb'] = (ii[:, None] + ii[None, :] >= 127).astype(np.float32).astype(ml_dtypes.bfloat16)
    m['gla_wg'] = f(np.asarray(inp['gla_w_gate']).transpose(0, 2, 1, 3))
    bg = np.asarray(inp['gla_b_gate']).reshape(DEPTH, 2, 4, 128)
    m['gla_bT'] = f(bg.transpose(0, 3, 1, 2).reshape(DEPTH, 128, 8))
    m['gla_nT'] = f(np.asarray(inp['gla_norm']).reshape(DEPTH, 2, 128).transpose(0, 2, 1))
    m['ident32'] = np.eye(128, dtype=np.float32)
    m['iota'] = f(np.repeat(np.arange(1024, dtype=np.float32)[None, :], 128, axis=0))

    def st_layout(a):
        a = np.asarray(a)
        sh = a.shape
        a = a.reshape(sh[0], 2, 16, 2, 64, *sh[4:])
        nd = a.ndim
        a = a.transpose(0, 3, 4, 1, 2, *range(5, nd))
        return a.reshape(sh[0], 128, 2, 16, *sh[4:])
    ls = np.broadcast_to(np.asarray(inp['s5_log_step'])[..., None], np.asarray(inp['s5_lam_re']).shape)
    m['s5_sc'] = f(np.stack([st_layout(inp['s5_lam_re']), st_layout(inp['s5_lam_im']), st_layout(ls)], axis=-1))

    def pad_layout(re, im):
        a = np.stack([st_layout(re), st_layout(im)], axis=4)
        out = np.zeros((DEPTH, 128, 2, 16, 2, 128), np.float32)
        for s_ in range(16):
            for gl in range(2):
                cb = 16 * (2 * (s_ % 4) + gl)
                out[:, gl * 64:(gl + 1) * 64, :, s_, :, cb:cb + 16] = a[:, gl * 64:(gl + 1) * 64, :, s_, :, :]
        return out
    m['s5_bpad'] = pad_layout(inp['s5_b_re'], inp['s5_b_im'])
    m['s5_cpad'] = pad_layout(np.asarray(inp['s5_c_re']).transpose(0, 1, 2, 4, 3), np.asarray(inp['s5_c_im']).transpose(0, 1, 2, 4, 3))
    m['s5_dT'] = f(np.asarray(inp['s5_d']).reshape(DEPTH, 4, 128).transpose(0, 2, 1))
    m['s5_bgT'] = f(np.asarray(inp['s5_b_glu']).reshape(DEPTH, 4, 128).transpose(0, 2, 1))
    m['s5_w_glu'] = f(inp['s5_w_glu'])
    m['hy_w1'] = f(inp['hy_w1'])
    m['hy_w2'] = f(inp['hy_w2'])
    m['hy_w3'] = f(inp['hy_w3'])
    m['hy_fb'] = f(np.stack([np.asarray(inp['hy_f1']), np.asarray(inp['hy_b1']), np.asarray(inp['hy_f2']), np.asarray(inp['hy_b2'])], axis=-1))
    cw = np.concatenate([np.asarray(inp['hy_conv_w']), np.asarray(inp['hy_conv_b'])[:, None, :]], axis=1)
    m['hy_cw'] = f(cw.reshape(DEPTH, 4, 12, 128).transpose(0, 3, 2, 1))
    m['hy_d_rep'] = f(np.repeat(np.asarray(inp['hy_d']).reshape(DEPTH, 1, 1024), 128, axis=1))
    m.update(hyena_consts(cfg))
    return m


def kernel(**inputs):
    cfg = CFG
    B = inputs['x'].shape[0]
    nc = build_program(cfg, nb=B)
    m = None
    for b in range(B):
        mb = prep_inputs(cfg, b, inputs)
        if m is None:
            m = {k: v for k, v in mb.items() if k not in PER_BATCH}
        for k in PER_BATCH:
            m['%s%d' % (k, b)] = mb[k]
    res = run_bass_kernel_spmd(nc, [m], core_ids=[0])
    return np.stack([np.asarray(res.results[0]['out%d' % b]) for b in range(B)], axis=0).astype(np.float32)
```
